# Optimizing a Trainium2 kernel written in Bass

```python
import math
import jax
import jax.numpy as jnp
from jax import lax
import numpy as np

D_MODEL = 4096
BATCH = 4
SEQ = 4096
DEPTH = 2

CTX_LEN = 256
GRID_W = 64
N_MOD = 6
NORM_EPS = 1e-6

POOL_WINDOWS = (2, 4, 8, 16)
POOL_GROUPS = 4
POOL_GROUP_DIM = D_MODEL // 16
POOL_DIM = POOL_GROUPS * POOL_GROUP_DIM

GDN_HEAD_DIM = 128
GDN_DIM = D_MODEL - POOL_DIM
GDN_HEADS = GDN_DIM // GDN_HEAD_DIM
GDN_CONV = 5
GDN_CHUNK = 64

FOURIER_GROUPS = 4
FOURIER_GROUP_DIM = D_MODEL // 16
FOURIER_DIM = FOURIER_GROUPS * FOURIER_GROUP_DIM

GLA_HEADS = 6
GLA_V_DIM = D_MODEL - FOURIER_DIM
GLA_K_DIM = GLA_V_DIM // 2
GLA_HEAD_K = GLA_K_DIM // GLA_HEADS
GLA_HEAD_V = GLA_V_DIM // GLA_HEADS
GLA_GATE_RANK = 16
GLA_GATE_TEMP = 16.0
GLA_CHUNK = 32

MOE_GROUPS = 4
MOE_EXPERTS_PER_GROUP = 8
MOE_EXPERTS = MOE_GROUPS * MOE_EXPERTS_PER_GROUP
MOE_TOP_K = 2
MOE_HIDDEN = D_MODEL // 8
MOE_BLOCK = 128

AB_SPLITS = (POOL_DIM, POOL_DIM + 3 * GDN_DIM, POOL_DIM + 4 * GDN_DIM, POOL_DIM + 4 * GDN_DIM + 2 * GDN_HEADS)
AB_IN = POOL_DIM + 4 * GDN_DIM + 4 * GDN_HEADS
CD_SPLITS = (FOURIER_DIM, FOURIER_DIM + GLA_K_DIM, FOURIER_DIM + 2 * GLA_K_DIM,
             FOURIER_DIM + 2 * GLA_K_DIM + GLA_V_DIM, FOURIER_DIM + 2 * GLA_K_DIM + 2 * GLA_V_DIM)
CD_IN = FOURIER_DIM + 2 * GLA_K_DIM + 2 * GLA_V_DIM + 2 * GLA_GATE_RANK

kernel_name = 'hybrid_pool_gdn_fourier_gla_hmoe_dit'


def rms_norm(x, w):
    x32 = x.astype(jnp.float32)
    y = x32 * lax.rsqrt(jnp.mean(x32 * x32, axis=-1, keepdims=True) + NORM_EPS)
    return (y * w.astype(jnp.float32)).astype(x.dtype)


def modulate(h, shift, scale):
    return h * (1 + scale) + shift


def l2_normalise(t):
    t32 = t.astype(jnp.float32)
    return t32 * lax.rsqrt(jnp.sum(t32 * t32, axis=-1, keepdims=True) + NORM_EPS)


def split_heads(t, n_heads):
    b, n, _ = t.shape
    return t.reshape(b, n, n_heads, -1).transpose(0, 2, 1, 3)


def gated_head_norm(o, z, w):
    b, h, n, dv = o.shape
    y = rms_norm(o.transpose(0, 2, 1, 3), w) * jax.nn.silu(z.reshape(b, n, h, dv).astype(jnp.float32))
    return y.reshape(b, n, h * dv).astype(z.dtype)


def centred_conv(u, w):
    k = w.shape[0]
    pad = k // 2
    n = u.shape[1]
    up = jnp.pad(u, ((0, 0), (pad, pad), (0, 0)))
    return sum(up[:, j:j + n] * w[j] for j in range(k))


def pool_mix(u, row_len, pool_w, pool_scale):
    b, n, _ = u.shape
    rows = n // row_len
    u5 = u.reshape(b, rows, row_len, POOL_GROUPS, POOL_GROUP_DIM).astype(jnp.float32)
    cs = jnp.pad(jnp.cumsum(u5, axis=2), ((0, 0), (0, 0), (1, 0), (0, 0), (0, 0)))
    pos = jnp.arange(row_len)[:, None]
    half = jnp.array(POOL_WINDOWS, dtype=jnp.int32)[None, :] // 2
    lo = jnp.clip(pos - half, 0, row_len - 1)
    hi = jnp.clip(pos + half - 1, 0, row_len - 1)
    g_idx = jnp.arange(POOL_GROUPS)[None, :]
    win_sum = cs[:, :, hi + 1, g_idx] - cs[:, :, lo, g_idx]
    count = (hi - lo + 1).astype(jnp.float32)[None, None, :, :, None]
    d = (win_sum / count - u5).astype(u.dtype)
    y = jnp.einsum('brwgc,gcd->brwgd', d, pool_w) * pool_scale.reshape(POOL_GROUPS, POOL_GROUP_DIM)
    return y.reshape(b, n, POOL_DIM)


def fourier_mix(u, w):
    b, n, _ = u.shape
    u4 = u.astype(jnp.float32).reshape(b, n, FOURIER_GROUPS, FOURIER_GROUP_DIM)
    f = jnp.fft.fft2(u4, axes=(1, 3), norm='ortho').real.astype(u.dtype)
    return jnp.einsum('bngc,gcd->bngd', f, w).reshape(b, n, FOURIER_DIM)


def gdn_chunked(q, k, v, beta, g, state0):
    b, h, n, dk = q.shape
    dv = v.shape[-1]
    c = GDN_CHUNK
    nc = n // c
    q, k, v = (t.astype(jnp.float32).reshape(b, h, nc, c, -1) for t in (q, k, v))
    beta = beta.astype(jnp.float32).reshape(b, h, nc, c)
    g = jnp.cumsum(g.astype(jnp.float32).reshape(b, h, nc, c), axis=-1)
    tril = jnp.tril(jnp.ones((c, c), dtype=bool))
    strict = jnp.tril(jnp.ones((c, c), dtype=bool), -1)
    decay = jnp.exp(jnp.where(tril, g[..., :, None] - g[..., None, :], -jnp.inf))
    k_beta = k * beta[..., None]
    lower = jnp.where(strict, jnp.einsum('bhncd,bhnsd->bhncs', k_beta, k) * decay, 0.0)
    rhs = jnp.concatenate([v * beta[..., None], k_beta * jnp.exp(g)[..., None]], axis=-1)
    sol = lax.linalg.triangular_solve(jnp.eye(c, dtype=jnp.float32) + lower, rhs,
                                      left_side=True, lower=True, unit_diagonal=True)
    u, w = sol[..., :dv], sol[..., dv:]
    attn = jnp.einsum('bhncd,bhnsd->bhncs', q, k) * decay
    q_dec = q * jnp.exp(g)[..., None]
    k_end = k * jnp.exp(g[..., -1:] - g)[..., None]
    g_end = jnp.exp(g[..., -1])

    def step(state, xs):
        u_i, w_i, attn_i, q_i, k_i, ge_i = xs
        v_new = u_i - jnp.einsum('bhck,bhkv->bhcv', w_i, state)
        o = jnp.einsum('bhck,bhkv->bhcv', q_i, state) + jnp.einsum('bhcs,bhsv->bhcv', attn_i, v_new)
        state = state * ge_i[..., None, None] + jnp.einsum('bhck,bhcv->bhkv', k_i, v_new)
        return state, o

    xs = tuple(jnp.moveaxis(t, 2, 0) for t in (u, w, attn, q_dec, k_end, g_end))
    state, o = lax.scan(step, state0, xs)
    return jnp.moveaxis(o, 0, 2).reshape(b, h, n, dv), state


def gla_chunked(q, k, v, gk, state0):
    b, h, n, dk = q.shape
    dv = v.shape[-1]
    c = GLA_CHUNK
    nc = n // c
    q, k, v, gk = (t.astype(jnp.float32).reshape(b, h, nc, c, -1) for t in (q, k, v, gk))
    gcum = jnp.cumsum(gk, axis=3)
    q_dec = q * jnp.exp(gcum)
    k_inv = k * jnp.exp(-gcum)
    tril = jnp.tril(jnp.ones((c, c), dtype=bool))
    attn = jnp.where(tril, jnp.einsum('bhnck,bhnsk->bhncs', q_dec, k_inv), 0.0)
    o_intra = jnp.einsum('bhncs,bhnsv->bhncv', attn, v)
    k_end = k * jnp.exp(gcum[..., -1:, :] - gcum)
    g_end = jnp.exp(gcum[..., -1, :])

    def step(state, xs):
        q_i, k_i, v_i, ge_i = xs
        o = jnp.einsum('bhck,bhkv->bhcv', q_i, state)
        state = state * ge_i[..., None] + jnp.einsum('bhck,bhcv->bhkv', k_i, v_i)
        return state, o

    xs = tuple(jnp.moveaxis(t, 2, 0) for t in (q_dec, k_end, v, g_end))
    state, o_inter = lax.scan(step, state0, xs)
    o = o_intra + jnp.moveaxis(o_inter, 0, 2)
    return o.reshape(b, h, n, dv), state


def orient(t, reverse):
    return jnp.flip(t, axis=2) if reverse else t


def bidirectional_scan(chunk_fn, ctx_dirs, lat_dirs, state0):
    o_ctx, o_lat = [], []
    for d in range(2):
        rev = d == 1
        oc, s_ctx = chunk_fn(*[orient(t, rev) for t in ctx_dirs[d]], state0)
        ol, _ = chunk_fn(*[orient(t, rev) for t in lat_dirs[d]], s_ctx)
        o_ctx.append(orient(oc, rev))
        o_lat.append(orient(ol, rev))
    return o_ctx[0] + o_ctx[1], o_lat[0] + o_lat[1]


def mixer_pool_gdn(h_ctx, h_lat, w_in, pool_w, pool_scale, conv_w, a_log, dt_bias, norm_w, w_out, want_ctx):
    n_ctx = h_ctx.shape[1]
    proj = jnp.concatenate([h_ctx, h_lat], axis=1) @ w_in
    sides = []
    for p in (proj[:, :n_ctx], proj[:, n_ctx:]):
        b, n, _ = p.shape
        pool_in, qkv, z, beta_raw, alpha_raw = jnp.split(p, AB_SPLITS, axis=-1)
        q, k, v = jnp.split(jax.nn.silu(centred_conv(qkv, conv_w)), 3, axis=-1)
        q = l2_normalise(split_heads(q, GDN_HEADS)) * (GDN_HEAD_DIM ** -0.5)
        k = l2_normalise(split_heads(k, GDN_HEADS))
        v = split_heads(v, GDN_HEADS)
        beta = jax.nn.sigmoid(beta_raw.astype(jnp.float32)).reshape(b, n, 2, GDN_HEADS).transpose(2, 0, 3, 1)
        alpha = alpha_raw.astype(jnp.float32).reshape(b, n, 2, GDN_HEADS)
        g = (-jnp.exp(a_log.astype(jnp.float32)) * jax.nn.softplus(alpha + dt_bias.astype(jnp.float32))).transpose(2, 0, 3, 1)
        sides.append((pool_in, z, [(q, k, v, beta[d], g[d]) for d in range(2)]))
    (pool_c, z_c, dirs_c), (pool_l, z_l, dirs_l) = sides
    state0 = jnp.zeros((h_lat.shape[0], GDN_HEADS, GDN_HEAD_DIM, GDN_HEAD_DIM), jnp.float32)
    o_c, o_l = bidirectional_scan(gdn_chunked, dirs_c, dirs_l, state0)
    out_l = jnp.concatenate([pool_mix(pool_l, GRID_W, pool_w, pool_scale),
                             gated_head_norm(o_l, z_l, norm_w)], axis=-1) @ w_out
    out_c = None
    if want_ctx:
        out_c = jnp.concatenate([pool_mix(pool_c, n_ctx, pool_w, pool_scale),
                                 gated_head_norm(o_c, z_c, norm_w)], axis=-1) @ w_out
    return out_c, out_l


def mixer_fourier_gla(h_ctx, h_lat, w_in, fourier_w, gate_up, gate_b, norm_w, w_out, want_ctx):
    n_ctx = h_ctx.shape[1]
    proj = jnp.concatenate([h_ctx, h_lat], axis=1) @ w_in
    sides = []
    for p in (proj[:, :n_ctx], proj[:, n_ctx:]):
        b, n, _ = p.shape
        f_in, q, k, v, zg, lr = jnp.split(p, CD_SPLITS, axis=-1)
        q = split_heads(q, GLA_HEADS) * (GLA_HEAD_K ** -0.5)
        k = split_heads(k, GLA_HEADS)
        v = split_heads(v, GLA_HEADS)
        lr = lr.reshape(b, n, 2, GLA_GATE_RANK)
        gk = jax.nn.log_sigmoid((jnp.einsum('bndr,drk->dbnk', lr, gate_up)
                                 + gate_b[:, None, None, :]).astype(jnp.float32)) / GLA_GATE_TEMP
        sides.append((f_in, zg, [(q, k, v, split_heads(gk[d], GLA_HEADS)) for d in range(2)]))
    (f_c, z_c, dirs_c), (f_l, z_l, dirs_l) = sides
    state0 = jnp.zeros((h_lat.shape[0], GLA_HEADS, GLA_HEAD_K, GLA_HEAD_V), jnp.float32)
    o_c, o_l = bidirectional_scan(gla_chunked, dirs_c, dirs_l, state0)
    out_l = jnp.concatenate([fourier_mix(f_l, fourier_w), gated_head_norm(o_l, z_l, norm_w)], axis=-1) @ w_out
    out_c = None
    if want_ctx:
        out_c = jnp.concatenate([fourier_mix(f_c, fourier_w), gated_head_norm(o_c, z_c, norm_w)], axis=-1) @ w_out
    return out_c, out_l


def hier_moe(h, group_w, group_b, expert_w, expert_b, w1, w3, w2):
    t, d = h.shape
    rows = jnp.arange(t)
    group_logits = (h @ group_w + group_b).astype(jnp.float32)
    group = jnp.argmax(group_logits, axis=-1)
    p_group = jax.nn.softmax(group_logits, axis=-1)[rows, group][:, None]
    expert_logits = jnp.einsum('td,gde->tge', h, expert_w) + expert_b
    sel = expert_logits[rows, group].astype(jnp.float32)
    top_val, top_idx = lax.top_k(sel, MOE_TOP_K)
    weight = (p_group * jax.nn.softmax(top_val, axis=-1)).reshape(-1).astype(h.dtype)
    expert = (group[:, None] * MOE_EXPERTS_PER_GROUP + top_idx).reshape(-1)
    token = jnp.repeat(rows, MOE_TOP_K)
    tk = t * MOE_TOP_K
    order = jnp.argsort(expert)
    e_s, tok_s, w_s = expert[order], token[order], weight[order]
    counts = jnp.bincount(expert, length=MOE_EXPERTS)
    start = jnp.cumsum(counts) - counts
    padded = (counts + MOE_BLOCK - 1) // MOE_BLOCK * MOE_BLOCK
    pad_end = jnp.cumsum(padded)
    pad_start = pad_end - padded
    dest = pad_start[e_s] + jnp.arange(tk) - start[e_s]
    n_blocks = -(-tk // MOE_BLOCK) + MOE_EXPERTS
    xbuf = jnp.zeros((n_blocks * MOE_BLOCK, d), h.dtype).at[dest].set(h[tok_s])
    block_expert = jnp.minimum(jnp.searchsorted(pad_end, jnp.arange(n_blocks) * MOE_BLOCK, side='right'),
                               MOE_EXPERTS - 1)

    def expert_block(args):
        xb, e = args
        return (jax.nn.silu(xb @ w1[e]) * (xb @ w3[e])) @ w2[e]

    ybuf = lax.map(expert_block, (xbuf.reshape(n_blocks, MOE_BLOCK, d), block_expert)).reshape(-1, d)
    return jnp.zeros_like(h).at[tok_s].add(ybuf[dest] * w_s[:, None])


def setup_inputs(seed: int = 0) -> dict:
    key = jax.random.key(seed)
    keys = iter(jax.random.split(key, 40))

    def normal(shape, scale):
        return scale * jax.random.normal(next(keys), shape, jnp.float32)

    def gain(shape):
        return 1.0 + 0.05 * jax.random.normal(next(keys), shape, jnp.float32)

    ne, no = (DEPTH + 1) // 2, DEPTH // 2
    d = D_MODEL
    a_log = jnp.log(jax.random.uniform(next(keys), (ne, 2, GDN_HEADS), jnp.float32, 1.0, 16.0))
    dt = jnp.exp(jax.random.uniform(next(keys), (ne, 2, GDN_HEADS), jnp.float32, math.log(1e-3), math.log(1e-1)))
    dt_bias = dt + jnp.log(-jnp.expm1(-dt))
    return {
        'x': normal((BATCH, SEQ, d), 1.0),
        'c': normal((BATCH, d), 1.0),
        'ctx': normal((BATCH, CTX_LEN, d), 1.0),
        'c_ctx': normal((d,), 1.0),
        'mod_w': normal((DEPTH, d, N_MOD * d), 0.5 * d ** -0.5),
        'mod_b': normal((DEPTH, N_MOD * d), 0.02),
        'norm1_w': gain((DEPTH, d)),
        'norm2_w': gain((DEPTH, d)),
        'ab_w_in': normal((ne, d, AB_IN), d ** -0.5),
        'pool_w': normal((ne, POOL_GROUPS, POOL_GROUP_DIM, POOL_GROUP_DIM), POOL_GROUP_DIM ** -0.5),
        'pool_scale': gain((ne, POOL_DIM)),
        'gdn_conv_w': normal((ne, GDN_CONV, 3 * GDN_DIM), GDN_CONV ** -0.5),
        'gdn_a_log': a_log,
        'gdn_dt_bias': dt_bias,
        'gdn_norm_w': gain((ne, GDN_HEAD_DIM)),
        'ab_w_out': normal((ne, d, d), d ** -0.5),
        'cd_w_in': normal((no, d, CD_IN), d ** -0.5),
        'fourier_w': normal((no, FOURIER_GROUPS, FOURIER_GROUP_DIM, FOURIER_GROUP_DIM), FOURIER_GROUP_DIM ** -0.5),
        'gla_gate_up': normal((no, 2, GLA_GATE_RANK, GLA_K_DIM), GLA_GATE_RANK ** -0.5),
        'gla_gate_b': normal((no, 2, GLA_K_DIM), 0.1),
        'gla_norm_w': gain((no, GLA_HEAD_V)),
        'cd_w_out': normal((no, d, d), d ** -0.5),
        'moe_group_w': normal((DEPTH, d, MOE_GROUPS), d ** -0.5),
        'moe_group_b': normal((DEPTH, MOE_GROUPS), 0.01),
        'moe_expert_w': normal((DEPTH, MOE_GROUPS, d, MOE_EXPERTS_PER_GROUP), d ** -0.5),
        'moe_expert_b': normal((DEPTH, MOE_GROUPS, MOE_EXPERTS_PER_GROUP), 0.01),
        'moe_w1': normal((DEPTH, MOE_EXPERTS, d, MOE_HIDDEN), d ** -0.5),
        'moe_w3': normal((DEPTH, MOE_EXPERTS, d, MOE_HIDDEN), d ** -0.5),
        'moe_w2': normal((DEPTH, MOE_EXPERTS, MOE_HIDDEN, d), MOE_HIDDEN ** -0.5),
        'final_norm_w': gain((d,)),
    }


def reference(x, c, ctx, c_ctx, mod_w, mod_b, norm1_w, norm2_w, ab_w_in, pool_w, pool_scale, gdn_conv_w,
              gdn_a_log, gdn_dt_bias, gdn_norm_w, ab_w_out, cd_w_in, fourier_w, gla_gate_up, gla_gate_b,
              gla_norm_w, cd_w_out, moe_group_w, moe_group_b, moe_expert_w, moe_expert_b, moe_w1, moe_w3,
              moe_w2, final_norm_w):
    d = x.shape[-1]
    x_lat, x_ctx = x, ctx
    for i in range(DEPTH):
        last = i == DEPTH - 1
        j = i // 2
        mod_l = (jax.nn.silu(c) @ mod_w[i] + mod_b[i])[:, None, :]
        mod_c = (jax.nn.silu(c_ctx) @ mod_w[i] + mod_b[i])[None, None, :]
        sh1_l, sc1_l, g1_l, sh2_l, sc2_l, g2_l = jnp.split(mod_l, N_MOD, axis=-1)
        sh1_c, sc1_c, g1_c, sh2_c, sc2_c, g2_c = jnp.split(mod_c, N_MOD, axis=-1)
        h_l = modulate(rms_norm(x_lat, norm1_w[i]), sh1_l, sc1_l)
        h_c = modulate(rms_norm(x_ctx, norm1_w[i]), sh1_c, sc1_c)
        if i % 2 == 0:
            m_c, m_l = mixer_pool_gdn(h_c, h_l, ab_w_in[j], pool_w[j], pool_scale[j], gdn_conv_w[j],
                                      gdn_a_log[j], gdn_dt_bias[j], gdn_norm_w[j], ab_w_out[j], not last)
        else:
            m_c, m_l = mixer_fourier_gla(h_c, h_l, cd_w_in[j], fourier_w[j], gla_gate_up[j], gla_gate_b[j],
                                         gla_norm_w[j], cd_w_out[j], not last)
        x_lat = x_lat + g1_l * m_l
        f_l = modulate(rms_norm(x_lat, norm2_w[i]), sh2_l, sc2_l).reshape(-1, d)
        moe_args = (moe_group_w[i], moe_group_b[i], moe_expert_w[i], moe_expert_b[i], moe_w1[i], moe_w3[i], moe_w2[i])
        if last:
            x_lat = x_lat + g2_l * hier_moe(f_l, *moe_args).reshape(x_lat.shape)
        else:
            x_ctx = x_ctx + g1_c * m_c
            f_c = modulate(rms_norm(x_ctx, norm2_w[i]), sh2_c, sc2_c).reshape(-1, d)
            y = hier_moe(jnp.concatenate([f_c, f_l], axis=0), *moe_args)
            n_c = f_c.shape[0]
            x_ctx = x_ctx + g2_c * y[:n_c].reshape(x_ctx.shape)
            x_lat = x_lat + g2_l * y[n_c:].reshape(x_lat.shape)
    return rms_norm(x_lat, final_norm_w)
```

```python
import numpy as np
from contextlib import ExitStack
import concourse.bass as bass
import concourse.mybir as mybir
from concourse.bass_utils import run_bass_kernel_spmd

F32 = mybir.dt.float32
BF16 = mybir.dt.bfloat16
I32 = mybir.dt.int32
AF = mybir.ActivationFunctionType
ALU = mybir.AluOpType
AX = mybir.AxisListType

D = 4096
KC = 32
NCTX = 256
NLAT = 4096
NTOK = NCTX + NLAT
NT = NTOK // 128
EPS = 1e-6

EPOCH = 30000
NDMA = 12


PSUM_NAMES = {"ps0", "psm", "pst", "pT", "pg", "pst4", "pl2", "ptm", "bank6", "gbcb", "bank"}


def is_psum_key(k):
    name = k if isinstance(k, str) else k[0]
    return name in PSUM_NAMES


class Prog:
    def __init__(self, nc, es):
        self.nc = nc
        self.es = es
        self.ops = []
        self.nt = 0

    def sb(self, shape, dt=F32, name=None):
        self.nt += 1
        return self.es.enter_context(self.nc.sbuf_tensor(name or f"sb{self.nt}", list(shape), dt))

    def ps(self, shape, dt=F32, name=None):
        self.nt += 1
        return self.es.enter_context(self.nc.psum_tensor(name or f"ps{self.nt}", list(shape), dt))

    def dram(self, name, shape, dt=F32, kind="Internal"):
        return self.nc.dram_tensor(name, list(shape), dt, kind=kind).ap()

    limit = None

    def op(self, eng, fn, r=(), w=()):
        if self.limit is not None and len(self.ops) >= self.limit:
            return
        self.ops.append([eng, fn, tuple(r), tuple(w), False])

    def dma(self, q, out, in_, r=(), w=(), **kw):
        if self.limit is not None and len(self.ops) >= self.limit:
            return
        self.ops.append([q, (lambda e, o=out, i=in_, k=kw: e.dma_start(out=o, in_=i, **k)),
                         tuple(r), tuple(w), True])

    def dmaop(self, q, fn, r=(), w=()):
        self.ops.append([q, fn, tuple(r), tuple(w), True])

    def barrier(self):
        self.ops.append(["*", None, (), (), False])

    def build(self):
        nc = self.nc
        ops = self.ops
        engs = ["pe", "act", "dve", "pool", "sp"]
        seq = {e: 0 for e in engs}
        dcount = {e: 0 for e in engs}
        dsem_uses = {}
        last_w = {}
        readers = {}
        plan = []
        waited = {e: {} for e in engs}
        done_tok = []
        latest = {}
        for idx, (eng, fn, r, w, is_dma) in enumerate(ops):
            if eng == "*":
                done_tok.append(None)
                for e in engs:
                    wl = []
                    for sk2, val in latest.items():
                        if waited[e].get(sk2, 0) >= val:
                            continue
                        waited[e][sk2] = val
                        wl.append((sk2, val))
                    plan.append((e, None, wl, None))
                last_w.clear()
                readers.clear()
                continue
            deps = set()
            for k in r:
                if k in last_w:
                    deps.add(last_w[k])
                if is_psum_key(k):
                    for rd in readers.get(k, ()):
                        if ops[rd][0] != eng:
                            deps.add(rd)
            for k in w:
                if k in last_w:
                    deps.add(last_w[k])
                for rd in readers.get(k, ()):
                    deps.add(rd)
            waits = {}
            for d in deps:
                if d == idx:
                    continue
                deng, dis_dma = ops[d][0], ops[d][4]
                if (not dis_dma) and deng == eng and eng == "pe" and not is_dma:
                    continue
                sk, val = done_tok[d]
                waits[sk] = max(waits.get(sk, 0), val)
            if is_dma:
                j = dcount[eng] % NDMA
                dcount[eng] += 1
                sk = ("d", eng, j)
                prev = dsem_uses.get(sk, 0)
                if prev > 0:
                    waits[sk] = max(waits.get(sk, 0), 16 * prev)
                dsem_uses[sk] = prev + 1
                tok = (sk, 16 * (prev + 1))
                inc = (sk, 16)
            else:
                s = seq[eng]
                seq[eng] += 1
                sk = ("c", eng, s // EPOCH)
                tok = (sk, s % EPOCH + 1)
                inc = (sk, 1)
            done_tok.append(tok)
            latest[tok[0]] = max(latest.get(tok[0], 0), tok[1])
            wl = []
            for sk2, val in waits.items():
                if waited[eng].get(sk2, 0) >= val:
                    continue
                waited[eng][sk2] = val
                wl.append((sk2, val))
            plan.append((eng, fn, wl, inc))
            for k in w:
                last_w[k] = idx
                readers[k] = []
            for k in r:
                if k not in w:
                    readers.setdefault(k, []).append(idx)
        sems = {}
        for i, sk in enumerate(sorted(latest.keys(), key=str)):
            sems[sk] = self.es.enter_context(nc.semaphore(f"s{i}"))
        self.nsems = len(sems)
        self.counts = dict(seq)
        self.dcounts = dict(dcount)
        per_eng = {e: [p for p in plan if p[0] == e] for e in engs}
        block = self.es.enter_context(nc.Block())

        def emit(e, name):
            for (_, fn, wl, inc) in per_eng[name]:
                for sk, val in wl:
                    e.wait_ge(sems[sk], val)
                if fn is None:
                    continue
                ins = fn(e)
                ins.then_inc(sems[inc[0]], inc[1])
            for sk, val in latest.items():
                e.wait_ge(sems[sk], val)

        @block.tensor
        def _(e):
            emit(e, "pe")

        @block.scalar
        def _(e):
            emit(e, "act")

        @block.vector
        def _(e):
            emit(e, "dve")

        @block.gpsimd
        def _(e):
            emit(e, "pool")

        @block.sync
        def _(e):
            emit(e, "sp")


class Arena:
    def __init__(self, t, ncols):
        self.t = t
        self.n = ncols
        self.off = 0

    def reset(self, to=0):
        self.off = to

    def f32(self, cols):
        a = self.t[:, self.off:self.off + cols]
        self.off += cols
        assert self.off <= self.n, f"arena overflow {self.off}"
        return a

    def bf16(self, cols):
        c32 = (cols + 1) // 2
        a = self.t[:, self.off:self.off + c32].bitcast(BF16)
        self.off += c32
        assert self.off <= self.n, f"arena overflow {self.off}"
        return a


C_ID = 0
C_ONES = 128
C_EPS = 256
NCONST = 260


def make_consts():
    c = np.zeros((128, NCONST), np.float32)
    c[:, C_ID:C_ID + 128] = np.eye(128, dtype=np.float32)
    c[:, C_ONES:C_ONES + 128] = 1.0
    c[:, C_EPS] = EPS
    return c


V_MODB = 0
V_N1 = 192
V_N2 = 224
NVEC = 256


def make_vecs(inp, layer):
    v = np.zeros((128, NVEC), np.float32)
    v[:, V_MODB:V_MODB + 192] = inp["mod_b"][layer].reshape(192, 128).T
    v[:, V_N1:V_N1 + 32] = inp["norm1_w"][layer].reshape(32, 128).T
    v[:, V_N2:V_N2 + 32] = inp["norm2_w"][layer].reshape(32, 128).T
    return v


class K:
    def allgather(self, name, shard_d, rows, cols, dt=F32, nsplit=1):
        p = self.p
        rs = rows // 8
        bounce = self.nc.dram_tensor(name + "_bn", [rs, cols], dt, kind="Internal").ap()
        full = self.nc.dram_tensor(name + "_ag", [rows, cols], dt, kind="Internal").ap()
        p.dma("pool", bounce, shard_d, w=[name + "_bn"])
        p.dmaop("pool", lambda e: e.collective_compute("AllGather", op=ALU.bypass, replica_groups=[list(range(8))],
                                                       ins=[bounce], outs=[full]), r=[name + "_bn"], w=[name + "_ag"])
        return full

    def __init__(self, dbg=None):
        self.dbg = dbg or {}
        self.nc = bass.Bass("TRN2", target_bir_lowering=False, num_devices=8)
        self.es = ExitStack()
        self.p = Prog(self.nc, self.es)
        self.outs = {}

    def ext_in(self, name, shape, dt=F32):
        return self.nc.dram_tensor(name, list(shape), dt, kind="ExternalInput").ap()

    def ext_out(self, name, shape, dt=F32):
        return self.nc.dram_tensor(name, list(shape), dt, kind="ExternalOutput").ap()

    def scratch(self, name, shape, dt=F32):
        kind = "ExternalOutput" if name in self.dbg else "Internal"
        if name in getattr(self, "dbg_in", ()):
            kind = "ExternalInput"
        return self.nc.dram_tensor(name, list(shape), dt, kind=kind).ap()

    def setup(self):
        p = self.p
        self.arena_t = p.sb([128, 44 * 1024], F32, "arena")
        self.A = Arena(self.arena_t, 44 * 1024)
        self.pers = p.sb([128, 2048], F32, "pers")
        self.PA = Arena(self.pers, 2048)
        self.PA_big = p.sb([128, 2048], F32, "persbig")[:, :]
        self.gate_sb = p.sb([128, NT * 32], F32, "gate_sb")[:, :]
        self.PA_v1 = self.PA_big
        self.psb = [p.ps([128, 512], F32, f"psb{i}")[:, :] for i in range(8)]
        self.consts_d = self.ext_in("consts", [128, NCONST])
        self.consts = self.PA.f32(NCONST)
        p.dma("sp", self.consts, self.consts_d, w=["consts"])
        self.ident = self.consts[:, C_ID:C_ID + 128]
        self.ones = self.consts[:, C_ONES:C_ONES + 128]
        self.epsc = self.consts[:, C_EPS:C_EPS + 1]

    def stage_mod(self, layer, cvec_d, modw_d, vecs_d):
        p, A = self.p, self.A
        A.reset()
        L = layer
        vecs = self.PA.f32(NVEC)
        self.vecs = vecs
        p.dma("sp", vecs, vecs_d, w=["vecs"])
        cv = A.f32(128)
        sT = A.f32(64)
        p.dma("sp", cv[0:64, :], cvec_d, w=["cv"])
        ps0 = self.psb[0]
        p.op("pe", lambda e: e.transpose(ps0[:, 0:64], cv[0:64, :], self.ident[0:64, 0:64]),
             r=["cv", "consts"], w=["ps0"])
        p.op("act", lambda e: e.activation(out=sT, in_=ps0[:, 0:64], func=AF.Silu), r=["ps0"], w=["sT"])
        sT3 = sT.rearrange("p (n k) -> p n k", n=2)
        psm = self.psb[1]
        NBUF = 2
        wbuf = [A.f32(32 * 512) for _ in range(NBUF)]
        for jb in range(48):
            wb = wbuf[jb % NBUF]
            wb3 = wb.rearrange("p (k c) -> p k c", k=32)
            src = modw_d[:, jb * 512:(jb + 1) * 512].rearrange("(k p) c -> p k c", p=128)
            for q in range(4):
                p.dma(["sp", "act", "pool", "sp"][q], wb3[:, q * 8:(q + 1) * 8, :], src[:, q * 8:(q + 1) * 8, :],
                      w=[("mw", jb % NBUF, q)])
            for jj in range(4):
                j = jb * 4 + jj
                for kc in range(32):
                    p.op("pe", lambda e, j=j, kc=kc, jj=jj, wb3=wb3: e.matmul(
                        psm[:, 2 * j:2 * j + 2], lhsT=wb3[:, kc, jj * 128:(jj + 1) * 128], rhs=sT3[:, :, kc],
                        start=(kc == 0), stop=(kc == 31)),
                        r=[("mw", jb % NBUF, kc // 8), "sT"], w=["psm"])
        modL = self.PA.f32(192)
        modC = self.PA.f32(192)
        psm3 = psm[:, 0:384].rearrange("p (j n) -> p j n", n=2)
        p.op("dve", lambda e: e.tensor_tensor(out=modL, in0=psm3[:, :, 0], in1=vecs[:, V_MODB:V_MODB + 192], op=ALU.add),
             r=["psm", "vecs"], w=["modL"])
        p.op("dve", lambda e: e.tensor_tensor(out=modC, in0=psm3[:, :, 1], in1=vecs[:, V_MODB:V_MODB + 192], op=ALU.add),
             r=["psm", "vecs"], w=["modC"])
        self.mod = {"L": modL, "C": modC}
        self.Asc = {}
        for side, m in (("L", modL), ("C", modC)):
            a1 = self.PA.f32(32)
            a2 = self.PA.f32(32)
            p.op("dve", lambda e, a1=a1, m=m: e.scalar_tensor_tensor(
                out=a1, in0=m[:, 32:64], scalar=1.0, in1=vecs[:, V_N1:V_N1 + 32], op0=ALU.add, op1=ALU.mult),
                r=["mod" + side, "vecs"], w=["A1" + side])
            p.op("dve", lambda e, a2=a2, m=m: e.scalar_tensor_tensor(
                out=a2, in0=m[:, 128:160], scalar=1.0, in1=vecs[:, V_N2:V_N2 + 32], op0=ALU.add, op1=ALU.mult),
                r=["mod" + side, "vecs"], w=["A2" + side])
            self.Asc[side] = (a1, a2)
        gbc_d = self.scratch(f"gbc{L}", [4, 128, D])
        self.gbc_d = gbc_d
        diag = [A.f32(128) for _ in range(2)]
        row = [A.f32(512) for _ in range(2)]
        cnt = 0
        for gi, (side, base) in enumerate((("L", 64), ("C", 64), ("L", 160), ("C", 160))):
            m = self.mod[side]
            for cb in range(8):
                pst = self.psb[2 + (cnt % 2)]
                rw = row[cnt % 2]
                for c4 in range(4):
                    kc = cb * 4 + c4
                    dg = diag[(cnt * 4 + c4) % 2]
                    dk = ("diag", (cnt * 4 + c4) % 2)
                    p.op("dve", lambda e, dg=dg, m=m, base=base, kc=kc: e.tensor_scalar(
                        out=dg, in0=self.ident, scalar1=m[:, base + kc:base + kc + 1], scalar2=None, op0=ALU.mult),
                        r=["mod" + side, "consts"], w=[dk])
                    p.op("pe", lambda e, dg=dg, pst=pst, c4=c4: e.matmul(
                        pst[:, c4 * 128:(c4 + 1) * 128], lhsT=self.ones, rhs=dg, start=True, stop=True),
                        r=[dk, "consts"], w=[("pst", cnt % 2)])
                p.op("act", lambda e, rw=rw, pst=pst: e.copy(out=rw, in_=pst), r=[("pst", cnt % 2)], w=[("row", cnt % 2)])
                p.dma("sp", gbc_d[gi, :, cb * 512:(cb + 1) * 512], rw, r=[("row", cnt % 2)], w=["gbc"])
                cnt += 1
        p.barrier()

    def stage_norm(self, x_d, which, out_bf_d=None, out_f32_d=None, tiles=range(NT)):
        p, A = self.p, self.A
        A.reset()
        NB = 2
        xt = [A.f32(D) for _ in range(NB)]
        junk = A.f32(D)
        hb = [A.bf16(D) for _ in range(NB)] if out_bf_d is not None else None
        hf = [A.f32(D) for _ in range(NB)] if out_f32_d is not None else None
        ss = [A.f32(1) for _ in range(NB)]
        rs = [A.f32(1) for _ in range(NB)]
        for it, t in enumerate(tiles):
            b = it % NB
            side = "C" if t < 2 else "L"
            Av = self.Asc[side][which]
            sh0 = 0 if which == 0 else 96
            Bv = self.mod[side]
            p.dma("sp" if it % 2 == 0 else "pool", xt[b], x_d[t * 128:(t + 1) * 128, :], w=[("xt", b)])
            p.op("act", lambda e, b=b: e.activation(out=junk, in_=xt[b], func=AF.Square, accum_out=ss[b]),
                 r=[("xt", b)], w=["junk", ("ss", b)])
            p.op("act", lambda e, b=b: e.activation(out=rs[b], in_=ss[b], func=AF.Sqrt, bias=self.epsc, scale=1.0 / D),
                 r=[("ss", b), "consts"], w=[("rs", b)])
            p.op("dve", lambda e, b=b: e.reciprocal(out=rs[b], in_=rs[b]), r=[("rs", b)], w=[("rs", b)])
            p.op("dve", lambda e, b=b: e.tensor_scalar(out=xt[b], in0=xt[b], scalar1=rs[b][:, 0:1], scalar2=None,
                                                      op0=ALU.mult), r=[("rs", b), ("xt", b)], w=[("xt", b)])
            for kc in range(KC):
                pi = kc % 4
                pst = self.psb[4 + pi]
                p.op("pe", lambda e, b=b, kc=kc, pst=pst: e.transpose(pst[:, 0:128], xt[b][:, kc * 128:(kc + 1) * 128],
                                                                     self.ident),
                     r=[("xt", b), "consts"], w=[("pT", pi)])
                dst = hf[b] if hf is not None else hb[b]
                wk = ("hf", b, kc) if hf is not None else ("hb", b, kc)
                p.op("act", lambda e, dst=dst, kc=kc, pst=pst, Av=Av, Bv=Bv, sh0=sh0: e.activation(
                    out=dst[:, kc * 128:(kc + 1) * 128], in_=pst[:, 0:128], func=AF.Identity,
                    scale=Av[:, kc:kc + 1], bias=Bv[:, sh0 + kc:sh0 + kc + 1]),
                    r=[("pT", pi), "mod" + side, "A1" + side, "A2" + side], w=[wk])
                if hf is not None and hb is not None:
                    p.op("pool", lambda e, b=b, kc=kc: e.tensor_copy(out=hb[b][:, kc * 128:(kc + 1) * 128],
                                                                    in_=hf[b][:, kc * 128:(kc + 1) * 128]),
                         r=[("hf", b, kc)], w=[("hb", b, kc)])
            if out_bf_d is not None:
                p.dma("act", out_bf_d[t], hb[b], r=[("hb", b, kc) for kc in range(KC)], w=["n_out_bf"])
            if out_f32_d is not None:
                p.dma("act", out_f32_d[t], hf[b], r=[("hf", b, kc) for kc in range(KC)], w=["n_out_f32"])
        p.barrier()


def build(layer_inputs=None, dbg=None):
    k = K(dbg)
    k.setup()
    return k


def _gemm_tm(self, at_d, kc_n, blocks, epilogue, tiles, arena_keep=None, at_bufs=3, wsets=2, at_c0=0):
    p, A = self.p, self.A
    if arena_keep is None:
        A.reset()
    else:
        A.reset(arena_keep)
    nsrc = max(len(b["srcs"]) for b in blocks)
    maxw = max(b["nbw"] for b in blocks)
    SG = 8
    stg = [A.f32(SG * maxw) for _ in range(3)]
    wb = [[A.bf16(kc_n * maxw) for _ in range(nsrc)] for _ in range(wsets)]
    at = [A.bf16(kc_n * 128) for _ in range(at_bufs)]
    self.ep_base = A.off
    sgc = 0
    atc = 0
    pgc = 0
    cast_engs = ["pool", "dve", "act"]
    for bi, blk in enumerate(blocks):
        nbw = blk["nbw"]
        ws = bi % wsets
        for si, src in enumerate(blk["srcs"]):
            src3 = src.rearrange("(k p) c -> p k c", p=128)
            wb3 = wb[ws][si][:, 0:kc_n * nbw].rearrange("p (k c) -> p k c", k=kc_n)
            for g in range(kc_n // SG):
                sb_ = sgc % 3
                st3 = stg[sb_][:, 0:SG * nbw].rearrange("p (k c) -> p k c", k=SG)
                p.dma("sp", st3, src3[:, g * SG:(g + 1) * SG, :], w=[("stg", sb_)])
                ce = cast_engs[sgc % 3]
                if ce == "act":
                    p.op("act", lambda e, st3=st3, wb3=wb3, g=g: e.copy(out=wb3[:, g * SG:(g + 1) * SG, :], in_=st3),
                         r=[("stg", sb_)], w=[("wb", ws, si, g)])
                else:
                    p.op(ce, lambda e, st3=st3, wb3=wb3, g=g: e.tensor_copy(out=wb3[:, g * SG:(g + 1) * SG, :], in_=st3),
                         r=[("stg", sb_)], w=[("wb", ws, si, g)])
                sgc += 1
        for t in tiles:
            ab = atc % at_bufs
            atc += 1
            p.dma("sp" if atc % 2 else "pool", at[ab], at_d[t][:, at_c0 * 128:(at_c0 + kc_n) * 128], w=[("at", ab)])
            at3 = at[ab].rearrange("p (k c) -> p k c", k=kc_n)
            pset = pgc % 2
            pgc += 1
            ps_list, pkeys = [], []
            for si in range(len(blk["srcs"])):
                ps = self.psb[pset * 2 + si]
                wb3 = wb[ws][si][:, 0:kc_n * nbw].rearrange("p (k c) -> p k c", k=kc_n)
                pk = ("pg", pset, si)
                for kc in range(kc_n):
                    p.op("pe", lambda e, ps=ps, at3=at3, wb3=wb3, kc=kc, nbw=nbw: e.matmul(
                        ps[:, 0:nbw], lhsT=at3[:, kc, :], rhs=wb3[:, kc, :], start=(kc == 0), stop=(kc == kc_n - 1)),
                        r=[("at", ab), ("wb", ws, si, kc // SG)], w=[pk])
                ps_list.append(ps)
                pkeys.append(pk)
            epilogue(t, bi, blk, ps_list, pkeys)
    p.barrier()


K.gemm_tm = _gemm_tm


GH = 24
V0_CW = 0
V0_ALOG = 360
V0_DTB = 408
V0_GNW = 456
V0_PSC = 584
NV0 = 1608


def make_vecs0(inp):
    v = np.zeros((128, NV0), np.float32)
    cw = inp["gdn_conv_w"][0]
    v[:, V0_CW:V0_CW + 360] = cw.reshape(5, 72, 128).transpose(2, 1, 0).reshape(128, 360)
    v[:, V0_ALOG:V0_ALOG + 48] = inp["gdn_a_log"][0].reshape(1, 48)
    v[:, V0_DTB:V0_DTB + 48] = inp["gdn_dt_bias"][0].reshape(1, 48)
    v[:, V0_GNW:V0_GNW + 128] = inp["gdn_norm_w"][0].reshape(1, 128)
    v[:, V0_PSC:V0_PSC + 1024] = inp["pool_scale"][0].reshape(1, 1024)
    return v


def _stage_inproj0(self, hT_d, w_d, v0_d):
    p, A = self.p, self.A
    self.v0 = self.PA_big
    p.dma("sp", self.v0[:, 0:NV0], v0_d, w=["v0"])
    self.poolu_d = self.scratch("poolu", [NTOK, 1024], F32)
    self.raw_d = self.scratch("rawqkv", [72, 128, NTOK], F32)
    self.z_d = self.scratch("zs", [NTOK, 3072], F32)
    self.beta_d = self.scratch("beta", [NTOK, 48], F32)
    self.g_d = self.scratch("gg", [NTOK, 48], F32)
    v0 = self.v0

    def mk_ep_simple(kind):
        def ep(t, bi, blk, ps_list, pkeys):
            A2 = Arena(self.arena_t, 44 * 1024)
            A2.reset(self.ep_base)
            bufs = [A2.f32(512) for _ in range(2)]
            i = ep.cnt % 2
            ep.cnt += 1
            ob = bufs[i]
            if kind == "pool":
                p.op("act", lambda e: e.copy(out=ob, in_=ps_list[0]), r=[pkeys[0]], w=[("epo", i)])
                p.dma("act", self.poolu_d[t * 128:(t + 1) * 128, bi * 512:(bi + 1) * 512], ob, r=[("epo", i)], w=["poolu"])
            else:
                p.op("act", lambda e: e.activation(out=ob, in_=ps_list[0], func=AF.Silu), r=[pkeys[0]], w=[("epo", i)])
                p.dma("act", self.z_d[t * 128:(t + 1) * 128, bi * 512:(bi + 1) * 512], ob, r=[("epo", i)], w=["zs"])
        ep.cnt = 0
        return ep

    blocks = [dict(srcs=[w_d[:, c:c + 512]], nbw=512) for c in range(0, 1024, 512)]
    self.gemm_tm(hT_d, KC, blocks, mk_ep_simple("pool"), range(NT))
    blocks = [dict(srcs=[w_d[:, 10240 + c:10240 + c + 512]], nbw=512) for c in range(0, 3072, 512)]
    self.gemm_tm(hT_d, KC, blocks, mk_ep_simple("z"), range(NT))

    def ep_gate(t, bi, blk, ps_list, pkeys):
        A2 = Arena(self.arena_t, 44 * 1024)
        A2.reset(self.ep_base)
        bb = [A2.f32(48) for _ in range(2)]
        gb = [A2.f32(48) for _ in range(2)]
        ea = A2.f32(48)
        i = ep_gate.cnt % 2
        if ep_gate.cnt == 0:
            p.op("act", lambda e: e.activation(out=ea, in_=v0[:, V0_ALOG:V0_ALOG + 48], func=AF.Exp), r=["v0"], w=["ea"])
        ep_gate.cnt += 1
        ps = ps_list[0]
        p.op("act", lambda e: e.activation(out=bb[i], in_=ps[:, 0:48], func=AF.Sigmoid), r=[pkeys[0]], w=[("bb", i)])
        p.op("dve", lambda e: e.tensor_tensor(out=gb[i], in0=ps[:, 48:96], in1=v0[:, V0_DTB:V0_DTB + 48], op=ALU.add),
             r=[pkeys[0], "v0"], w=[("gb", i)])
        p.op("act", lambda e: e.activation(out=gb[i], in_=gb[i], func=AF.Exp), r=[("gb", i)], w=[("gb", i)])
        p.op("act", lambda e: e.activation(out=gb[i], in_=gb[i], func=AF.Ln, bias=self.ones[:, 0:1]), r=[("gb", i), "consts"],
             w=[("gb", i)])
        p.op("dve", lambda e: e.scalar_tensor_tensor(out=gb[i], in0=gb[i], scalar=-1.0, in1=ea, op0=ALU.mult, op1=ALU.mult),
             r=[("gb", i), "ea"], w=[("gb", i)])
        p.dma("act", self.beta_d[t * 128:(t + 1) * 128, :], bb[i], r=[("bb", i)], w=["betad"])
        p.dma("act", self.g_d[t * 128:(t + 1) * 128, :], gb[i], r=[("gb", i)], w=["gd"])
    ep_gate.cnt = 0
    self.gemm_tm(hT_d, KC, [dict(srcs=[w_d[:, 13312:13408]], nbw=96)], ep_gate, range(NT))

    def ep_qkv(t, bi, blk, ps_list, pkeys):
        A2 = Arena(self.arena_t, 44 * 1024)
        A2.reset(self.ep_base)
        tm = [A2.f32(512) for _ in range(2)]
        fm = [A2.f32(512) for _ in range(2)]
        i = ep_qkv.cnt % 2
        ep_qkv.cnt += 1
        p.op("act", lambda e: e.copy(out=tm[i], in_=ps_list[0]), r=[pkeys[0]], w=[("tm", i)])
        pst = self.psb[4 + i]
        for j in range(4):
            p.op("pe", lambda e, j=j: e.transpose(pst[:, j * 128:(j + 1) * 128], tm[i][:, j * 128:(j + 1) * 128], self.ident),
                 r=[("tm", i), "consts"], w=[("pst4", i)])
        p.op("dve", lambda e: e.tensor_copy(out=fm[i], in_=pst), r=[("pst4", i)], w=[("fm", i)])
        p.dma("act", self.raw_d[bi * 4:(bi + 1) * 4, :, t * 128:(t + 1) * 128].rearrange("j p c -> p j c"),
              fm[i].rearrange("p (j c) -> p j c", j=4), r=[("fm", i)], w=["rawd"])
    ep_qkv.cnt = 0
    blocks = [dict(srcs=[w_d[:, 1024 + c:1024 + c + 512]], nbw=512) for c in range(0, 9216, 512)]
    self.gemm_tm(hT_d, KC, blocks, ep_qkv, range(NT))


K.stage_inproj0 = _stage_inproj0


def _stage_conv0(self):
    p, A = self.p, self.A
    A.reset()
    v0 = self.v0
    self.qT_d = self.scratch("qT", [GH, 128, NTOK], F32)
    self.kT_d = self.scratch("kT", [GH, 128, NTOK], F32)
    self.ktm_d = self.scratch("ktm", [NT, GH, 128, 128], F32)
    self.vtm_d = self.scratch("vtm", [NT, GH, 128, 128], F32)
    W = NTOK + 8
    buf = [A.f32(W) for _ in range(2)]
    acc = [A.f32(W) for _ in range(2)]
    sq = A.f32(512)
    rst = A.f32(512)
    tmo = [A.f32(512) for _ in range(2)]
    for b in range(2):
        p.op("pool", lambda e, b=b: e.memset(buf[b], 0.0), w=[("cb", b)])
    NV = NTOK + 4

    def seg(ap, ofs=0):
        return ap

    tcnt = 0
    for ci in range(72):
        b = ci % 2
        kind = ci // 24
        h = ci % 24
        p.dma("sp", buf[b][:, 2:2 + NCTX], self.raw_d[ci][:, 0:NCTX], w=[("cb", b)])
        p.dma("pool", buf[b][:, 6 + NCTX:6 + NTOK], self.raw_d[ci][:, NCTX:NTOK], w=[("cb", b)])
        ve = "dve"
        cw = v0[:, V0_CW + ci * 5:V0_CW + ci * 5 + 5]
        a = acc[b]
        p.op(ve, lambda e, a=a, b=b, cw=cw: e.tensor_scalar(out=a[:, 2:2 + NV], in0=buf[b][:, 0:NV], scalar1=cw[:, 0:1],
                                                           scalar2=None, op0=ALU.mult), r=[("cb", b), "v0"], w=[("acc", b)])
        for j in range(1, 5):
            p.op(ve, lambda e, a=a, b=b, cw=cw, j=j: e.scalar_tensor_tensor(
                out=a[:, 2:2 + NV], in0=buf[b][:, j:j + NV], scalar=cw[:, j:j + 1], in1=a[:, 2:2 + NV],
                op0=ALU.mult, op1=ALU.add), r=[("cb", b), "v0", ("acc", b)], w=[("acc", b)])
        p.op("act", lambda e, a=a: e.activation(out=a[:, 2:2 + NV], in_=a[:, 2:2 + NV], func=AF.Silu),
             r=[("acc", b)], w=[("acc", b)])
        if kind < 2:
            for c0 in range(2, 2 + NV, 512):
                cn = min(512, 2 + NV - c0)
                ps = self.psb[(c0 // 512) % 2]
                pk = ("pl2", (c0 // 512) % 2)
                p.op("act", lambda e, a=a, c0=c0, cn=cn: e.activation(out=sq[:, 0:cn], in_=a[:, c0:c0 + cn], func=AF.Square),
                     r=[("acc", b)], w=["sq"])
                p.op("pe", lambda e, ps=ps, cn=cn: e.matmul(ps[:, 0:cn], lhsT=self.ones, rhs=sq[:, 0:cn], start=True, stop=True),
                     r=["sq", "consts"], w=[pk])
                p.op("act", lambda e, ps=ps, cn=cn: e.activation(out=rst[:, 0:cn], in_=ps[:, 0:cn], func=AF.Sqrt, bias=self.epsc),
                     r=[pk, "consts"], w=["rst"])
                p.op("dve", lambda e, cn=cn: e.reciprocal(out=rst[:, 0:cn], in_=rst[:, 0:cn]), r=["rst"], w=["rst"])
                sc = (128.0 ** -0.5) if kind == 0 else 1.0
                p.op("dve", lambda e, a=a, c0=c0, cn=cn, sc=sc: e.scalar_tensor_tensor(
                    out=a[:, c0:c0 + cn], in0=a[:, c0:c0 + cn], scalar=sc, in1=rst[:, 0:cn], op0=ALU.mult, op1=ALU.mult),
                    r=["rst", ("acc", b)], w=[("acc", b)])
            dst = self.qT_d if kind == 0 else self.kT_d
            p.dma("act", dst[h][:, 0:NCTX], a[:, 2:2 + NCTX], r=[("acc", b)], w=["qkT"])
            p.dma("act", dst[h][:, NCTX:NTOK], a[:, 6 + NCTX:6 + NTOK], r=[("acc", b)], w=["qkT"])
        if kind >= 1:
            dst = self.ktm_d if kind == 1 else self.vtm_d
            for t in range(NT):
                o0 = (2 + t * 128) if t < 2 else (6 + t * 128)
                i = tcnt % 2
                tcnt += 1
                ps = self.psb[4 + i]
                p.op("pe", lambda e, a=a, o0=o0, ps=ps: e.transpose(ps[:, 0:128], a[:, o0:o0 + 128], self.ident),
                     r=[("acc", b), "consts"], w=[("ptm", i)])
                p.op("act" if t % 2 else "dve", (lambda e, ps=ps, i=i: e.copy(out=tmo[i][:, 0:128], in_=ps[:, 0:128])) if t % 2 else
                     (lambda e, ps=ps, i=i: e.tensor_copy(out=tmo[i][:, 0:128], in_=ps[:, 0:128])),
                     r=[("ptm", i)], w=[("tmo", i)])
                p.dma("sp", dst[t, h], tmo[i][:, 0:128], r=[("tmo", i)], w=["kvtm"])
    p.barrier()


K.stage_conv0 = _stage_conv0


C2_CUMF = 0
C2_CUMB = 128
C2_SEL = 256
C2_MFI = 1280
C2_MFS = 2304
C2_MBI = 3328
C2_MBS = 4352
NC2 = 5376


def make_consts2():
    c = np.zeros((128, NC2), np.float32)
    i = np.arange(128)
    c[:, C2_CUMF:C2_CUMF + 128] = (i[:, None] <= i[None, :])
    c[:, C2_CUMB:C2_CUMB + 128] = (i[:, None] >= i[None, :])
    for h in range(8):
        c[h, C2_SEL + h * 128:C2_SEL + (h + 1) * 128] = 1.0
    cc, ss = i[:, None], i[None, :]
    for off, m in ((C2_MFI, ss <= cc), (C2_MFS, ss < cc), (C2_MBI, ss >= cc), (C2_MBS, ss > cc)):
        c[:, off:off + 1024] = np.tile(m.astype(np.float32), (1, 8))
    return c


def _stage_gdn(self, c2_d, nsteps=NT):
    p, A = self.p, self.A
    A.reset()
    HG = 8
    self.o_d = self.scratch("ogdn", [2, NTOK, GH * 128], F32)
    c2 = A.f32(NC2)
    p.dma("sp", c2, c2_d, w=["c2"])
    S = A.f32(2 * GH * 128)
    p.op("pool", lambda e: e.memset(S, 0.0), w=[("S", d, h) for d in range(2) for h in range(GH)])
    NBUF = 2
    qTb = [A.f32(HG * 128) for _ in range(NBUF)]
    kTb = [A.f32(HG * 128) for _ in range(NBUF)]
    kb = [A.f32(HG * 128) for _ in range(NBUF)]
    vb = [A.f32(HG * 128) for _ in range(NBUF)]
    gt = [A.f32(24) for _ in range(2)]
    bt = [A.f32(24) for _ in range(2)]
    Gs = [A.f32(24) for _ in range(2)]
    eG = [A.f32(24) for _ in range(2)]
    eEnd = [A.f32(24) for _ in range(2)]
    ge = [A.f32(24) for _ in range(2)]
    bw = [A.f32(24) for _ in range(2)]
    negb = [A.f32(24) for _ in range(2)]
    GT8 = [A.f32(128) for _ in range(2)]
    xm = [A.f32(HG * 128) for _ in range(2)]
    eGbc = [A.f32(HG * 128) for _ in range(2)]
    Di = [A.f32(HG * 128) for _ in range(2)]
    Ds = [A.f32(HG * 128) for _ in range(2)]
    osb = [A.f32(HG * 128) for _ in range(2)]
    NH = 3
    Pb = [[A.f32(128) for _ in range(2)] for _ in range(NH)]
    Qb = [[A.f32(128) for _ in range(2)] for _ in range(NH)]
    Rb = [[A.f32(128) for _ in range(2)] for _ in range(NH)]
    attn = [A.f32(128) for _ in range(NH)]
    attnT = [A.f32(128) for _ in range(NH)]
    Vb = [A.f32(128) for _ in range(NH)]
    Kbg = [A.f32(128) for _ in range(NH)]
    kend = [A.f32(128) for _ in range(NH)]
    usb = [A.f32(128) for _ in range(NH)]
    wT = [A.f32(128) for _ in range(NH)]
    qdT = [A.f32(128) for _ in range(NH)]
    vn = [A.f32(128) for _ in range(NH)]

    slot_ctr = [0]
    BANKS = [2, 3, 4, 5, 7]

    def nbank():
        i = BANKS[slot_ctr[0] % len(BANKS)]
        slot_ctr[0] += 1
        return self.psb[i], ("bank", i)

    cp_ctr = [0]

    def evac(dst, src, r, w):
        i = cp_ctr[0] % 2
        cp_ctr[0] += 1
        if i == 0:
            p.op("act", lambda e: e.copy(out=dst, in_=src), r=r, w=w)
        else:
            p.op("dve", lambda e: e.tensor_copy(out=dst, in_=src), r=r, w=w)

    order = {0: list(range(NT)), 1: [1, 0] + list(range(NT - 1, 1, -1))}
    uc = 0
    gc = 0
    hc = 0
    for step in range(nsteps):
        for d in range(2):
            t = order[d][step]
            ub = uc % 2
            uc += 1
            cum = c2[:, C2_CUMF:C2_CUMF + 128] if d == 0 else c2[:, C2_CUMB:C2_CUMB + 128]
            mI = c2[:, C2_MFI:C2_MFI + 1024] if d == 0 else c2[:, C2_MBI:C2_MBI + 1024]
            mS = c2[:, C2_MFS:C2_MFS + 1024] if d == 0 else c2[:, C2_MBS:C2_MBS + 1024]
            p.dma("sp", gt[ub], self.g_d[t * 128:(t + 1) * 128, d * 24:(d + 1) * 24], w=[("gt", ub)])
            p.dma("sp", bt[ub], self.beta_d[t * 128:(t + 1) * 128, d * 24:(d + 1) * 24], w=[("bt", ub)])
            ps6 = self.psb[6]
            p.op("pe", lambda e, cum=cum, ub=ub: e.matmul(ps6[:, 0:24], lhsT=cum, rhs=gt[ub], start=True, stop=True),
                 r=["c2", ("gt", ub)], w=["bank6"])
            p.op("pe", lambda e, ub=ub: e.matmul(ps6[:, 32:56], lhsT=self.ones, rhs=gt[ub], start=True, stop=True),
                 r=["consts", ("gt", ub)], w=["bank6"])
            p.op("dve", lambda e, ub=ub: e.tensor_copy(out=Gs[ub], in_=ps6[:, 0:24]), r=["bank6"], w=[("Gs", ub)])
            p.op("act", lambda e, ub=ub: e.activation(out=eG[ub], in_=ps6[:, 0:24], func=AF.Exp), r=["bank6"], w=[("eG", ub)])
            p.op("act", lambda e, ub=ub: e.activation(out=ge[ub], in_=ps6[:, 32:56], func=AF.Exp), r=["bank6"], w=[("ge", ub)])
            p.op("dve", lambda e, ub=ub: e.tensor_tensor(out=eEnd[ub], in0=ps6[:, 32:56], in1=Gs[ub], op=ALU.subtract),
                 r=["bank6", ("Gs", ub)], w=[("eEnd", ub)])
            p.op("act", lambda e, ub=ub: e.activation(out=eEnd[ub], in_=eEnd[ub], func=AF.Exp), r=[("eEnd", ub)], w=[("eEnd", ub)])
            p.op("pool", lambda e, ub=ub: e.tensor_tensor(out=bw[ub], in0=bt[ub], in1=eG[ub], op=ALU.mult),
                 r=[("bt", ub), ("eG", ub)], w=[("bw", ub)])
            p.op("pool", lambda e, ub=ub: e.tensor_scalar(out=negb[ub], in0=bt[ub], scalar1=-1.0, scalar2=None, op0=ALU.mult),
                 r=[("bt", ub)], w=[("negb", ub)])
            for grp in range(GH // HG):
                h0 = grp * HG
                gb = gc % 2
                gc += 1
                tsl = slice(t * 128, (t + 1) * 128)
                p.dma("sp", qTb[gb].rearrange("p (h c) -> p h c", h=HG),
                      self.qT_d[h0:h0 + HG, :, tsl].rearrange("h p c -> p h c"), w=[("qTb", gb)])
                p.dma("pool", kTb[gb].rearrange("p (h c) -> p h c", h=HG),
                      self.kT_d[h0:h0 + HG, :, tsl].rearrange("h p c -> p h c"), w=[("kTb", gb)])
                p.dma("sp", kb[gb].rearrange("p (h c) -> p h c", h=HG),
                      self.ktm_d[t, h0:h0 + HG].rearrange("h p c -> p h c"), w=[("kb", gb)])
                p.dma("pool", vb[gb].rearrange("p (h c) -> p h c", h=HG),
                      self.vtm_d[t, h0:h0 + HG].rearrange("h p c -> p h c"), w=[("vb", gb)])
                p.op("pe", lambda e, ub=ub, h0=h0: e.transpose(ps6[0:HG, 64:192], Gs[ub][:, h0:h0 + HG], self.ident),
                     r=[("Gs", ub), "consts"], w=["bank6"])
                p.op("act", lambda e, gb=gb: e.copy(out=GT8[gb][0:HG, :], in_=ps6[0:HG, 64:192]), r=["bank6"], w=[("GT8", gb)])
                for j in range(HG):
                    bank = self.psb[j // 4]
                    p.op("pe", lambda e, j=j, bank=bank, gb=gb: e.matmul(
                        bank[:, (j % 4) * 128:(j % 4 + 1) * 128], lhsT=c2[0:HG, C2_SEL + j * 128:C2_SEL + (j + 1) * 128],
                        rhs=GT8[gb][0:HG, :], start=True, stop=True), r=["c2", ("GT8", gb)], w=[("gbcb", j // 4)])
                for j in range(HG):
                    bank = self.psb[j // 4]
                    h = h0 + j
                    p.op("dve", lambda e, j=j, bank=bank, gb=gb, ub=ub, h=h: e.tensor_scalar(
                        out=xm[gb][:, j * 128:(j + 1) * 128], in0=bank[:, (j % 4) * 128:(j % 4 + 1) * 128],
                        scalar1=Gs[ub][:, h:h + 1], scalar2=0.0, op0=ALU.subtract, op1=ALU.max),
                        r=[("gbcb", j // 4), ("Gs", ub)], w=[("xm", gb)])
                import os
                _x = os.environ.get("GDNX", "")
                for bk in range(2):
                    p.op("act", lambda e, bk=bk, gb=gb: e.activation(out=eGbc[gb][:, bk * 512:(bk + 1) * 512], in_=self.psb[bk],
                                                                     func=(AF.Identity if _x == "copy" else AF.Exp)),
                         r=[("gbcb", bk)] + ([("xm", gb)] if _x == "dep" else []), w=[("eGbc", gb, bk)])
                p.op("act", lambda e, gb=gb: e.activation(out=xm[gb], in_=xm[gb], func=AF.Exp, scale=-1.0),
                     r=[("xm", gb)], w=[("xm", gb)])
                p.op("pool", lambda e, gb=gb, mI=mI: e.tensor_tensor(out=Di[gb], in0=xm[gb], in1=mI, op=ALU.mult),
                     r=[("xm", gb), "c2"], w=[("Di", gb)])
                p.op("pool", lambda e, gb=gb, mS=mS: e.tensor_tensor(out=Ds[gb], in0=xm[gb], in1=mS, op=ALU.mult),
                     r=[("xm", gb), "c2"], w=[("Ds", gb)])
                for j in range(HG):
                    h = h0 + j
                    hb = hc % NH
                    hc += 1
                    js = slice(j * 128, (j + 1) * 128)
                    kT_h, qT_h, k_h, v_h = kTb[gb][:, js], qTb[gb][:, js], kb[gb][:, js], vb[gb][:, js]
                    bk1, k1 = nbank()
                    psA, psQ = bk1[:, 0:128], bk1[:, 128:256]
                    p.op("pe", lambda e, psA=psA, kT_h=kT_h: e.matmul(psA, lhsT=kT_h, rhs=kT_h, start=True, stop=True),
                         r=[("kTb", gb)], w=[k1])
                    p.op("pe", lambda e, psQ=psQ, kT_h=kT_h, qT_h=qT_h: e.matmul(psQ, lhsT=qT_h, rhs=kT_h, start=True, stop=True),
                         r=[("kTb", gb), ("qTb", gb)], w=[k1])
                    P0, Q0 = Pb[hb][0], Qb[hb][0]
                    p.op("dve", lambda e, P0=P0, psA=psA, ub=ub, h=h, gb=gb, js=js: e.scalar_tensor_tensor(
                        out=P0, in0=psA, scalar=negb[ub][:, h:h + 1], in1=Ds[gb][:, js], op0=ALU.mult, op1=ALU.mult),
                        r=[k1, ("negb", ub), ("Ds", gb)], w=[("P", hb, 0)])
                    p.op("dve", lambda e, hb=hb, psQ=psQ, gb=gb, js=js: e.tensor_tensor(
                        out=attn[hb], in0=psQ, in1=Di[gb][:, js], op=ALU.mult), r=[k1, ("Di", gb)], w=[("attn", hb)])
                    bk2, k2 = nbank()
                    psT, psT2 = bk2[:, 0:128], bk2[:, 128:256]
                    p.op("pe", lambda e, psT=psT, P0=P0: e.transpose(psT, P0, self.ident), r=[("P", hb, 0), "consts"], w=[k2])
                    p.op("pe", lambda e, psT2=psT2, hb=hb: e.transpose(psT2, attn[hb], self.ident),
                         r=[("attn", hb), "consts"], w=[k2])
                    evac(Q0, psT, [k2], [("Q", hb, 0)])
                    evac(attnT[hb], psT2, [k2], [("attnT", hb)])
                    R0 = Rb[hb][0]
                    p.op("pool", lambda e, R0=R0, Q0=Q0: e.tensor_tensor(out=R0, in0=Q0, in1=self.ident, op=ALU.add),
                         r=[("Q", hb, 0), "consts"], w=[("R", hb, 0)])
                    p.op("pool", lambda e, hb=hb, v_h=v_h, ub=ub, h=h: e.tensor_scalar(
                        out=Vb[hb], in0=v_h, scalar1=bt[ub][:, h:h + 1], scalar2=None, op0=ALU.mult),
                        r=[("vb", gb), ("bt", ub)], w=[("Vb", hb)])
                    p.op("pool", lambda e, hb=hb, k_h=k_h, ub=ub, h=h: e.tensor_scalar(
                        out=Kbg[hb], in0=k_h, scalar1=bw[ub][:, h:h + 1], scalar2=None, op0=ALU.mult),
                        r=[("kb", gb), ("bw", ub)], w=[("Kbg", hb)])
                    p.op("pool", lambda e, hb=hb, k_h=k_h, ub=ub, h=h: e.tensor_scalar(
                        out=kend[hb], in0=k_h, scalar1=eEnd[ub][:, h:h + 1], scalar2=None, op0=ALU.mult),
                        r=[("kb", gb), ("eEnd", ub)], w=[("kend", hb)])
                    p.op("pool", lambda e, hb=hb, qT_h=qT_h, gb=gb, js=js: e.tensor_tensor(
                        out=qdT[hb], in0=qT_h, in1=eGbc[gb][:, js], op=ALU.mult),
                        r=[("qTb", gb), ("eGbc", gb, j // 4)], w=[("qdT", hb)])
                    cur = 0
                    for lvl in range(6):
                        nxt = 1 - cur
                        Pc, Qc, Pn, Qn = Pb[hb][cur], Qb[hb][cur], Pb[hb][nxt], Qb[hb][nxt]
                        bk3, k3 = nbank()
                        psP, psQ2 = bk3[:, 0:128], bk3[:, 128:256]
                        p.op("pe", lambda e, psP=psP, Pc=Pc, Qc=Qc: e.matmul(psP, lhsT=Qc, rhs=Pc, start=True, stop=True),
                             r=[("P", hb, cur), ("Q", hb, cur)], w=[k3])
                        if lvl < 5:
                            p.op("pe", lambda e, psQ2=psQ2, Pc=Pc, Qc=Qc: e.matmul(psQ2, lhsT=Pc, rhs=Qc, start=True, stop=True),
                                 r=[("P", hb, cur), ("Q", hb, cur)], w=[k3])
                        evac(Pn, psP, [k3], [("P", hb, nxt)])
                        if lvl < 5:
                            evac(Qn, psQ2, [k3], [("Q", hb, nxt)])
                        Rc, Rn = Rb[hb][cur], Rb[hb][nxt]
                        bk4, k4 = nbank()
                        psR = bk4[:, 0:128]
                        p.op("pe", lambda e, psR=psR, Pn=Pn, Rc=Rc: e.matmul(psR, lhsT=Pn, rhs=Rc, start=True, stop=True),
                             r=[("P", hb, nxt), ("R", hb, cur)], w=[k4])
                        p.op("dve", lambda e, psR=psR, Rc=Rc, Rn=Rn: e.tensor_tensor(out=Rn, in0=psR, in1=Rc, op=ALU.add),
                             r=[k4, ("R", hb, cur)], w=[("R", hb, nxt)])
                        cur = nxt
                    Rf = Rb[hb][cur]
                    kRf = ("R", hb, cur)
                    bk5, k5 = nbank()
                    psU, psW = bk5[:, 0:128], bk5[:, 128:256]
                    p.op("pe", lambda e, psU=psU, Rf=Rf, hb=hb: e.matmul(psU, lhsT=Rf, rhs=Vb[hb], start=True, stop=True),
                         r=[kRf, ("Vb", hb)], w=[k5])
                    p.op("pe", lambda e, psW=psW, Rf=Rf, hb=hb: e.matmul(psW, lhsT=Kbg[hb], rhs=Rf, start=True, stop=True),
                         r=[kRf, ("Kbg", hb)], w=[k5])
                    evac(usb[hb], psU, [k5], [("usb", hb)])
                    evac(wT[hb], psW, [k5], [("wT", hb)])
                    Sh = S[:, (d * GH + h) * 128:(d * GH + h + 1) * 128]
                    kS = ("S", d, h)
                    bk6, k6 = nbank()
                    psa = bk6[:, 0:128]
                    p.op("pe", lambda e, hb=hb, Sh=Sh, psa=psa: e.matmul(psa, lhsT=wT[hb], rhs=Sh, start=True, stop=True),
                         r=[("wT", hb), kS], w=[k6])
                    p.op("dve", lambda e, hb=hb, psa=psa: e.tensor_tensor(out=vn[hb], in0=usb[hb], in1=psa, op=ALU.subtract),
                         r=[k6, ("usb", hb)], w=[("vn", hb)])
                    bk7, k7 = nbank()
                    psb_, psc = bk7[:, 0:128], bk7[:, 128:256]
                    p.op("pe", lambda e, hb=hb, Sh=Sh, psb_=psb_: e.matmul(psb_, lhsT=qdT[hb], rhs=Sh, start=True, stop=False),
                         r=[("qdT", hb), kS], w=[k7])
                    p.op("pe", lambda e, hb=hb, psb_=psb_: e.matmul(psb_, lhsT=attnT[hb], rhs=vn[hb], start=False, stop=True),
                         r=[("attnT", hb), ("vn", hb)], w=[k7])
                    p.op("pe", lambda e, hb=hb, psc=psc: e.matmul(psc, lhsT=kend[hb], rhs=vn[hb], start=True, stop=True),
                         r=[("kend", hb), ("vn", hb)], w=[k7])
                    p.op("act", lambda e, gb=gb, js=js, psb_=psb_: e.copy(out=osb[gb][:, js], in_=psb_), r=[k7], w=[("osb", gb, j)])
                    p.op("dve", lambda e, Sh=Sh, ub=ub, h=h, psc=psc: e.scalar_tensor_tensor(
                        out=Sh, in0=Sh, scalar=ge[ub][:, h:h + 1], in1=psc, op0=ALU.mult, op1=ALU.add),
                        r=[k7, kS, ("ge", ub)], w=[kS])
                p.dma("act", self.o_d[d, t * 128:(t + 1) * 128, h0 * 128:(h0 + HG) * 128], osb[gb],
                      r=[("osb", gb, j) for j in range(HG)], w=["od"])
    p.barrier()


K.stage_gdn = _stage_gdn


POOL_WIN = (2, 4, 8, 16)
NC3 = 20 * 128


def _pool_M(row_len, n, w):
    pos = np.arange(n) % row_len
    base = np.arange(n) - pos
    half = w // 2
    lo = np.clip(pos - half, 0, row_len - 1)
    hi = np.clip(pos + half - 1, 0, row_len - 1)
    M = np.zeros((n, n), np.float64)
    for tp in range(n):
        cnt = hi[tp] - lo[tp] + 1
        M[tp, base[tp] + lo[tp]:base[tp] + hi[tp] + 1] = 1.0 / cnt
        M[tp, tp] -= 1.0
    return M


def make_consts3():
    c = np.zeros((128, NC3), np.float32)
    for g, w in enumerate(POOL_WIN):
        Ml = _pool_M(64, 128, w)
        c[:, g * 128:(g + 1) * 128] = Ml.T
        Mc = _pool_M(256, 256, w)
        for i in range(2):
            for j in range(2):
                idx = 4 + g * 4 + i * 2 + j
                c[:, idx * 128:(idx + 1) * 128] = Mc[i * 128:(i + 1) * 128, j * 128:(j + 1) * 128].T
    return c


def _stage_mixout0(self, c3_d, poolw_d, tiles=range(NT)):
    p, A = self.p, self.A
    A.reset()
    v0 = self.v0
    self.yT_d = self.scratch("yT0", [NT, 128, KC * 128], BF16)
    c3 = A.f32(NC3)
    p.dma("sp", c3, c3_d, w=["c3"])
    pw = A.f32(8 * 256)
    p.dma("sp", pw.rearrange("p (g k d) -> p g k d", g=4, k=2), poolw_d.rearrange("g (k p) d -> p g k d", p=128), w=["pw"])
    pu = [A.f32(2048) for _ in range(2)]
    of_ = A.f32(3072)
    ob_ = A.f32(3072)
    zs = A.f32(3072)
    sq = A.f32(3072)
    y = [A.f32(D) for _ in range(2)]
    dT = A.f32(1024)
    yT = [A.bf16(D) for _ in range(2)]
    ss = A.f32(24)
    rstd = A.f32(24)
    inv128 = 1.0 / 128.0
    for it, t in enumerate(tiles):
        b = it % 2
        tsl = slice(t * 128, (t + 1) * 128)
        if t < 2:
            p.dma("sp", pu[b].rearrange("p (j c) -> p j c", j=2), self.poolu_d[0:256, :].rearrange("(j p) c -> p j c", p=128),
                  w=[("pu", b)])
        else:
            p.dma("sp", pu[b][:, 0:1024], self.poolu_d[tsl, :], w=[("pu", b)])
        p.dma("pool", of_, self.o_d[0, tsl, :], w=["of"])
        p.dma("pool", ob_, self.o_d[1, tsl, :], w=["ob"])
        p.dma("sp", zs, self.z_d[tsl, :], w=["zs"])
        for cc in range(8):
            g = cc // 2
            ps = self.psb[cc // 4]
            pk = ("pg", 0, cc // 4)
            o_ = ps[:, (cc % 4) * 128:(cc % 4 + 1) * 128]
            if t < 2:
                for j in range(2):
                    idx = 4 + g * 4 + t * 2 + j
                    p.op("pe", lambda e, o_=o_, b=b, cc=cc, j=j, idx=idx: e.matmul(
                        o_, lhsT=pu[b][:, j * 1024 + cc * 128:j * 1024 + (cc + 1) * 128], rhs=c3[:, idx * 128:(idx + 1) * 128],
                        start=(j == 0), stop=(j == 1)), r=[("pu", b), "c3"], w=[pk])
            else:
                p.op("pe", lambda e, o_=o_, b=b, cc=cc, g=g: e.matmul(
                    o_, lhsT=pu[b][:, cc * 128:(cc + 1) * 128], rhs=c3[:, g * 128:(g + 1) * 128], start=True, stop=True),
                    r=[("pu", b), "c3"], w=[pk])
        for h2 in range(2):
            p.op("act", lambda e, h2=h2: e.copy(out=dT[:, h2 * 512:(h2 + 1) * 512], in_=self.psb[h2]),
                 r=[("pg", 0, h2)], w=[("dT", h2)])
        for g in range(4):
            ps = self.psb[2 + g // 2]
            pk = ("pg", 1, g // 2)
            o_ = ps[:, (g % 2) * 256:(g % 2 + 1) * 256]
            for k2 in range(2):
                cc = g * 2 + k2
                p.op("pe", lambda e, o_=o_, cc=cc, g=g, k2=k2: e.matmul(
                    o_, lhsT=dT[:, cc * 128:(cc + 1) * 128], rhs=pw[:, (g * 2 + k2) * 256:(g * 2 + k2 + 1) * 256],
                    start=(k2 == 0), stop=(k2 == 1)), r=[("dT", cc // 4), "pw"], w=[pk])
        for h2 in range(2):
            p.op("dve", lambda e, h2=h2, b=b: e.tensor_tensor(
                out=y[b][:, h2 * 512:(h2 + 1) * 512], in0=self.psb[2 + h2], in1=v0[:, V0_PSC + h2 * 512:V0_PSC + (h2 + 1) * 512],
                op=ALU.mult), r=[("pg", 1, h2), "v0"], w=[("y", b, h2)])
        p.op("pool", lambda e: e.tensor_tensor(out=of_, in0=of_, in1=ob_, op=ALU.add), r=["of", "ob"], w=["of"])
        p.op("pool", lambda e: e.tensor_tensor(out=sq, in0=of_, in1=of_, op=ALU.mult), r=["of"], w=["sq"])
        p.op("dve", lambda e: e.tensor_reduce(out=ss, in_=sq.rearrange("p (h d) -> p h d", h=24), axis=AX.X, op=ALU.add),
             r=["sq"], w=["ss"])
        p.op("act", lambda e: e.activation(out=rstd, in_=ss, func=AF.Sqrt, bias=self.epsc, scale=inv128),
             r=["ss", "consts"], w=["rstd"])
        p.op("dve", lambda e: e.reciprocal(out=rstd, in_=rstd), r=["rstd"], w=["rstd"])
        for h in range(24):
            p.op("dve", lambda e, h=h, b=b: e.scalar_tensor_tensor(
                out=y[b][:, 1024 + h * 128:1024 + (h + 1) * 128], in0=of_[:, h * 128:(h + 1) * 128], scalar=rstd[:, h:h + 1],
                in1=v0[:, V0_GNW:V0_GNW + 128], op0=ALU.mult, op1=ALU.mult), r=["of", "rstd", "v0"], w=[("y", b, 2 + h)])
        p.op("pool", lambda e, b=b: e.tensor_tensor(out=y[b][:, 1024:4096], in0=y[b][:, 1024:4096], in1=zs, op=ALU.mult),
             r=[("y", b, 2 + h) for h in range(24)] + ["zs"], w=[("y", b, "g")])
        for q4 in range(8):
            ps = self.psb[4 + q4 % 4]
            pk = ("pT", q4 % 4)
            for j in range(4):
                kc = q4 * 4 + j
                p.op("pe", lambda e, ps=ps, j=j, kc=kc, b=b: e.transpose(ps[:, j * 128:(j + 1) * 128],
                                                                          y[b][:, kc * 128:(kc + 1) * 128], self.ident),
                     r=[("y", b, 0), ("y", b, 1), ("y", b, "g"), "consts"], w=[pk])
            if q4 % 2 == 0:
                p.op("act", lambda e, ps=ps, q4=q4, b=b: e.copy(out=yT[b][:, q4 * 512:(q4 + 1) * 512], in_=ps),
                     r=[pk], w=[("yT", b, q4)])
            else:
                p.op("dve", lambda e, ps=ps, q4=q4, b=b: e.tensor_copy(out=yT[b][:, q4 * 512:(q4 + 1) * 512], in_=ps),
                     r=[pk], w=[("yT", b, q4)])
        p.dma("act", self.yT_d[t], yT[b], r=[("yT", b, q4) for q4 in range(8)], w=["yTd"])
    p.barrier()


K.stage_mixout0 = _stage_mixout0


def _make_res_ep(self, src_d, dst_d, gi_L, gi_C, part_d=None):
    p = self.p
    st = {"cnt": 0, "blk": -1}

    def ep(t, bi, blk, ps_list, pkeys):
        A2 = Arena(self.arena_t, 44 * 1024)
        A2.reset(self.ep_base)
        gs = {"L": A2.f32(512), "C": A2.f32(512)}
        xs = [A2.f32(512) for _ in range(2)]
        tmp = [A2.f32(512) for _ in range(2)]
        pp = [A2.f32(512) for _ in range(2)]
        c0 = blk["c0"]
        nbw = blk["nbw"]
        if st["blk"] != bi:
            st["blk"] = bi
            p.dma("sp", gs["L"][:, 0:nbw], self.gbc_d[gi_L, :, c0:c0 + nbw], w=[("gs", "L")])
            p.dma("sp", gs["C"][:, 0:nbw], self.gbc_d[gi_C, :, c0:c0 + nbw], w=[("gs", "C")])
        i = st["cnt"] % 2
        st["cnt"] += 1
        side = "C" if t < 2 else "L"
        tsl = slice(t * 128, (t + 1) * 128)
        p.dma("pool", xs[i][:, 0:nbw], src_d[tsl, c0:c0 + nbw], w=[("xs", i)])
        if part_d is not None:
            p.dma("pool", pp[i][:, 0:nbw], part_d[tsl, c0:c0 + nbw], w=[("pp", i)])
            p.op("dve", lambda e: e.tensor_tensor(out=pp[i][:, 0:nbw], in0=ps_list[0][:, 0:nbw], in1=pp[i][:, 0:nbw], op=ALU.add),
                 r=[pkeys[0], ("pp", i)], w=[("pp", i)])
            p.op("dve", lambda e: e.tensor_tensor(out=tmp[i][:, 0:nbw], in0=pp[i][:, 0:nbw], in1=gs[side][:, 0:nbw], op=ALU.mult),
                 r=[("pp", i), ("gs", side)], w=[("tmp", i)])
        else:
            p.op("dve", lambda e: e.tensor_tensor(out=tmp[i][:, 0:nbw], in0=ps_list[0][:, 0:nbw], in1=gs[side][:, 0:nbw], op=ALU.mult),
                 r=[pkeys[0], ("gs", side)], w=[("tmp", i)])
        p.op("pool", lambda e: e.tensor_tensor(out=tmp[i][:, 0:nbw], in0=tmp[i][:, 0:nbw], in1=xs[i][:, 0:nbw], op=ALU.add),
             r=[("tmp", i), ("xs", i)], w=[("tmp", i)])
        p.dma("act", dst_d[tsl, c0:c0 + nbw], tmp[i][:, 0:nbw], r=[("tmp", i)], w=["resout"])
    return ep


K.make_res_ep = _make_res_ep


def _stage_outproj(self, yT_d, wout_d, src_d, dst_d, tiles=range(NT)):
    blocks = [dict(srcs=[wout_d[:, c:c + 512]], nbw=512, c0=c) for c in range(0, D, 512)]
    self.gemm_tm(yT_d, KC, blocks, self.make_res_ep(src_d, dst_d, 0, 1), tiles)


K.stage_outproj = _stage_outproj


def make_router(inp, layer):
    gw = inp["moe_group_w"][layer]
    ew = inp["moe_expert_w"][layer].transpose(1, 0, 2).reshape(D, 32)
    w = np.concatenate([gw, ew], 1)
    wr = np.ascontiguousarray(w.reshape(KC, 128, 36).transpose(1, 0, 2)).reshape(128, KC * 36)
    bias = np.concatenate([inp["moe_group_b"][layer].reshape(4), inp["moe_expert_b"][layer].reshape(32)])
    rb = np.broadcast_to(bias[None, :], (128, 36))
    return np.ascontiguousarray(np.concatenate([wr, rb], 1).astype(np.float32))


NRT = KC * 36 + 36


def _stage_router(self, fT32_d, rt_d, tiles=range(NT)):
    p, A = self.p, self.A
    A.reset()
    rt = A.f32(NRT)
    p.dma("sp", rt, rt_d, w=["rt"])
    wr = rt[:, 0:KC * 36].rearrange("p (k c) -> p k c", k=KC)
    rb = rt[:, KC * 36:KC * 36 + 36]
    ft = [A.f32(D) for _ in range(2)]
    gate = self.gate_sb

    def T(n):
        return A.f32(n)
    lg, ohg, eg, sel, oh1, sel2, oh2, g8 = T(36), T(4), T(4), T(8), T(8), T(8), T(8), T(8)
    gmax, ngmax, sumeg, pg, top1, top2, dlt, e21, den, w1, w2 = [T(1) for _ in range(11)]
    for it, t in enumerate(tiles):
        b = it % 2
        p.dma("sp" if it % 2 else "pool", ft[b], fT32_d[t], w=[("ft", b)])
        ft3 = ft[b].rearrange("p (k c) -> p k c", k=KC)
        ps = self.psb[it % 2]
        pk = ("pg", 0, it % 2)
        for kc in range(KC):
            p.op("pe", lambda e, ps=ps, ft3=ft3, kc=kc: e.matmul(ps[:, 0:36], lhsT=ft3[:, kc, :], rhs=wr[:, kc, :],
                                                                 start=(kc == 0), stop=(kc == KC - 1)),
                 r=[("ft", b), "rt"], w=[pk])
        R = "rtr"

        def dv(fn, extra_r=()):
            p.op("dve", fn, r=[R] + list(extra_r), w=[R])

        def ac(fn):
            p.op("act", fn, r=[R], w=[R])
        dv(lambda e, ps=ps: e.tensor_tensor(out=lg, in0=ps[:, 0:36], in1=rb, op=ALU.add), [pk, "rt"])
        dv(lambda e: e.tensor_reduce(out=gmax, in_=lg[:, 0:4], axis=AX.X, op=ALU.max))
        dv(lambda e: e.tensor_scalar(out=ohg, in0=lg[:, 0:4], scalar1=gmax[:, 0:1], scalar2=None, op0=ALU.is_equal))
        dv(lambda e: e.tensor_scalar(out=ngmax, in0=gmax, scalar1=-1.0, scalar2=None, op0=ALU.mult))
        ac(lambda e: e.activation(out=eg, in_=lg[:, 0:4], func=AF.Exp, bias=ngmax[:, 0:1]))
        dv(lambda e: e.tensor_reduce(out=sumeg, in_=eg, axis=AX.X, op=ALU.add))
        dv(lambda e: e.reciprocal(out=pg, in_=sumeg))
        dv(lambda e: e.tensor_scalar(out=sel, in0=lg[:, 4:12], scalar1=ohg[:, 0:1], scalar2=None, op0=ALU.mult))
        for g in range(1, 4):
            dv(lambda e, g=g: e.scalar_tensor_tensor(out=sel, in0=lg[:, 4 + 8 * g:12 + 8 * g], scalar=ohg[:, g:g + 1], in1=sel,
                                                      op0=ALU.mult, op1=ALU.add))
        dv(lambda e: e.tensor_reduce(out=top1, in_=sel, axis=AX.X, op=ALU.max))
        dv(lambda e: e.tensor_scalar(out=oh1, in0=sel, scalar1=top1[:, 0:1], scalar2=None, op0=ALU.is_equal))
        dv(lambda e: e.scalar_tensor_tensor(out=sel2, in0=oh1, scalar=-1.0e30, in1=sel, op0=ALU.mult, op1=ALU.add))
        dv(lambda e: e.tensor_reduce(out=top2, in_=sel2, axis=AX.X, op=ALU.max))
        dv(lambda e: e.tensor_scalar(out=oh2, in0=sel2, scalar1=top2[:, 0:1], scalar2=None, op0=ALU.is_equal))
        dv(lambda e: e.tensor_tensor(out=dlt, in0=top2, in1=top1, op=ALU.subtract))
        ac(lambda e: e.activation(out=e21, in_=dlt, func=AF.Exp))
        dv(lambda e: e.tensor_scalar(out=den, in0=e21, scalar1=1.0, scalar2=None, op0=ALU.add))
        dv(lambda e: e.reciprocal(out=w1, in_=den))
        dv(lambda e: e.tensor_tensor(out=w2, in0=e21, in1=w1, op=ALU.mult))
        dv(lambda e: e.tensor_tensor(out=w1, in0=w1, in1=pg, op=ALU.mult))
        dv(lambda e: e.tensor_tensor(out=w2, in0=w2, in1=pg, op=ALU.mult))
        dv(lambda e: e.tensor_scalar(out=g8, in0=oh1, scalar1=w1[:, 0:1], scalar2=None, op0=ALU.mult))
        dv(lambda e: e.scalar_tensor_tensor(out=g8, in0=oh2, scalar=w2[:, 0:1], in1=g8, op0=ALU.mult, op1=ALU.add))
        for g in range(4):
            p.op("dve", lambda e, g=g, t=t: e.tensor_scalar(out=gate[:, t * 32 + g * 8:t * 32 + (g + 1) * 8], in0=g8,
                                                           scalar1=ohg[:, g:g + 1], scalar2=None, op0=ALU.mult),
                 r=[R], w=[("gate", t)])
    p.barrier()


K.stage_router = _stage_router


def _stage_moe(self, fT_d, w1_d, w3_d, w2_d, src_d, dst_d, tiles=range(NT), tag="0"):
    p = self.p
    gate = self.gate_sb
    self.aT_d = self.scratch(f"aT_{tag}", [NT, 128, 128 * 128], BF16)
    self.ypart_d = self.scratch(f"ypart_{tag}", [NTOK, D], F32)
    st = {"cnt": 0}

    def ep_up(t, bi, blk, ps_list, pkeys):
        A2 = Arena(self.arena_t, 44 * 1024)
        A2.reset(self.ep_base)
        s_sb = [A2.f32(512) for _ in range(2)]
        a_sb = [A2.f32(512) for _ in range(2)]
        aT = [A2.bf16(512) for _ in range(2)]
        i = st["cnt"] % 2
        st["cnt"] += 1
        ex = blk["e"]
        p.op("act", lambda e: e.activation(out=s_sb[i], in_=ps_list[0], func=AF.Silu), r=[pkeys[0]], w=[("s_sb", i)])
        p.op("dve", lambda e: e.scalar_tensor_tensor(out=a_sb[i], in0=s_sb[i], scalar=gate[:, t * 32 + ex:t * 32 + ex + 1],
                                                     in1=ps_list[1], op0=ALU.mult, op1=ALU.mult),
             r=[("s_sb", i), pkeys[1], ("gate", t)], w=[("a_sb", i)])
        pst = self.psb[4 + i]
        for j in range(4):
            p.op("pe", lambda e, j=j: e.transpose(pst[:, j * 128:(j + 1) * 128], a_sb[i][:, j * 128:(j + 1) * 128], self.ident),
                 r=[("a_sb", i), "consts"], w=[("pst4", i)])
        p.op("act", lambda e: e.copy(out=aT[i], in_=pst), r=[("pst4", i)], w=[("aT", i)])
        p.dma("act", self.aT_d[t][:, ex * 512:(ex + 1) * 512], aT[i], r=[("aT", i)], w=["aTd"])

    blocks = [dict(srcs=[w1_d[ex], w3_d[ex]], nbw=512, e=ex) for ex in range(32)]
    self.gemm_tm(fT_d, KC, blocks, ep_up, tiles, wsets=1)

    st2 = {"cnt": 0}

    def ep_part(t, bi, blk, ps_list, pkeys):
        A2 = Arena(self.arena_t, 44 * 1024)
        A2.reset(self.ep_base)
        pb = [A2.f32(512) for _ in range(2)]
        i = st2["cnt"] % 2
        st2["cnt"] += 1
        c0 = blk["c0"]
        p.op("act", lambda e: e.copy(out=pb[i], in_=ps_list[0]), r=[pkeys[0]], w=[("pb", i)])
        p.dma("act", self.ypart_d[t * 128:(t + 1) * 128, c0:c0 + 512], pb[i], r=[("pb", i)], w=["ypd"])

    blocks = [dict(srcs=[w2_d[0:8192, c:c + 512]], nbw=512, c0=c) for c in range(0, D, 512)]
    self.gemm_tm(self.aT_d, 64, blocks, ep_part, tiles, wsets=1, at_c0=0)
    blocks = [dict(srcs=[w2_d[8192:16384, c:c + 512]], nbw=512, c0=c) for c in range(0, D, 512)]
    self.gemm_tm(self.aT_d, 64, blocks, self.make_res_ep(src_d, dst_d, 2, 3, part_d=self.ypart_d), tiles, wsets=1, at_c0=64)


K.stage_moe = _stage_moe


LH = 6
V1_GB = 0
V1_NW = 24
NV1 = 536
C4_CC = 0
C4_SC = 512
NC4 = 1024


def make_vecs1(inp):
    v = np.zeros((128, NV1), np.float32)
    gb = inp["gla_gate_b"][0]
    v[:, V1_GB:V1_GB + 24] = gb.reshape(2, 12, 128).transpose(2, 0, 1).reshape(128, 24)
    v[:, V1_NW:V1_NW + 512] = inp["gla_norm_w"][0].reshape(1, 512)
    return v


def make_gup(inp):
    gu = inp["gla_gate_up"][0]
    v = np.zeros((64, 3072), np.float32)
    v[0:16, 0:1536] = gu[0]
    v[32:48, 1536:3072] = gu[1]
    return v


def make_lrw(inp):
    w = inp["cd_w_in"][0][:, 10240:10272]
    o = np.zeros((D, 64), np.float32)
    o[:, 0:16] = w[:, 0:16]
    o[:, 32:48] = w[:, 16:32]
    return o


def make_consts4():
    c = np.zeros((128, NC4), np.float32)
    i = np.arange(256)
    ang = 2 * np.pi * np.outer(i, i) / 256.0
    Cc = np.cos(ang) / 16.0
    Sc = np.sin(ang) / 16.0
    c[:, C4_CC:C4_CC + 512] = Cc.reshape(2, 128, 256).transpose(1, 0, 2).reshape(128, 512)
    c[:, C4_SC:C4_SC + 512] = Sc.reshape(2, 128, 256).transpose(1, 0, 2).reshape(128, 512)
    return c


def make_dft_big():
    import ml_dtypes
    i = np.arange(4096)
    ang = 2 * np.pi * ((np.outer(i, i)) % 4096) / 4096.0
    cn = (np.cos(ang) / 64.0).astype(ml_dtypes.bfloat16)
    sn = (-np.sin(ang) / 64.0).astype(ml_dtypes.bfloat16)
    return cn, sn


def _stage_inproj1(self, hT_d, w_d, lrw_d, v1_d):
    p = self.p
    self.v1t = self.PA_v1
    p.dma("sp", self.v1t[:, 0:NV1], v1_d, w=["v1"])
    self.rawf_d = self.scratch("rawf", [8, 128, NTOK], F32)
    self.rawqk_d = self.scratch("rawqk1", [24, 128, NTOK], F32)
    self.v1_d = self.scratch("v1tm", [NTOK, 3072], F32)
    self.z1_d = self.scratch("zs1", [NTOK, 3072], F32)
    self.lrT_d = self.scratch("lrT", [64, NTOK], F32)

    def mk_ep_tm(dst, silu):
        st = {"cnt": 0}

        def ep(t, bi, blk, ps_list, pkeys):
            A2 = Arena(self.arena_t, 44 * 1024)
            A2.reset(self.ep_base)
            bufs = [A2.f32(512) for _ in range(2)]
            i = st["cnt"] % 2
            st["cnt"] += 1
            ob = bufs[i]
            if silu:
                p.op("act", lambda e: e.activation(out=ob, in_=ps_list[0], func=AF.Silu), r=[pkeys[0]], w=[("epo", i)])
            else:
                p.op("act", lambda e: e.copy(out=ob, in_=ps_list[0]), r=[pkeys[0]], w=[("epo", i)])
            p.dma("act", dst[t * 128:(t + 1) * 128, bi * 512:(bi + 1) * 512], ob, r=[("epo", i)], w=["eptm"])
        return ep

    def mk_ep_fm(dst, ncol=512):
        st = {"cnt": 0}

        def ep(t, bi, blk, ps_list, pkeys):
            A2 = Arena(self.arena_t, 44 * 1024)
            A2.reset(self.ep_base)
            tm = [A2.f32(512) for _ in range(2)]
            fm = [A2.f32(512) for _ in range(2)]
            i = st["cnt"] % 2
            st["cnt"] += 1
            nj = ncol // 128 if ncol >= 128 else 1
            p.op("act", lambda e: e.copy(out=tm[i][:, 0:ncol], in_=ps_list[0][:, 0:ncol]), r=[pkeys[0]], w=[("tm", i)])
            pst = self.psb[4 + i]
            if ncol >= 128:
                for j in range(nj):
                    p.op("pe", lambda e, j=j: e.transpose(pst[:, j * 128:(j + 1) * 128], tm[i][:, j * 128:(j + 1) * 128], self.ident),
                         r=[("tm", i), "consts"], w=[("pst4", i)])
                p.op("dve", lambda e: e.tensor_copy(out=fm[i], in_=pst), r=[("pst4", i)], w=[("fm", i)])
                p.dma("act", dst[bi * 4:(bi + 1) * 4, :, t * 128:(t + 1) * 128].rearrange("j p c -> p j c"),
                      fm[i].rearrange("p (j c) -> p j c", j=4), r=[("fm", i)], w=["epfm"])
            else:
                p.op("pe", lambda e: e.transpose(pst[0:ncol, 0:128], tm[i][:, 0:ncol], self.ident),
                     r=[("tm", i), "consts"], w=[("pst4", i)])
                p.op("dve", lambda e: e.tensor_copy(out=fm[i][0:ncol, 0:128], in_=pst[0:ncol, 0:128]), r=[("pst4", i)], w=[("fm", i)])
                p.dma("act", dst[:, t * 128:(t + 1) * 128], fm[i][0:ncol, 0:128], r=[("fm", i)], w=["epfm"])
        return ep

    self.gemm_tm(hT_d, KC, [dict(srcs=[w_d[:, c:c + 512]], nbw=512) for c in range(0, 1024, 512)], mk_ep_fm(self.rawf_d), range(NT))
    self.gemm_tm(hT_d, KC, [dict(srcs=[w_d[:, 1024 + c:1024 + c + 512]], nbw=512) for c in range(0, 3072, 512)],
                 mk_ep_fm(self.rawqk_d), range(NT))
    self.gemm_tm(hT_d, KC, [dict(srcs=[w_d[:, 4096 + c:4096 + c + 512]], nbw=512) for c in range(0, 3072, 512)],
                 mk_ep_tm(self.v1_d, False), range(NT))
    self.gemm_tm(hT_d, KC, [dict(srcs=[w_d[:, 7168 + c:7168 + c + 512]], nbw=512) for c in range(0, 3072, 512)],
                 mk_ep_tm(self.z1_d, True), range(NT))
    self.gemm_tm(hT_d, KC, [dict(srcs=[lrw_d], nbw=64)], mk_ep_fm(self.lrT_d, ncol=64), range(NT))


K.stage_inproj1 = _stage_inproj1


def _stage_gk(self, gup_d):
    p, A = self.p, self.A
    A.reset()
    v1 = self.v1t
    self.gkT_d = self.scratch("gkT", [2, 12, 128, NTOK], F32)
    lrT = A.f32(NTOK)
    p.dma("sp", lrT[0:64, :], self.lrT_d, w=["lrT"])
    gup = A.f32(3072)
    p.dma("sp", gup[0:64, :], gup_d, w=["gup"])
    negb = A.f32(24)
    p.op("dve", lambda e: e.tensor_scalar(out=negb, in0=v1[:, V1_GB:V1_GB + 24], scalar1=-1.0, scalar2=None, op0=ALU.mult),
         r=["v1"], w=["negb1"])
    ob = [A.f32(512) for _ in range(2)]
    cnt = 0
    for d in range(2):
        for ch in range(12):
            for c0 in range(0, NTOK, 512):
                cn = min(512, NTOK - c0)
                i = cnt % 2
                cnt += 1
                ps = self.psb[i]
                pk = ("pg", 0, i)
                p.op("pe", lambda e, ps=ps, d=d, ch=ch, c0=c0, cn=cn: e.matmul(
                    ps[:, 0:cn], lhsT=gup[0:64, d * 1536 + ch * 128:d * 1536 + (ch + 1) * 128],
                    rhs=lrT[0:64, c0:c0 + cn], start=True, stop=True), r=["gup", "lrT"], w=[pk])
                p.op("act", lambda e, ps=ps, i=i, d=d, ch=ch, cn=cn: e.activation(
                    out=ob[i][:, 0:cn], in_=ps[:, 0:cn], func=AF.Exp, bias=negb[:, d * 12 + ch:d * 12 + ch + 1], scale=-1.0),
                    r=[pk, "negb1"], w=[("gko", i)])
                p.op("act", lambda e, i=i, cn=cn: e.activation(out=ob[i][:, 0:cn], in_=ob[i][:, 0:cn], func=AF.Ln, bias=self.ones[:, 0:1]),
                     r=[("gko", i), "consts"], w=[("gko", i)])
                p.op("dve", lambda e, i=i, cn=cn: e.tensor_scalar(out=ob[i][:, 0:cn], in0=ob[i][:, 0:cn], scalar1=-1.0 / 16.0,
                                                                 scalar2=None, op0=ALU.mult), r=[("gko", i)], w=[("gko", i)])
                p.dma("sp", self.gkT_d[d, ch, :, c0:c0 + cn], ob[i][:, 0:cn], r=[("gko", i)], w=["gkd"])
    p.barrier()


K.stage_gk = _stage_gk


def _stage_gla(self, c2_d, nsteps=NT):
    p, A = self.p, self.A
    A.reset()
    self.o1_d = self.scratch("ogla", [2, NTOK, 3072], F32)
    c2 = A.f32(NC2)
    p.dma("sp", c2, c2_d, w=["c2"])
    S = A.f32(2 * LH * 2 * 512)
    p.op("pool", lambda e: e.memset(S, 0.0), w=[("S1", d, h) for d in range(2) for h in range(LH)])
    onesf = self.ones
    NB = 2
    qT = [A.f32(256) for _ in range(NB)]
    kT = [A.f32(256) for _ in range(NB)]
    gk = [A.f32(256) for _ in range(NB)]
    vt = [A.f32(512) for _ in range(NB)]
    G = [A.f32(256) for _ in range(NB)]
    eP = [A.f32(256) for _ in range(NB)]
    eN = [A.f32(256) for _ in range(NB)]
    eE = [A.f32(256) for _ in range(NB)]
    qd = [A.f32(256) for _ in range(NB)]
    ki = [A.f32(256) for _ in range(NB)]
    keT = [A.f32(256) for _ in range(NB)]
    ke = [A.f32(256) for _ in range(NB)]
    attnT = [A.f32(128) for _ in range(NB)]
    tot = [A.f32(2) for _ in range(NB)]
    ge = [A.f32(2) for _ in range(NB)]
    osb = [A.f32(512) for _ in range(NB)]
    bctr = [0]
    BANKS = [0, 1, 2, 3, 4, 5, 6, 7]

    def nbank():
        i = BANKS[bctr[0] % len(BANKS)]
        bctr[0] += 1
        return self.psb[i], ("bank", i)

    order = {0: list(range(NT)), 1: [1, 0] + list(range(NT - 1, 1, -1))}
    uc = 0
    for step in range(nsteps):
        for d in range(2):
            t = order[d][step]
            tsl = slice(t * 128, (t + 1) * 128)
            mk = c2[:, C2_MBI:C2_MBI + 128] if d == 0 else c2[:, C2_MFI:C2_MFI + 128]
            for h in range(LH):
                b = uc % NB
                uc += 1
                p.dma("sp", qT[b].rearrange("p (k c) -> p k c", k=2), self.rawqk_d[2 * h:2 * h + 2, :, tsl].rearrange("k p c -> p k c"),
                      w=[("qT1", b)])
                p.dma("pool", kT[b].rearrange("p (k c) -> p k c", k=2),
                      self.rawqk_d[12 + 2 * h:12 + 2 * h + 2, :, tsl].rearrange("k p c -> p k c"), w=[("kT1", b)])
                p.dma("sp", gk[b].rearrange("p (k c) -> p k c", k=2), self.gkT_d[d, 2 * h:2 * h + 2, :, tsl].rearrange("k p c -> p k c"),
                      w=[("gk1", b)])
                p.dma("pool", vt[b], self.v1_d[tsl, h * 512:(h + 1) * 512], w=[("vt1", b)])
                for kc in range(2):
                    ks = slice(kc * 128, (kc + 1) * 128)
                    p.op("dve", lambda e, b=b, ks=ks: e.tensor_tensor_scan(out=G[b][:, ks], data0=onesf, data1=gk[b][:, ks],
                                                                          initial=0.0, op0=ALU.mult, op1=ALU.add),
                         r=[("gk1", b), "consts"], w=[("G1", b, kc)])
                    p.op("pool", lambda e, b=b, kc=kc: e.tensor_copy(out=tot[b][:, kc:kc + 1], in_=G[b][:, kc * 128 + 127:kc * 128 + 128]),
                         r=[("G1", b, kc)], w=[("tot1", b, kc)])
                    if d == 1:
                        p.op("dve", lambda e, b=b, ks=ks, kc=kc: e.tensor_scalar(out=G[b][:, ks], in0=G[b][:, ks], scalar1=-1.0,
                                                                                 scalar2=tot[b][:, kc:kc + 1], op0=ALU.mult, op1=ALU.add),
                             r=[("G1", b, kc), ("tot1", b, kc)], w=[("G1", b, kc)])
                        p.op("dve", lambda e, b=b, ks=ks: e.tensor_tensor(out=G[b][:, ks], in0=G[b][:, ks], in1=gk[b][:, ks], op=ALU.add),
                             r=[("G1", b, kc), ("gk1", b)], w=[("G1", b, kc)])
                    p.op("act", lambda e, b=b, ks=ks: e.activation(out=eP[b][:, ks], in_=G[b][:, ks], func=AF.Exp),
                         r=[("G1", b, kc)], w=[("eP", b, kc)])
                    p.op("act", lambda e, b=b, ks=ks: e.activation(out=eN[b][:, ks], in_=G[b][:, ks], func=AF.Exp, scale=-1.0),
                         r=[("G1", b, kc)], w=[("eN", b, kc)])
                    p.op("act", lambda e, b=b, ks=ks, kc=kc: e.activation(out=eE[b][:, ks], in_=G[b][:, ks], func=AF.Exp, scale=-1.0,
                                                                          bias=tot[b][:, kc:kc + 1]),
                         r=[("G1", b, kc), ("tot1", b, kc)], w=[("eE", b, kc)])
                    p.op("act", lambda e, b=b, kc=kc: e.activation(out=ge[b][:, kc:kc + 1], in_=tot[b][:, kc:kc + 1], func=AF.Exp),
                         r=[("tot1", b, kc)], w=[("ge1", b, kc)])
                    p.op("dve", lambda e, b=b, ks=ks: e.scalar_tensor_tensor(out=qd[b][:, ks], in0=qT[b][:, ks], scalar=0.0625,
                                                                             in1=eP[b][:, ks], op0=ALU.mult, op1=ALU.mult),
                         r=[("qT1", b), ("eP", b, kc)], w=[("qd", b, kc)])
                    p.op("pool", lambda e, b=b, ks=ks: e.tensor_tensor(out=ki[b][:, ks], in0=kT[b][:, ks], in1=eN[b][:, ks], op=ALU.mult),
                         r=[("kT1", b), ("eN", b, kc)], w=[("ki", b, kc)])
                    p.op("pool", lambda e, b=b, ks=ks: e.tensor_tensor(out=keT[b][:, ks], in0=kT[b][:, ks], in1=eE[b][:, ks], op=ALU.mult),
                         r=[("kT1", b), ("eE", b, kc)], w=[("keT", b, kc)])
                bk1, k1 = nbank()
                for kc in range(2):
                    ks = slice(kc * 128, (kc + 1) * 128)
                    p.op("pe", lambda e, bk1=bk1, b=b, ks=ks, kc=kc: e.matmul(bk1[:, 0:128], lhsT=ki[b][:, ks], rhs=qd[b][:, ks],
                                                                              start=(kc == 0), stop=(kc == 1)),
                         r=[("ki", b, kc), ("qd", b, kc)], w=[k1])
                p.op("dve", lambda e, bk1=bk1, b=b, mk=mk: e.tensor_tensor(out=attnT[b], in0=bk1[:, 0:128], in1=mk, op=ALU.mult),
                     r=[k1, "c2"], w=[("attnT1", b)])
                bk2, k2 = nbank()
                for kc in range(2):
                    ks = slice(kc * 128, (kc + 1) * 128)
                    p.op("pe", lambda e, bk2=bk2, b=b, ks=ks: e.transpose(bk2[:, ks], keT[b][:, ks], self.ident),
                         r=[("keT", b, kc), "consts"], w=[k2])
                p.op("act", lambda e, bk2=bk2, b=b: e.copy(out=ke[b], in_=bk2[:, 0:256]), r=[k2], w=[("ke", b)])
                bk3, k3 = nbank()
                Sb = (d * LH + h) * 1024
                kS = ("S1", d, h)
                for kc in range(2):
                    ks = slice(kc * 128, (kc + 1) * 128)
                    p.op("pe", lambda e, bk3=bk3, b=b, ks=ks, kc=kc, Sb=Sb: e.matmul(
                        bk3, lhsT=qd[b][:, ks], rhs=S[:, Sb + kc * 512:Sb + (kc + 1) * 512], start=(kc == 0), stop=False),
                        r=[("qd", b, kc), kS], w=[k3])
                p.op("pe", lambda e, bk3=bk3, b=b: e.matmul(bk3, lhsT=attnT[b], rhs=vt[b], start=False, stop=True),
                     r=[("attnT1", b), ("vt1", b)], w=[k3])
                p.op("act", lambda e, bk3=bk3, b=b: e.copy(out=osb[b], in_=bk3), r=[k3], w=[("osb1", b)])
                p.dma("act", self.o1_d[d, tsl, h * 512:(h + 1) * 512], osb[b], r=[("osb1", b)], w=["o1d"])
                for kc in range(2):
                    ks = slice(kc * 128, (kc + 1) * 128)
                    bk4, k4 = nbank()
                    p.op("pe", lambda e, bk4=bk4, b=b, ks=ks: e.matmul(bk4, lhsT=ke[b][:, ks], rhs=vt[b], start=True, stop=True),
                         r=[("ke", b), ("vt1", b)], w=[k4])
                    Ss = S[:, Sb + kc * 512:Sb + (kc + 1) * 512]
                    p.op("dve", lambda e, bk4=bk4, Ss=Ss, b=b, kc=kc: e.scalar_tensor_tensor(
                        out=Ss, in0=Ss, scalar=ge[b][:, kc:kc + 1], in1=bk4, op0=ALU.mult, op1=ALU.add),
                        r=[k4, kS, ("ge1", b, kc)], w=[kS])
    p.barrier()


K.stage_gla = _stage_gla


def _stage_fourier(self, c4_d, cn_d, sn_d):
    p, A = self.p, self.A
    A.reset()
    self.PQ_d = self.scratch("PQ", [4, 128, 32 * 512], BF16)
    self.fT_d = self.scratch("fTf", [8, 128, NLAT], F32)
    c4 = A.f32(NC4)
    p.dma("sp", c4, c4_d, w=["c4"])
    ut = [A.f32(256) for _ in range(2)]
    pq = [A.bf16(512) for _ in range(2)]
    cnt = 0
    for g in range(4):
        for tt in range(32):
            t = tt + 2
            i = cnt % 2
            cnt += 1
            p.dma("sp", ut[i].rearrange("p (k c) -> p k c", k=2),
                  self.rawf_d[2 * g:2 * g + 2, :, t * 128:(t + 1) * 128].rearrange("k p c -> p k c"), w=[("ut", i)])
            ps = self.psb[i]
            pk = ("pg", 0, i)
            for part, off in ((0, C4_CC), (1, C4_SC)):
                for k2 in range(2):
                    p.op("pe", lambda e, ps=ps, i=i, k2=k2, part=part, off=off: e.matmul(
                        ps[:, part * 256:(part + 1) * 256], lhsT=ut[i][:, k2 * 128:(k2 + 1) * 128],
                        rhs=c4[:, off + k2 * 256:off + (k2 + 1) * 256], start=(k2 == 0), stop=(k2 == 1)),
                        r=[("ut", i), "c4"], w=[pk])
            p.op("act", lambda e, ps=ps, i=i: e.copy(out=pq[i], in_=ps), r=[pk], w=[("pq", i)])
            p.dma("act", self.PQ_d[g][:, tt * 512:(tt + 1) * 512], pq[i], r=[("pq", i)], w=["PQd"])
    p.barrier()
    A.reset()
    cnb = A.bf16(32 * 512)
    snb = A.bf16(32 * 512)
    PQ = [A.bf16(32 * 512) for _ in range(2)]
    ob = [A.f32(512) for _ in range(2)]
    cnt = 0
    oc = 0
    for nb in range(8):
        p.dma("sp", cnb.rearrange("p (t c) -> p t c", t=32), cn_d[:, nb * 512:(nb + 1) * 512].rearrange("(t p) c -> p t c", p=128),
              w=["cnb"])
        p.dma("pool", snb.rearrange("p (t c) -> p t c", t=32), sn_d[:, nb * 512:(nb + 1) * 512].rearrange("(t p) c -> p t c", p=128),
              w=["snb"])
        for g in range(4):
            i = cnt % 2
            cnt += 1
            p.dma("sp" if i else "pool", PQ[i], self.PQ_d[g], w=[("PQ", i)])
            for cc in range(2):
                j = oc % 2
                oc += 1
                ps = self.psb[j]
                pk = ("pg", 0, j)
                for tt in range(32):
                    p.op("pe", lambda e, ps=ps, i=i, tt=tt, cc=cc: e.matmul(
                        ps, lhsT=PQ[i][:, tt * 512 + cc * 128:tt * 512 + (cc + 1) * 128], rhs=cnb[:, tt * 512:(tt + 1) * 512],
                        start=(tt == 0), stop=False), r=[("PQ", i), "cnb"], w=[pk])
                    p.op("pe", lambda e, ps=ps, i=i, tt=tt, cc=cc: e.matmul(
                        ps, lhsT=PQ[i][:, tt * 512 + 256 + cc * 128:tt * 512 + 256 + (cc + 1) * 128], rhs=snb[:, tt * 512:(tt + 1) * 512],
                        start=False, stop=(tt == 31)), r=[("PQ", i), "snb"], w=[pk])
                p.op("act", lambda e, ps=ps, j=j: e.copy(out=ob[j], in_=ps), r=[pk], w=[("fob", j)])
                p.dma("act", self.fT_d[g * 2 + cc][:, nb * 512:(nb + 1) * 512], ob[j], r=[("fob", j)], w=["fTd"])
    p.barrier()


K.stage_fourier = _stage_fourier


def _stage_mixout1(self, fw_d, tiles=range(2, NT)):
    p, A = self.p, self.A
    A.reset()
    v1 = self.v1t
    self.yT1_d = self.scratch("yT1", [NT, 128, KC * 128], BF16)
    fw = A.f32(8 * 256)
    p.dma("sp", fw.rearrange("p (g k d) -> p g k d", g=4, k=2), fw_d.rearrange("g (k p) d -> p g k d", p=128), w=["fw"])
    ft = [A.f32(1024) for _ in range(2)]
    of_ = A.f32(3072)
    ob_ = A.f32(3072)
    zs = A.f32(3072)
    sq = A.f32(3072)
    y = [A.f32(D) for _ in range(2)]
    yT = [A.bf16(D) for _ in range(2)]
    ss = A.f32(8)
    rstd = A.f32(8)
    for it, t in enumerate(tiles):
        b = it % 2
        tt = t - 2
        tsl = slice(t * 128, (t + 1) * 128)
        p.dma("sp", ft[b].rearrange("p (k c) -> p k c", k=8), self.fT_d[:, :, tt * 128:(tt + 1) * 128].rearrange("k p c -> p k c"),
              w=[("ft1", b)])
        p.dma("pool", of_, self.o1_d[0, tsl, :], w=["of"])
        p.dma("pool", ob_, self.o1_d[1, tsl, :], w=["ob"])
        p.dma("sp", zs, self.z1_d[tsl, :], w=["zs"])
        for g in range(4):
            ps = self.psb[g // 2]
            pk = ("pg", 0, g // 2)
            o_ = ps[:, (g % 2) * 256:(g % 2 + 1) * 256]
            for k2 in range(2):
                cc = g * 2 + k2
                p.op("pe", lambda e, o_=o_, cc=cc, b=b: e.matmul(
                    o_, lhsT=ft[b][:, cc * 128:(cc + 1) * 128], rhs=fw[:, cc * 256:(cc + 1) * 256], start=(cc % 2 == 0), stop=(cc % 2 == 1)),
                    r=[("ft1", b), "fw"], w=[pk])
        for h2 in range(2):
            p.op("act", lambda e, h2=h2, b=b: e.copy(out=y[b][:, h2 * 512:(h2 + 1) * 512], in_=self.psb[h2]),
                 r=[("pg", 0, h2)], w=[("y", b, h2)])
        p.op("pool", lambda e: e.tensor_tensor(out=of_, in0=of_, in1=ob_, op=ALU.add), r=["of", "ob"], w=["of"])
        p.op("pool", lambda e: e.tensor_tensor(out=sq, in0=of_, in1=of_, op=ALU.mult), r=["of"], w=["sq"])
        p.op("dve", lambda e: e.tensor_reduce(out=ss[:, 0:6], in_=sq.rearrange("p (h d) -> p h d", h=6), axis=AX.X, op=ALU.add),
             r=["sq"], w=["ss"])
        p.op("act", lambda e: e.activation(out=rstd[:, 0:6], in_=ss[:, 0:6], func=AF.Sqrt, bias=self.epsc, scale=1.0 / 512.0),
             r=["ss", "consts"], w=["rstd"])
        p.op("dve", lambda e: e.reciprocal(out=rstd[:, 0:6], in_=rstd[:, 0:6]), r=["rstd"], w=["rstd"])
        for h in range(6):
            p.op("dve", lambda e, h=h, b=b: e.scalar_tensor_tensor(
                out=y[b][:, 1024 + h * 512:1024 + (h + 1) * 512], in0=of_[:, h * 512:(h + 1) * 512], scalar=rstd[:, h:h + 1],
                in1=v1[:, V1_NW:V1_NW + 512], op0=ALU.mult, op1=ALU.mult), r=["of", "rstd", "v1"], w=[("y", b, 2 + h)])
        p.op("pool", lambda e, b=b: e.tensor_tensor(out=y[b][:, 1024:4096], in0=y[b][:, 1024:4096], in1=zs, op=ALU.mult),
             r=[("y", b, 2 + h) for h in range(6)] + ["zs"], w=[("y", b, "g")])
        for q4 in range(8):
            ps = self.psb[4 + q4 % 4]
            pk = ("pT", q4 % 4)
            for j in range(4):
                kc = q4 * 4 + j
                p.op("pe", lambda e, ps=ps, j=j, kc=kc, b=b: e.transpose(ps[:, j * 128:(j + 1) * 128],
                                                                          y[b][:, kc * 128:(kc + 1) * 128], self.ident),
                     r=[("y", b, 0), ("y", b, 1), ("y", b, "g"), "consts"], w=[pk])
            if q4 % 2 == 0:
                p.op("act", lambda e, ps=ps, q4=q4, b=b: e.copy(out=yT[b][:, q4 * 512:(q4 + 1) * 512], in_=ps),
                     r=[pk], w=[("yT", b, q4)])
            else:
                p.op("dve", lambda e, ps=ps, q4=q4, b=b: e.tensor_copy(out=yT[b][:, q4 * 512:(q4 + 1) * 512], in_=ps),
                     r=[pk], w=[("yT", b, q4)])
        p.dma("act", self.yT1_d[t], yT[b], r=[("yT", b, q4) for q4 in range(8)], w=["yTd"])
    p.barrier()


K.stage_mixout1 = _stage_mixout1


def _stage_final(self, x_d, fnw_d, out_d, tiles=range(2, NT)):
    p, A = self.p, self.A
    A.reset()
    fnw = A.f32(D)
    p.dma("sp", fnw, fnw_d, w=["fnw"])
    xt = [A.f32(D) for _ in range(2)]
    junk = A.f32(D)
    ss = [A.f32(1) for _ in range(2)]
    for it, t in enumerate(tiles):
        b = it % 2
        p.dma("sp", xt[b], x_d[t * 128:(t + 1) * 128, :], w=[("xt", b)])
        p.op("act", lambda e, b=b: e.activation(out=junk, in_=xt[b], func=AF.Square, accum_out=ss[b]), r=[("xt", b)], w=["junk", ("ss", b)])
        p.op("act", lambda e, b=b: e.activation(out=ss[b], in_=ss[b], func=AF.Sqrt, bias=self.epsc, scale=1.0 / D),
             r=[("ss", b), "consts"], w=[("ss", b)])
        p.op("dve", lambda e, b=b: e.reciprocal(out=ss[b], in_=ss[b]), r=[("ss", b)], w=[("ss", b)])
        p.op("dve", lambda e, b=b: e.scalar_tensor_tensor(out=xt[b], in0=xt[b], scalar=ss[b][:, 0:1], in1=fnw, op0=ALU.mult, op1=ALU.mult),
             r=[("xt", b), ("ss", b), "fnw"], w=[("xt", b)])
        p.dma("act", out_d[(t - 2) * 128:(t - 1) * 128, :], xt[b], r=[("xt", b)], w=["outd"])
    p.barrier()


K.stage_final = _stage_final


W_NAMES = ["modw0", "modw1", "win0", "wout0", "poolw", "win1", "lrw", "gup", "wout1", "fw",
           "w1_0", "w3_0", "w2_0", "w1_1", "w3_1", "w2_1"]


def build_full():
    k = K()
    k.setup()
    e = k.ext_in
    xin_d = e("xin", [NTOK, D])
    cvec_d = e("cvec", [64, 128])
    modw_d = [e("modw0", [D, 6 * D]), e("modw1", [D, 6 * D])]
    vecs_d = [e("vecs0", [128, NVEC]), e("vecs1", [128, NVEC])]
    v0_d = e("v0", [128, NV0])
    v1_d = e("v1", [128, NV1])
    c2_d = e("c2", [128, NC2])
    c3_d = e("c3", [128, NC3])
    c4_d = e("c4", [128, NC4])
    cn_d = e("cn", [4096, 4096], BF16)
    sn_d = e("sn", [4096, 4096], BF16)
    win0_d = e("win0", [D, 13408])
    wout0_d = e("wout0", [D, D])
    poolw_d = e("poolw", [4, 256, 256])
    win1_d = e("win1", [D, 10272])
    lrw_d = e("lrw", [D, 64])
    gup_d = e("gup", [64, 3072])
    wout1_d = e("wout1", [D, D])
    fw_d = e("fw", [4, 256, 256])
    rt_d = [e("rt0", [128, NRT]), e("rt1", [128, NRT])]
    w1_d = [e("w1_0", [32, D, 512]), e("w1_1", [32, D, 512])]
    w3_d = [e("w3_0", [32, D, 512]), e("w3_1", [32, D, 512])]
    w2_d = [e("w2_0", [32 * 512, D]), e("w2_1", [32 * 512, D])]
    fnw_d = e("fnw", [128, D])
    out_d = k.ext_out("out", [NLAT, D])
    k.stage_mod(0, cvec_d, modw_d[0], vecs_d[0])
    hT0 = k.scratch("hT0", [NT, 128, KC * 128], BF16)
    k.stage_norm(xin_d, 0, out_bf_d=hT0)
    k.stage_inproj0(hT0, win0_d, v0_d)
    k.stage_conv0()
    k.stage_gdn(c2_d)
    k.stage_mixout0(c3_d, poolw_d)
    x1_d = k.scratch("x1", [NTOK, D], F32)
    k.stage_outproj(k.yT_d, wout0_d, xin_d, x1_d)
    fT0 = k.scratch("fT0", [NT, 128, KC * 128], BF16)
    fT0_32 = k.scratch("fT0_32", [NT, 128, KC * 128], F32)
    k.stage_norm(x1_d, 1, out_bf_d=fT0, out_f32_d=fT0_32)
    k.stage_router(fT0_32, rt_d[0])
    x2_d = k.scratch("x2", [NTOK, D], F32)
    k.stage_moe(fT0, w1_d[0], w3_d[0], w2_d[0], x1_d, x2_d, tag="0")
    LT = range(2, NT)
    k.stage_mod(1, cvec_d, modw_d[1], vecs_d[1])
    hT1 = k.scratch("hT1", [NT, 128, KC * 128], BF16)
    k.stage_norm(x2_d, 0, out_bf_d=hT1)
    k.stage_inproj1(hT1, win1_d, lrw_d, v1_d)
    k.stage_gk(gup_d)
    k.stage_gla(c2_d)
    k.stage_fourier(c4_d, cn_d, sn_d)
    k.stage_mixout1(fw_d)
    x3_d = k.scratch("x3", [NTOK, D], F32)
    k.stage_outproj(k.yT1_d, wout1_d, x2_d, x3_d, tiles=LT)
    fT1 = k.scratch("fT1", [NT, 128, KC * 128], BF16)
    fT1_32 = k.scratch("fT1_32", [NT, 128, KC * 128], F32)
    k.stage_norm(x3_d, 1, out_bf_d=fT1, out_f32_d=fT1_32, tiles=LT)
    k.stage_router(fT1_32, rt_d[1], tiles=LT)
    x4_d = k.scratch("x4", [NTOK, D], F32)
    k.stage_moe(fT1, w1_d[1], w3_d[1], w2_d[1], x3_d, x4_d, tiles=LT, tag="1")
    k.stage_final(x4_d, fnw_d, out_d)
    k.p.build()
    return k


def host_inputs(inp, b, shared):
    m = dict(shared)
    m["xin"] = np.ascontiguousarray(np.concatenate([inp["ctx"][b], inp["x"][b]], 0))
    m["cvec"] = np.ascontiguousarray(np.concatenate([inp["c"][b].reshape(32, 128), inp["c_ctx"].reshape(32, 128)], 0))
    return m


def host_shared(inp):
    cn, sn = make_dft_big()
    s = {
        "consts": make_consts(), "c2": make_consts2(), "c3": make_consts3(), "c4": make_consts4(), "cn": cn, "sn": sn,
        "modw0": inp["mod_w"][0], "modw1": inp["mod_w"][1],
        "vecs0": make_vecs(inp, 0), "vecs1": make_vecs(inp, 1), "v0": make_vecs0(inp), "v1": make_vecs1(inp),
        "win0": inp["ab_w_in"][0], "wout0": inp["ab_w_out"][0], "poolw": inp["pool_w"][0],
        "win1": inp["cd_w_in"][0], "lrw": make_lrw(inp), "gup": make_gup(inp), "wout1": inp["cd_w_out"][0],
        "fw": inp["fourier_w"][0], "rt0": make_router(inp, 0), "rt1": make_router(inp, 1),
        "w1_0": inp["moe_w1"][0], "w3_0": inp["moe_w3"][0], "w2_0": inp["moe_w2"][0].reshape(32 * 512, D),
        "w1_1": inp["moe_w1"][1], "w3_1": inp["moe_w3"][1], "w2_1": inp["moe_w2"][1].reshape(32 * 512, D),
        "fnw": np.ascontiguousarray(np.broadcast_to(inp["final_norm_w"].reshape(1, D), (128, D))),
    }
    return {k_: np.ascontiguousarray(v) for k_, v in s.items()}


N_CORES = 4


def kernel(**inputs):
    inp = {k_: np.asarray(v) for k_, v in inputs.items()}
    k = build_full()
    shared = host_shared(inp)
    in_maps = [host_inputs(inp, b, shared) for b in range(N_CORES)]
    res = run_bass_kernel_spmd(k.nc, in_maps, core_ids=list(range(N_CORES)))
    out = np.stack([np.asarray(res.results[b]["out"]) for b in range(N_CORES)], 0)
    return out.astype(np.float32)
```

```python
import numpy as np
from contextlib import ExitStack
import concourse.bass as bass
import concourse.mybir as mybir
from concourse.bass_utils import run_bass_kernel_spmd

F32 = mybir.dt.float32
BF16 = mybir.dt.bfloat16
I32 = mybir.dt.int32
AF = mybir.ActivationFunctionType
ALU = mybir.AluOpType
AX = mybir.AxisListType

D = 4096
KC = 32
NCTX = 256
NLAT = 4096
NTOK = NCTX + NLAT
NT = NTOK // 128
EPS = 1e-6

EPOCH = 30000
NDMA = 12


PSUM_NAMES = {"ps0", "psm", "pst", "pT", "pg", "pst4", "pl2", "ptm", "bank6", "gbcb", "bank"}


def is_psum_key(k):
    name = k if isinstance(k, str) else k[0]
    return name in PSUM_NAMES


class Prog:
    def __init__(self, nc, es):
        self.nc = nc
        self.es = es
        self.ops = []
        self.nt = 0

    def sb(self, shape, dt=F32, name=None):
        self.nt += 1
        return self.es.enter_context(self.nc.sbuf_tensor(name or f"sb{self.nt}", list(shape), dt))

    def ps(self, shape, dt=F32, name=None):
        self.nt += 1
        return self.es.enter_context(self.nc.psum_tensor(name or f"ps{self.nt}", list(shape), dt))

    def dram(self, name, shape, dt=F32, kind="Internal"):
        return self.nc.dram_tensor(name, list(shape), dt, kind=kind).ap()

    limit = None

    def op(self, eng, fn, r=(), w=()):
        if self.limit is not None and len(self.ops) >= self.limit:
            return
        self.ops.append([eng, fn, tuple(r), tuple(w), False])

    def dma(self, q, out, in_, r=(), w=(), **kw):
        if self.limit is not None and len(self.ops) >= self.limit:
            return
        self.ops.append([q, (lambda e, o=out, i=in_, k=kw: e.dma_start(out=o, in_=i, **k)),
                         tuple(r), tuple(w), True])

    def reg(self, e, v):
        key = (id(e), v)
        if not hasattr(self, "_regc"):
            self._regc = {}
        if key not in self._regc:
            self._regc[key] = e.to_reg(v)
        return self._regc[key]

    def dmaop(self, q, fn, r=(), w=()):
        self.ops.append([q, fn, tuple(r), tuple(w), True])

    def barrier(self):
        self.ops.append(["*", None, (), (), False])

    def build(self):
        nc = self.nc
        ops = self.ops
        engs = ["pe", "act", "dve", "pool", "sp"]
        seq = {e: 0 for e in engs}
        dcount = {e: 0 for e in engs}
        dsem_uses = {}
        last_w = {}
        readers = {}
        plan = []
        waited = {e: {} for e in engs}
        done_tok = []
        latest = {}
        for idx, (eng, fn, r, w, is_dma) in enumerate(ops):
            if eng == "*":
                done_tok.append(None)
                for e in engs:
                    wl = []
                    for sk2, val in latest.items():
                        if waited[e].get(sk2, 0) >= val:
                            continue
                        waited[e][sk2] = val
                        wl.append((sk2, val))
                    plan.append((e, None, wl, None))
                last_w.clear()
                readers.clear()
                continue
            deps = set()
            for k in r:
                if k in last_w:
                    deps.add(last_w[k])
                if is_psum_key(k):
                    for rd in readers.get(k, ()):
                        if ops[rd][0] != eng:
                            deps.add(rd)
            for k in w:
                if k in last_w:
                    deps.add(last_w[k])
                for rd in readers.get(k, ()):
                    deps.add(rd)
            waits = {}
            for d in deps:
                if d == idx:
                    continue
                deng, dis_dma = ops[d][0], ops[d][4]
                if (not dis_dma) and deng == eng and eng == "pe" and not is_dma:
                    continue
                sk, val = done_tok[d]
                waits[sk] = max(waits.get(sk, 0), val)
            if is_dma:
                j = dcount[eng] % NDMA
                dcount[eng] += 1
                sk = ("d", eng, j)
                prev = dsem_uses.get(sk, 0)
                if prev > 0:
                    waits[sk] = max(waits.get(sk, 0), 16 * prev)
                dsem_uses[sk] = prev + 1
                tok = (sk, 16 * (prev + 1))
                inc = (sk, 16)
            else:
                s = seq[eng]
                seq[eng] += 1
                sk = ("c", eng, s // EPOCH)
                tok = (sk, s % EPOCH + 1)
                inc = (sk, 1)
            done_tok.append(tok)
            latest[tok[0]] = max(latest.get(tok[0], 0), tok[1])
            wl = []
            for sk2, val in waits.items():
                if waited[eng].get(sk2, 0) >= val:
                    continue
                waited[eng][sk2] = val
                wl.append((sk2, val))
            plan.append((eng, fn, wl, inc))
            for k in w:
                last_w[k] = idx
                readers[k] = []
            for k in r:
                if k not in w:
                    readers.setdefault(k, []).append(idx)
        sems = {}
        for i, sk in enumerate(sorted(latest.keys(), key=str)):
            sems[sk] = self.es.enter_context(nc.semaphore(f"s{i}"))
        self.nsems = len(sems)
        self.counts = dict(seq)
        self.dcounts = dict(dcount)
        per_eng = {e: [p for p in plan if p[0] == e] for e in engs}
        block = self.es.enter_context(nc.Block())

        def emit(e, name):
            for (_, fn, wl, inc) in per_eng[name]:
                for sk, val in wl:
                    e.wait_ge(sems[sk], val)
                if fn is None:
                    continue
                ins = fn(e)
                ins.then_inc(sems[inc[0]], inc[1])
            for sk, val in latest.items():
                e.wait_ge(sems[sk], val)

        @block.tensor
        def _(e):
            emit(e, "pe")

        @block.scalar
        def _(e):
            emit(e, "act")

        @block.vector
        def _(e):
            emit(e, "dve")

        @block.gpsimd
        def _(e):
            emit(e, "pool")

        @block.sync
        def _(e):
            emit(e, "sp")


class Arena:
    def __init__(self, t, ncols):
        self.t = t
        self.n = ncols
        self.off = 0

    def reset(self, to=0):
        self.off = to

    def f32(self, cols):
        a = self.t[:, self.off:self.off + cols]
        self.off += cols
        assert self.off <= self.n, f"arena overflow {self.off}"
        return a

    def bf16(self, cols):
        c32 = (cols + 1) // 2
        a = self.t[:, self.off:self.off + c32].bitcast(BF16)
        self.off += c32
        assert self.off <= self.n, f"arena overflow {self.off}"
        return a


C_ID = 0
C_ONES = 128
C_EPS = 256
C_IOTA = 257
NCONST = 260


def make_consts():
    c = np.zeros((128, NCONST), np.float32)
    c[:, C_ID:C_ID + 128] = np.eye(128, dtype=np.float32)
    c[:, C_ONES:C_ONES + 128] = 1.0
    c[:, C_EPS] = EPS
    c[:, C_IOTA] = np.arange(128)
    return c


V_MODB = 0
V_N1 = 192
V_N2 = 224
NVEC = 256


def make_vecs(inp, layer):
    v = np.zeros((128, NVEC), np.float32)
    v[:, V_MODB:V_MODB + 192] = inp["mod_b"][layer].reshape(192, 128).T
    v[:, V_N1:V_N1 + 32] = inp["norm1_w"][layer].reshape(32, 128).T
    v[:, V_N2:V_N2 + 32] = inp["norm2_w"][layer].reshape(32, 128).T
    return v


class K:
    def allgather(self, name, shard_d, rows, cols, dt=F32, nsplit=1):
        p = self.p
        rs = rows // 8
        bounce = self.nc.dram_tensor(name + "_bn", [rs, cols], dt, kind="Internal").ap()
        full = self.nc.dram_tensor(name + "_ag", [rows, cols], dt, kind="Internal").ap()
        p.dma("pool", bounce, shard_d, w=[name + "_bn"])
        p.dmaop("pool", lambda e: e.collective_compute("AllGather", op=ALU.bypass, replica_groups=[list(range(8))],
                                                       ins=[bounce], outs=[full]), r=[name + "_bn"], w=[name + "_ag"])
        return full

    def __init__(self, dbg=None):
        self.dbg = dbg or {}
        self.nc = bass.Bass("TRN2", target_bir_lowering=False, num_devices=8)
        self.es = ExitStack()
        self.p = Prog(self.nc, self.es)
        self.outs = {}

    def ext_in(self, name, shape, dt=F32):
        return self.nc.dram_tensor(name, list(shape), dt, kind="ExternalInput").ap()

    def ext_out(self, name, shape, dt=F32):
        return self.nc.dram_tensor(name, list(shape), dt, kind="ExternalOutput").ap()

    def scratch(self, name, shape, dt=F32):
        kind = "ExternalOutput" if name in self.dbg else "Internal"
        if name in getattr(self, "dbg_in", ()):
            kind = "ExternalInput"
        return self.nc.dram_tensor(name, list(shape), dt, kind=kind).ap()

    def setup(self):
        p = self.p
        self.arena_t = p.sb([128, 44 * 1024], F32, "arena")
        self.A = Arena(self.arena_t, 44 * 1024)
        self.pers = p.sb([128, 2048], F32, "pers")
        self.PA = Arena(self.pers, 2048)
        self.PA_big = p.sb([128, 2048], F32, "persbig")[:, :]
        self.gate_sb = p.sb([128, NT * 32], F32, "gate_sb")[:, :]
        self.PA_v1 = self.PA_big
        self.psb = [p.ps([128, 512], F32, f"psb{i}")[:, :] for i in range(8)]
        self.consts_d = self.ext_in("consts", [128, NCONST])
        self.consts = self.PA.f32(NCONST)
        p.dma("sp", self.consts, self.consts_d, w=["consts"])
        self.ident = self.consts[:, C_ID:C_ID + 128]
        self.ones = self.consts[:, C_ONES:C_ONES + 128]
        self.epsc = self.consts[:, C_EPS:C_EPS + 1]

    def stage_mod(self, layer, cvec_d, modw_d, vecs_d):
        p, A = self.p, self.A
        A.reset()
        L = layer
        vecs = self.PA.f32(NVEC)
        self.vecs = vecs
        p.dma("sp", vecs, vecs_d, w=["vecs"])
        cv = A.f32(128)
        sT = A.f32(64)
        p.dma("sp", cv[0:64, :], cvec_d, w=["cv"])
        ps0 = self.psb[0]
        p.op("pe", lambda e: e.transpose(ps0[:, 0:64], cv[0:64, :], self.ident[0:64, 0:64]),
             r=["cv", "consts"], w=["ps0"])
        p.op("act", lambda e: e.activation(out=sT, in_=ps0[:, 0:64], func=AF.Silu), r=["ps0"], w=["sT"])
        sT3 = sT.rearrange("p (n k) -> p n k", n=2)
        psm = self.psb[1]
        NBUF = 2
        wbuf = [A.f32(32 * 512) for _ in range(NBUF)]
        for jb in range(48):
            wb = wbuf[jb % NBUF]
            wb3 = wb.rearrange("p (k c) -> p k c", k=32)
            src = modw_d[:, jb * 512:(jb + 1) * 512].rearrange("(k p) c -> p k c", p=128)
            for q in range(4):
                p.dma(["sp", "act", "pool", "sp"][q], wb3[:, q * 8:(q + 1) * 8, :], src[:, q * 8:(q + 1) * 8, :],
                      w=[("mw", jb % NBUF, q)])
            for jj in range(4):
                j = jb * 4 + jj
                for kc in range(32):
                    p.op("pe", lambda e, j=j, kc=kc, jj=jj, wb3=wb3: e.matmul(
                        psm[:, 2 * j:2 * j + 2], lhsT=wb3[:, kc, jj * 128:(jj + 1) * 128], rhs=sT3[:, :, kc],
                        start=(kc == 0), stop=(kc == 31)),
                        r=[("mw", jb % NBUF, kc // 8), "sT"], w=["psm"])
        modL = self.PA.f32(192)
        modC = self.PA.f32(192)
        psm3 = psm[:, 0:384].rearrange("p (j n) -> p j n", n=2)
        p.op("dve", lambda e: e.tensor_tensor(out=modL, in0=psm3[:, :, 0], in1=vecs[:, V_MODB:V_MODB + 192], op=ALU.add),
             r=["psm", "vecs"], w=["modL"])
        p.op("dve", lambda e: e.tensor_tensor(out=modC, in0=psm3[:, :, 1], in1=vecs[:, V_MODB:V_MODB + 192], op=ALU.add),
             r=["psm", "vecs"], w=["modC"])
        self.mod = {"L": modL, "C": modC}
        self.Asc = {}
        for side, m in (("L", modL), ("C", modC)):
            a1 = self.PA.f32(32)
            a2 = self.PA.f32(32)
            p.op("dve", lambda e, a1=a1, m=m: e.scalar_tensor_tensor(
                out=a1, in0=m[:, 32:64], scalar=1.0, in1=vecs[:, V_N1:V_N1 + 32], op0=ALU.add, op1=ALU.mult),
                r=["mod" + side, "vecs"], w=["A1" + side])
            p.op("dve", lambda e, a2=a2, m=m: e.scalar_tensor_tensor(
                out=a2, in0=m[:, 128:160], scalar=1.0, in1=vecs[:, V_N2:V_N2 + 32], op0=ALU.add, op1=ALU.mult),
                r=["mod" + side, "vecs"], w=["A2" + side])
            self.Asc[side] = (a1, a2)
        gbc_d = self.scratch(f"gbc{L}", [8, 128, D])
        self.gbc_d = gbc_d
        diag = [A.f32(128) for _ in range(2)]
        row = [A.f32(512) for _ in range(2)]
        cnt = 0
        srcs = [(self.mod["L"], 64, "modL"), (self.mod["C"], 64, "modC"), (self.mod["L"], 160, "modL"), (self.mod["C"], 160, "modC"),
                (self.Asc["L"][1], 0, "A2L"), (self.Asc["C"][1], 0, "A2C"), (self.mod["L"], 96, "modL"), (self.mod["C"], 96, "modC")]
        for gi, (m, base, mkey) in enumerate(srcs):
            for cb in range(8):
                pst = self.psb[2 + (cnt % 2)]
                rw = row[cnt % 2]
                for c4 in range(4):
                    kc = cb * 4 + c4
                    dg = diag[(cnt * 4 + c4) % 2]
                    dk = ("diag", (cnt * 4 + c4) % 2)
                    p.op("dve", lambda e, dg=dg, m=m, base=base, kc=kc: e.tensor_scalar(
                        out=dg, in0=self.ident, scalar1=m[:, base + kc:base + kc + 1], scalar2=None, op0=ALU.mult),
                        r=[mkey, "consts"], w=[dk])
                    p.op("pe", lambda e, dg=dg, pst=pst, c4=c4: e.matmul(
                        pst[:, c4 * 128:(c4 + 1) * 128], lhsT=self.ones, rhs=dg, start=True, stop=True),
                        r=[dk, "consts"], w=[("pst", cnt % 2)])
                p.op("act", lambda e, rw=rw, pst=pst: e.copy(out=rw, in_=pst), r=[("pst", cnt % 2)], w=[("row", cnt % 2)])
                p.dma("sp", gbc_d[gi, :, cb * 512:(cb + 1) * 512], rw, r=[("row", cnt % 2)], w=["gbc"])
                cnt += 1
        p.barrier()

    def stage_norm(self, x_d, which, out_bf_d=None, out_f32_d=None, tiles=range(NT), out_tm_d=None):
        p, A = self.p, self.A
        A.reset()
        NB = 2
        xt = [A.f32(D) for _ in range(NB)]
        junk = A.f32(D)
        hb = [A.bf16(D) for _ in range(NB)] if out_bf_d is not None else None
        hf = [A.f32(D) for _ in range(NB)] if out_f32_d is not None else None
        ss = [A.f32(1) for _ in range(NB)]
        rs = [A.f32(1) for _ in range(NB)]
        if out_tm_d is not None:
            rows = {}
            for ri, (side_, kind_) in enumerate((("L", "A"), ("C", "A"), ("L", "B"), ("C", "B"))):
                rows[(side_, kind_)] = A.f32(D)
                p.dma("sp", rows[(side_, kind_)], self.gbc_d[4 + ri], w=[("nrow", ri)])
            ftm = [A.bf16(D) for _ in range(NB)]
            ftmp = junk
        for it, t in enumerate(tiles):
            b = it % NB
            side = "C" if t < 2 else "L"
            Av = self.Asc[side][which]
            sh0 = 0 if which == 0 else 96
            Bv = self.mod[side]
            p.dma("sp" if it % 2 == 0 else "pool", xt[b], x_d[t * 128:(t + 1) * 128, :], w=[("xt", b)])
            p.op("act", lambda e, b=b: e.activation(out=junk, in_=xt[b], func=AF.Square, accum_out=ss[b]),
                 r=[("xt", b)], w=["junk", ("ss", b)])
            p.op("act", lambda e, b=b: e.activation(out=rs[b], in_=ss[b], func=AF.Sqrt, bias=self.epsc, scale=1.0 / D),
                 r=[("ss", b), "consts"], w=[("rs", b)])
            p.op("dve", lambda e, b=b: e.reciprocal(out=rs[b], in_=rs[b]), r=[("rs", b)], w=[("rs", b)])
            p.op("dve", lambda e, b=b: e.tensor_scalar(out=xt[b], in0=xt[b], scalar1=rs[b][:, 0:1], scalar2=None,
                                                      op0=ALU.mult), r=[("rs", b), ("xt", b)], w=[("xt", b)])
            if out_tm_d is not None:
                ra, rb_ = rows[(side, "A")], rows[(side, "B")]
                p.op("pool", lambda e, b=b, ra=ra: e.tensor_tensor(out=ftmp, in0=xt[b], in1=ra, op=ALU.mult),
                     r=[("xt", b)] + [("nrow", i_) for i_ in range(4)], w=["junk"])
                p.op("pool", lambda e, b=b, rb_=rb_: e.tensor_tensor(out=ftm[b], in0=ftmp, in1=rb_, op=ALU.add),
                     r=["junk"] + [("nrow", i_) for i_ in range(4)], w=[("ftm", b)])
                p.dma("pool", out_tm_d[t * 128:(t + 1) * 128, :], ftm[b], r=[("ftm", b)], w=["ftmd"])
            for kc in range(KC):
                pi = kc % 4
                pst = self.psb[4 + pi]
                p.op("pe", lambda e, b=b, kc=kc, pst=pst: e.transpose(pst[:, 0:128], xt[b][:, kc * 128:(kc + 1) * 128],
                                                                     self.ident),
                     r=[("xt", b), "consts"], w=[("pT", pi)])
                dst = hf[b] if hf is not None else hb[b]
                wk = ("hf", b, kc) if hf is not None else ("hb", b, kc)
                p.op("act", lambda e, dst=dst, kc=kc, pst=pst, Av=Av, Bv=Bv, sh0=sh0: e.activation(
                    out=dst[:, kc * 128:(kc + 1) * 128], in_=pst[:, 0:128], func=AF.Identity,
                    scale=Av[:, kc:kc + 1], bias=Bv[:, sh0 + kc:sh0 + kc + 1]),
                    r=[("pT", pi), "mod" + side, "A1" + side, "A2" + side], w=[wk])
                if hf is not None and hb is not None:
                    p.op("pool", lambda e, b=b, kc=kc: e.tensor_copy(out=hb[b][:, kc * 128:(kc + 1) * 128],
                                                                    in_=hf[b][:, kc * 128:(kc + 1) * 128]),
                         r=[("hf", b, kc)], w=[("hb", b, kc)])
            if out_bf_d is not None:
                p.dma("act", out_bf_d[t], hb[b], r=[("hb", b, kc) for kc in range(KC)], w=["n_out_bf"])
            if out_f32_d is not None:
                p.dma("act", out_f32_d[t], hf[b], r=[("hf", b, kc) for kc in range(KC)], w=["n_out_f32"])
        p.barrier()


def build(layer_inputs=None, dbg=None):
    k = K(dbg)
    k.setup()
    return k


def _gemm_tm(self, at_d, kc_n, blocks, epilogue, tiles, arena_keep=None, at_bufs=3, wsets=2, at_c0=0):
    p, A = self.p, self.A
    if arena_keep is None:
        A.reset()
    else:
        A.reset(arena_keep)
    nsrc = max(len(b["srcs"]) for b in blocks)
    maxw = max(b["nbw"] for b in blocks)
    SG = 8
    stg = [A.f32(SG * maxw) for _ in range(3)]
    wb = [[A.bf16(kc_n * maxw) for _ in range(nsrc)] for _ in range(wsets)]
    at = [A.bf16(kc_n * 128) for _ in range(at_bufs)]
    self.ep_base = A.off
    sgc = 0
    atc = 0
    pgc = 0
    cast_engs = ["pool", "dve", "act"]
    for bi, blk in enumerate(blocks):
        nbw = blk["nbw"]
        ws = bi % wsets
        for si, src in enumerate(blk["srcs"]):
            src3 = src.rearrange("(k p) c -> p k c", p=128)
            wb3 = wb[ws][si][:, 0:kc_n * nbw].rearrange("p (k c) -> p k c", k=kc_n)
            for g in range(kc_n // SG):
                sb_ = sgc % 3
                st3 = stg[sb_][:, 0:SG * nbw].rearrange("p (k c) -> p k c", k=SG)
                p.dma("sp", st3, src3[:, g * SG:(g + 1) * SG, :], w=[("stg", sb_)])
                ce = cast_engs[sgc % 3]
                if ce == "act":
                    p.op("act", lambda e, st3=st3, wb3=wb3, g=g: e.copy(out=wb3[:, g * SG:(g + 1) * SG, :], in_=st3),
                         r=[("stg", sb_)], w=[("wb", ws, si, g)])
                else:
                    p.op(ce, lambda e, st3=st3, wb3=wb3, g=g: e.tensor_copy(out=wb3[:, g * SG:(g + 1) * SG, :], in_=st3),
                         r=[("stg", sb_)], w=[("wb", ws, si, g)])
                sgc += 1
        for t in tiles:
            ab = atc % at_bufs
            atc += 1
            p.dma("sp" if atc % 2 else "pool", at[ab], at_d[t][:, at_c0 * 128:(at_c0 + kc_n) * 128], w=[("at", ab)])
            at3 = at[ab].rearrange("p (k c) -> p k c", k=kc_n)
            pset = pgc % 2
            pgc += 1
            ps_list, pkeys = [], []
            for si in range(len(blk["srcs"])):
                ps = self.psb[pset * 2 + si]
                wb3 = wb[ws][si][:, 0:kc_n * nbw].rearrange("p (k c) -> p k c", k=kc_n)
                pk = ("pg", pset, si)
                for kc in range(kc_n):
                    p.op("pe", lambda e, ps=ps, at3=at3, wb3=wb3, kc=kc, nbw=nbw: e.matmul(
                        ps[:, 0:nbw], lhsT=at3[:, kc, :], rhs=wb3[:, kc, :], start=(kc == 0), stop=(kc == kc_n - 1)),
                        r=[("at", ab), ("wb", ws, si, kc // SG)], w=[pk])
                ps_list.append(ps)
                pkeys.append(pk)
            epilogue(t, bi, blk, ps_list, pkeys)
    p.barrier()


K.gemm_tm = _gemm_tm


GH = 24
V0_CW = 0
V0_ALOG = 360
V0_DTB = 408
V0_GNW = 456
V0_PSC = 584
NV0 = 1608


def make_vecs0(inp):
    v = np.zeros((128, NV0), np.float32)
    cw = inp["gdn_conv_w"][0]
    v[:, V0_CW:V0_CW + 360] = cw.reshape(5, 72, 128).transpose(2, 1, 0).reshape(128, 360)
    v[:, V0_ALOG:V0_ALOG + 48] = inp["gdn_a_log"][0].reshape(1, 48)
    v[:, V0_DTB:V0_DTB + 48] = inp["gdn_dt_bias"][0].reshape(1, 48)
    v[:, V0_GNW:V0_GNW + 128] = inp["gdn_norm_w"][0].reshape(1, 128)
    v[:, V0_PSC:V0_PSC + 1024] = inp["pool_scale"][0].reshape(1, 1024)
    return v


def _stage_inproj0(self, hT_d, w_d, v0_d):
    p, A = self.p, self.A
    self.v0 = self.PA_big
    p.dma("sp", self.v0[:, 0:NV0], v0_d, w=["v0"])
    self.poolu_d = self.scratch("poolu", [NTOK, 1024], F32)
    self.raw_d = self.scratch("rawqkv", [72, 128, NTOK], F32)
    self.z_d = self.scratch("zs", [NTOK, 3072], F32)
    self.beta_d = self.scratch("beta", [NTOK, 48], F32)
    self.g_d = self.scratch("gg", [NTOK, 48], F32)
    v0 = self.v0

    def mk_ep_simple(kind):
        def ep(t, bi, blk, ps_list, pkeys):
            A2 = Arena(self.arena_t, 44 * 1024)
            A2.reset(self.ep_base)
            bufs = [A2.f32(512) for _ in range(2)]
            i = ep.cnt % 2
            ep.cnt += 1
            ob = bufs[i]
            if kind == "pool":
                p.op("act", lambda e: e.copy(out=ob, in_=ps_list[0]), r=[pkeys[0]], w=[("epo", i)])
                p.dma("act", self.poolu_d[t * 128:(t + 1) * 128, bi * 512:(bi + 1) * 512], ob, r=[("epo", i)], w=["poolu"])
            else:
                p.op("act", lambda e: e.activation(out=ob, in_=ps_list[0], func=AF.Silu), r=[pkeys[0]], w=[("epo", i)])
                p.dma("act", self.z_d[t * 128:(t + 1) * 128, bi * 512:(bi + 1) * 512], ob, r=[("epo", i)], w=["zs"])
        ep.cnt = 0
        return ep

    blocks = [dict(srcs=[w_d[:, c:c + 512]], nbw=512) for c in range(0, 1024, 512)]
    self.gemm_tm(hT_d, KC, blocks, mk_ep_simple("pool"), range(NT))
    blocks = [dict(srcs=[w_d[:, 10240 + c:10240 + c + 512]], nbw=512) for c in range(0, 3072, 512)]
    self.gemm_tm(hT_d, KC, blocks, mk_ep_simple("z"), range(NT))

    def ep_gate(t, bi, blk, ps_list, pkeys):
        A2 = Arena(self.arena_t, 44 * 1024)
        A2.reset(self.ep_base)
        bb = [A2.f32(48) for _ in range(2)]
        gb = [A2.f32(48) for _ in range(2)]
        ea = A2.f32(48)
        i = ep_gate.cnt % 2
        if ep_gate.cnt == 0:
            p.op("act", lambda e: e.activation(out=ea, in_=v0[:, V0_ALOG:V0_ALOG + 48], func=AF.Exp), r=["v0"], w=["ea"])
        ep_gate.cnt += 1
        ps = ps_list[0]
        p.op("act", lambda e: e.activation(out=bb[i], in_=ps[:, 0:48], func=AF.Sigmoid), r=[pkeys[0]], w=[("bb", i)])
        p.op("dve", lambda e: e.tensor_tensor(out=gb[i], in0=ps[:, 48:96], in1=v0[:, V0_DTB:V0_DTB + 48], op=ALU.add),
             r=[pkeys[0], "v0"], w=[("gb", i)])
        p.op("act", lambda e: e.activation(out=gb[i], in_=gb[i], func=AF.Exp), r=[("gb", i)], w=[("gb", i)])
        p.op("act", lambda e: e.activation(out=gb[i], in_=gb[i], func=AF.Ln, bias=self.ones[:, 0:1]), r=[("gb", i), "consts"],
             w=[("gb", i)])
        p.op("dve", lambda e: e.scalar_tensor_tensor(out=gb[i], in0=gb[i], scalar=-1.0, in1=ea, op0=ALU.mult, op1=ALU.mult),
             r=[("gb", i), "ea"], w=[("gb", i)])
        p.dma("act", self.beta_d[t * 128:(t + 1) * 128, :], bb[i], r=[("bb", i)], w=["betad"])
        p.dma("act", self.g_d[t * 128:(t + 1) * 128, :], gb[i], r=[("gb", i)], w=["gd"])
    ep_gate.cnt = 0
    self.gemm_tm(hT_d, KC, [dict(srcs=[w_d[:, 13312:13408]], nbw=96)], ep_gate, range(NT))

    def ep_qkv(t, bi, blk, ps_list, pkeys):
        A2 = Arena(self.arena_t, 44 * 1024)
        A2.reset(self.ep_base)
        tm = [A2.f32(512) for _ in range(2)]
        fm = [A2.f32(512) for _ in range(2)]
        i = ep_qkv.cnt % 2
        ep_qkv.cnt += 1
        p.op("act", lambda e: e.copy(out=tm[i], in_=ps_list[0]), r=[pkeys[0]], w=[("tm", i)])
        pst = self.psb[4 + i]
        for j in range(4):
            p.op("pe", lambda e, j=j: e.transpose(pst[:, j * 128:(j + 1) * 128], tm[i][:, j * 128:(j + 1) * 128], self.ident),
                 r=[("tm", i), "consts"], w=[("pst4", i)])
        p.op("dve", lambda e: e.tensor_copy(out=fm[i], in_=pst), r=[("pst4", i)], w=[("fm", i)])
        p.dma("act", self.raw_d[bi * 4:(bi + 1) * 4, :, t * 128:(t + 1) * 128].rearrange("j p c -> p j c"),
              fm[i].rearrange("p (j c) -> p j c", j=4), r=[("fm", i)], w=["rawd"])
    ep_qkv.cnt = 0
    blocks = [dict(srcs=[w_d[:, 1024 + c:1024 + c + 512]], nbw=512) for c in range(0, 9216, 512)]
    self.gemm_tm(hT_d, KC, blocks, ep_qkv, range(NT))


K.stage_inproj0 = _stage_inproj0


def _stage_conv0(self):
    p, A = self.p, self.A
    A.reset()
    v0 = self.v0
    self.qT_d = self.scratch("qT", [GH, 128, NTOK], F32)
    self.kT_d = self.scratch("kT", [GH, 128, NTOK], F32)
    self.ktm_d = self.scratch("ktm", [NT, GH, 128, 128], F32)
    self.vtm_d = self.scratch("vtm", [NT, GH, 128, 128], F32)
    W = NTOK + 8
    buf = [A.f32(W) for _ in range(2)]
    acc = [A.f32(W) for _ in range(2)]
    sq = A.f32(512)
    rst = A.f32(512)
    tmo = [A.f32(512) for _ in range(2)]
    for b in range(2):
        p.op("pool", lambda e, b=b: e.memset(buf[b], 0.0), w=[("cb", b)])
    NV = NTOK + 4

    def seg(ap, ofs=0):
        return ap

    tcnt = 0
    for ci in range(72):
        b = ci % 2
        kind = ci // 24
        h = ci % 24
        p.dma("sp", buf[b][:, 2:2 + NCTX], self.raw_d[ci][:, 0:NCTX], w=[("cb", b)])
        p.dma("pool", buf[b][:, 6 + NCTX:6 + NTOK], self.raw_d[ci][:, NCTX:NTOK], w=[("cb", b)])
        ve = "dve"
        cw = v0[:, V0_CW + ci * 5:V0_CW + ci * 5 + 5]
        a = acc[b]
        p.op(ve, lambda e, a=a, b=b, cw=cw: e.tensor_scalar(out=a[:, 2:2 + NV], in0=buf[b][:, 0:NV], scalar1=cw[:, 0:1],
                                                           scalar2=None, op0=ALU.mult), r=[("cb", b), "v0"], w=[("acc", b)])
        for j in range(1, 5):
            p.op(ve, lambda e, a=a, b=b, cw=cw, j=j: e.scalar_tensor_tensor(
                out=a[:, 2:2 + NV], in0=buf[b][:, j:j + NV], scalar=cw[:, j:j + 1], in1=a[:, 2:2 + NV],
                op0=ALU.mult, op1=ALU.add), r=[("cb", b), "v0", ("acc", b)], w=[("acc", b)])
        p.op("act", lambda e, a=a: e.activation(out=a[:, 2:2 + NV], in_=a[:, 2:2 + NV], func=AF.Silu),
             r=[("acc", b)], w=[("acc", b)])
        if kind < 2:
            for c0 in range(2, 2 + NV, 512):
                cn = min(512, 2 + NV - c0)
                ps = self.psb[(c0 // 512) % 2]
                pk = ("pl2", (c0 // 512) % 2)
                p.op("act", lambda e, a=a, c0=c0, cn=cn: e.activation(out=sq[:, 0:cn], in_=a[:, c0:c0 + cn], func=AF.Square),
                     r=[("acc", b)], w=["sq"])
                p.op("pe", lambda e, ps=ps, cn=cn: e.matmul(ps[:, 0:cn], lhsT=self.ones, rhs=sq[:, 0:cn], start=True, stop=True),
                     r=["sq", "consts"], w=[pk])
                p.op("act", lambda e, ps=ps, cn=cn: e.activation(out=rst[:, 0:cn], in_=ps[:, 0:cn], func=AF.Sqrt, bias=self.epsc),
                     r=[pk, "consts"], w=["rst"])
                p.op("dve", lambda e, cn=cn: e.reciprocal(out=rst[:, 0:cn], in_=rst[:, 0:cn]), r=["rst"], w=["rst"])
                sc = (128.0 ** -0.5) if kind == 0 else 1.0
                p.op("dve", lambda e, a=a, c0=c0, cn=cn, sc=sc: e.scalar_tensor_tensor(
                    out=a[:, c0:c0 + cn], in0=a[:, c0:c0 + cn], scalar=sc, in1=rst[:, 0:cn], op0=ALU.mult, op1=ALU.mult),
                    r=["rst", ("acc", b)], w=[("acc", b)])
            dst = self.qT_d if kind == 0 else self.kT_d
            p.dma("act", dst[h][:, 0:NCTX], a[:, 2:2 + NCTX], r=[("acc", b)], w=["qkT"])
            p.dma("act", dst[h][:, NCTX:NTOK], a[:, 6 + NCTX:6 + NTOK], r=[("acc", b)], w=["qkT"])
        if kind >= 1:
            dst = self.ktm_d if kind == 1 else self.vtm_d
            for t in range(NT):
                o0 = (2 + t * 128) if t < 2 else (6 + t * 128)
                i = tcnt % 2
                tcnt += 1
                ps = self.psb[4 + i]
                p.op("pe", lambda e, a=a, o0=o0, ps=ps: e.transpose(ps[:, 0:128], a[:, o0:o0 + 128], self.ident),
                     r=[("acc", b), "consts"], w=[("ptm", i)])
                p.op("act" if t % 2 else "dve", (lambda e, ps=ps, i=i: e.copy(out=tmo[i][:, 0:128], in_=ps[:, 0:128])) if t % 2 else
                     (lambda e, ps=ps, i=i: e.tensor_copy(out=tmo[i][:, 0:128], in_=ps[:, 0:128])),
                     r=[("ptm", i)], w=[("tmo", i)])
                p.dma("sp", dst[t, h], tmo[i][:, 0:128], r=[("tmo", i)], w=["kvtm"])
    p.barrier()


K.stage_conv0 = _stage_conv0


C2_CUMF = 0
C2_CUMB = 128
C2_SEL = 256
C2_MFI = 1280
C2_MFS = 2304
C2_MBI = 3328
C2_MBS = 4352
NC2 = 5376


def make_consts2():
    c = np.zeros((128, NC2), np.float32)
    i = np.arange(128)
    c[:, C2_CUMF:C2_CUMF + 128] = (i[:, None] <= i[None, :])
    c[:, C2_CUMB:C2_CUMB + 128] = (i[:, None] >= i[None, :])
    for h in range(8):
        c[h, C2_SEL + h * 128:C2_SEL + (h + 1) * 128] = 1.0
    cc, ss = i[:, None], i[None, :]
    for off, m in ((C2_MFI, ss <= cc), (C2_MFS, ss < cc), (C2_MBI, ss >= cc), (C2_MBS, ss > cc)):
        c[:, off:off + 1024] = np.tile(m.astype(np.float32), (1, 8))
    return c


def _stage_gdn(self, c2_d, nsteps=NT):
    p, A = self.p, self.A
    A.reset()
    HG = 8
    self.o_d = self.scratch("ogdn", [2, NTOK, GH * 128], F32)
    c2 = A.f32(NC2)
    p.dma("sp", c2, c2_d, w=["c2"])
    S = A.f32(2 * GH * 128)
    p.op("pool", lambda e: e.memset(S, 0.0), w=[("S", d, h) for d in range(2) for h in range(GH)])
    NBUF = 2
    qTb = [A.f32(HG * 128) for _ in range(NBUF)]
    kTb = [A.f32(HG * 128) for _ in range(NBUF)]
    kb = [A.f32(HG * 128) for _ in range(NBUF)]
    vb = [A.f32(HG * 128) for _ in range(NBUF)]
    gt = [A.f32(24) for _ in range(2)]
    bt = [A.f32(24) for _ in range(2)]
    Gs = [A.f32(24) for _ in range(2)]
    eG = [A.f32(24) for _ in range(2)]
    eEnd = [A.f32(24) for _ in range(2)]
    ge = [A.f32(24) for _ in range(2)]
    bw = [A.f32(24) for _ in range(2)]
    negb = [A.f32(24) for _ in range(2)]
    GT8 = [A.f32(128) for _ in range(2)]
    xm = [A.f32(HG * 128) for _ in range(2)]
    eGbc = [A.f32(HG * 128) for _ in range(2)]
    Di = [A.f32(HG * 128) for _ in range(2)]
    Ds = [A.f32(HG * 128) for _ in range(2)]
    osb = [A.f32(HG * 128) for _ in range(2)]
    NH = 4
    HGI = 4
    Pb = [[A.f32(128) for _ in range(2)] for _ in range(NH)]
    Qb = [[A.f32(128) for _ in range(2)] for _ in range(NH)]
    Rb = [[A.f32(128) for _ in range(2)] for _ in range(NH)]
    attn = [A.f32(128) for _ in range(NH)]
    attnT = [A.f32(128) for _ in range(NH)]
    Vb = [A.f32(128) for _ in range(NH)]
    Kbg = [A.f32(128) for _ in range(NH)]
    kend = [A.f32(128) for _ in range(NH)]
    usb = [A.f32(128) for _ in range(NH)]
    wT = [A.f32(128) for _ in range(NH)]
    qdT = [A.f32(128) for _ in range(NH)]
    vn = [A.f32(128) for _ in range(NH)]

    slot_ctr = [0]
    BANKS = [2, 3, 4, 5, 7]

    def nbank():
        i = BANKS[slot_ctr[0] % len(BANKS)]
        slot_ctr[0] += 1
        return self.psb[i], ("bank", i)

    cp_ctr = [0]

    def evac(dst, src, r, w):
        i = cp_ctr[0] % 2
        cp_ctr[0] += 1
        if i == 0:
            p.op("act", lambda e: e.copy(out=dst, in_=src), r=r, w=w)
        else:
            p.op("dve", lambda e: e.tensor_copy(out=dst, in_=src), r=r, w=w)

    order = {0: list(range(NT)), 1: [1, 0] + list(range(NT - 1, 1, -1))}
    uc = 0
    gc = 0
    hc = 0
    for step in range(nsteps):
        for d in range(2):
            t = order[d][step]
            ub = uc % 2
            uc += 1
            cum = c2[:, C2_CUMF:C2_CUMF + 128] if d == 0 else c2[:, C2_CUMB:C2_CUMB + 128]
            mI = c2[:, C2_MFI:C2_MFI + 1024] if d == 0 else c2[:, C2_MBI:C2_MBI + 1024]
            mS = c2[:, C2_MFS:C2_MFS + 1024] if d == 0 else c2[:, C2_MBS:C2_MBS + 1024]
            p.dma("sp", gt[ub], self.g_d[t * 128:(t + 1) * 128, d * 24:(d + 1) * 24], w=[("gt", ub)])
            p.dma("sp", bt[ub], self.beta_d[t * 128:(t + 1) * 128, d * 24:(d + 1) * 24], w=[("bt", ub)])
            ps6 = self.psb[6]
            p.op("pe", lambda e, cum=cum, ub=ub: e.matmul(ps6[:, 0:24], lhsT=cum, rhs=gt[ub], start=True, stop=True),
                 r=["c2", ("gt", ub)], w=["bank6"])
            p.op("pe", lambda e, ub=ub: e.matmul(ps6[:, 32:56], lhsT=self.ones, rhs=gt[ub], start=True, stop=True),
                 r=["consts", ("gt", ub)], w=["bank6"])
            p.op("dve", lambda e, ub=ub: e.tensor_copy(out=Gs[ub], in_=ps6[:, 0:24]), r=["bank6"], w=[("Gs", ub)])
            p.op("act", lambda e, ub=ub: e.activation(out=eG[ub], in_=ps6[:, 0:24], func=AF.Exp), r=["bank6"], w=[("eG", ub)])
            p.op("act", lambda e, ub=ub: e.activation(out=ge[ub], in_=ps6[:, 32:56], func=AF.Exp), r=["bank6"], w=[("ge", ub)])
            p.op("dve", lambda e, ub=ub: e.tensor_tensor(out=eEnd[ub], in0=ps6[:, 32:56], in1=Gs[ub], op=ALU.subtract),
                 r=["bank6", ("Gs", ub)], w=[("eEnd", ub)])
            p.op("act", lambda e, ub=ub: e.activation(out=eEnd[ub], in_=eEnd[ub], func=AF.Exp), r=[("eEnd", ub)], w=[("eEnd", ub)])
            p.op("pool", lambda e, ub=ub: e.tensor_tensor(out=bw[ub], in0=bt[ub], in1=eG[ub], op=ALU.mult),
                 r=[("bt", ub), ("eG", ub)], w=[("bw", ub)])
            p.op("pool", lambda e, ub=ub: e.tensor_scalar(out=negb[ub], in0=bt[ub], scalar1=-1.0, scalar2=None, op0=ALU.mult),
                 r=[("bt", ub)], w=[("negb", ub)])
            for grp in range(GH // HG):
                h0 = grp * HG
                gb = gc % 2
                gc += 1
                tsl = slice(t * 128, (t + 1) * 128)
                p.dma("sp", qTb[gb].rearrange("p (h c) -> p h c", h=HG),
                      self.qT_d[h0:h0 + HG, :, tsl].rearrange("h p c -> p h c"), w=[("qTb", gb)])
                p.dma("pool", kTb[gb].rearrange("p (h c) -> p h c", h=HG),
                      self.kT_d[h0:h0 + HG, :, tsl].rearrange("h p c -> p h c"), w=[("kTb", gb)])
                p.dma("sp", kb[gb].rearrange("p (h c) -> p h c", h=HG),
                      self.ktm_d[t, h0:h0 + HG].rearrange("h p c -> p h c"), w=[("kb", gb)])
                p.dma("pool", vb[gb].rearrange("p (h c) -> p h c", h=HG),
                      self.vtm_d[t, h0:h0 + HG].rearrange("h p c -> p h c"), w=[("vb", gb)])
                p.op("pe", lambda e, ub=ub, h0=h0: e.transpose(ps6[0:HG, 64:192], Gs[ub][:, h0:h0 + HG], self.ident),
                     r=[("Gs", ub), "consts"], w=["bank6"])
                p.op("act", lambda e, gb=gb: e.copy(out=GT8[gb][0:HG, :], in_=ps6[0:HG, 64:192]), r=["bank6"], w=[("GT8", gb)])
                for j in range(HG):
                    bank = self.psb[j // 4]
                    p.op("pe", lambda e, j=j, bank=bank, gb=gb: e.matmul(
                        bank[:, (j % 4) * 128:(j % 4 + 1) * 128], lhsT=c2[0:HG, C2_SEL + j * 128:C2_SEL + (j + 1) * 128],
                        rhs=GT8[gb][0:HG, :], start=True, stop=True), r=["c2", ("GT8", gb)], w=[("gbcb", j // 4)])
                for j in range(HG):
                    bank = self.psb[j // 4]
                    h = h0 + j
                    p.op("dve", lambda e, j=j, bank=bank, gb=gb, ub=ub, h=h: e.tensor_scalar(
                        out=xm[gb][:, j * 128:(j + 1) * 128], in0=bank[:, (j % 4) * 128:(j % 4 + 1) * 128],
                        scalar1=Gs[ub][:, h:h + 1], scalar2=0.0, op0=ALU.subtract, op1=ALU.max),
                        r=[("gbcb", j // 4), ("Gs", ub)], w=[("xm", gb)])
                import os
                _x = os.environ.get("GDNX", "")
                for bk in range(2):
                    p.op("act", lambda e, bk=bk, gb=gb: e.activation(out=eGbc[gb][:, bk * 512:(bk + 1) * 512], in_=self.psb[bk],
                                                                     func=(AF.Identity if _x == "copy" else AF.Exp)),
                         r=[("gbcb", bk)] + ([("xm", gb)] if _x == "dep" else []), w=[("eGbc", gb, bk)])
                p.op("act", lambda e, gb=gb: e.activation(out=xm[gb], in_=xm[gb], func=AF.Exp, scale=-1.0),
                     r=[("xm", gb)], w=[("xm", gb)])
                p.op("pool", lambda e, gb=gb, mI=mI: e.tensor_tensor(out=Di[gb], in0=xm[gb], in1=mI, op=ALU.mult),
                     r=[("xm", gb), "c2"], w=[("Di", gb)])
                p.op("pool", lambda e, gb=gb, mS=mS: e.tensor_tensor(out=Ds[gb], in0=xm[gb], in1=mS, op=ALU.mult),
                     r=[("xm", gb), "c2"], w=[("Ds", gb)])
                def head_gen(j, hb, h0=h0, gb=gb, ub=ub, d=d):
                    h = h0 + j
                    js = slice(j * 128, (j + 1) * 128)
                    kT_h, qT_h, k_h, v_h = kTb[gb][:, js], qTb[gb][:, js], kb[gb][:, js], vb[gb][:, js]
                    bk1, k1 = nbank()
                    psA, psQ = bk1[:, 0:128], bk1[:, 128:256]
                    p.op("pe", lambda e, psA=psA, kT_h=kT_h: e.matmul(psA, lhsT=kT_h, rhs=kT_h, start=True, stop=True),
                         r=[("kTb", gb)], w=[k1])
                    p.op("pe", lambda e, psQ=psQ, kT_h=kT_h, qT_h=qT_h: e.matmul(psQ, lhsT=qT_h, rhs=kT_h, start=True, stop=True),
                         r=[("kTb", gb), ("qTb", gb)], w=[k1])
                    yield
                    P0, Q0 = Pb[hb][0], Qb[hb][0]
                    p.op("dve", lambda e, P0=P0, psA=psA, ub=ub, h=h, gb=gb, js=js: e.scalar_tensor_tensor(
                        out=P0, in0=psA, scalar=negb[ub][:, h:h + 1], in1=Ds[gb][:, js], op0=ALU.mult, op1=ALU.mult),
                        r=[k1, ("negb", ub), ("Ds", gb)], w=[("P", hb, 0)])
                    p.op("dve", lambda e, hb=hb, psQ=psQ, gb=gb, js=js: e.tensor_tensor(
                        out=attn[hb], in0=psQ, in1=Di[gb][:, js], op=ALU.mult), r=[k1, ("Di", gb)], w=[("attn", hb)])
                    yield
                    bk2, k2 = nbank()
                    psT, psT2 = bk2[:, 0:128], bk2[:, 128:256]
                    p.op("pe", lambda e, psT=psT, P0=P0: e.transpose(psT, P0, self.ident), r=[("P", hb, 0), "consts"], w=[k2])
                    p.op("pe", lambda e, psT2=psT2, hb=hb: e.transpose(psT2, attn[hb], self.ident),
                         r=[("attn", hb), "consts"], w=[k2])
                    yield
                    evac(Q0, psT, [k2], [("Q", hb, 0)])
                    evac(attnT[hb], psT2, [k2], [("attnT", hb)])
                    R0 = Rb[hb][0]
                    p.op("pool", lambda e, R0=R0, Q0=Q0: e.tensor_tensor(out=R0, in0=Q0, in1=self.ident, op=ALU.add),
                         r=[("Q", hb, 0), "consts"], w=[("R", hb, 0)])
                    p.op("pool", lambda e, hb=hb, v_h=v_h, ub=ub, h=h: e.tensor_scalar(
                        out=Vb[hb], in0=v_h, scalar1=bt[ub][:, h:h + 1], scalar2=None, op0=ALU.mult),
                        r=[("vb", gb), ("bt", ub)], w=[("Vb", hb)])
                    p.op("pool", lambda e, hb=hb, k_h=k_h, ub=ub, h=h: e.tensor_scalar(
                        out=Kbg[hb], in0=k_h, scalar1=bw[ub][:, h:h + 1], scalar2=None, op0=ALU.mult),
                        r=[("kb", gb), ("bw", ub)], w=[("Kbg", hb)])
                    p.op("pool", lambda e, hb=hb, k_h=k_h, ub=ub, h=h: e.tensor_scalar(
                        out=kend[hb], in0=k_h, scalar1=eEnd[ub][:, h:h + 1], scalar2=None, op0=ALU.mult),
                        r=[("kb", gb), ("eEnd", ub)], w=[("kend", hb)])
                    p.op("pool", lambda e, hb=hb, qT_h=qT_h, gb=gb, js=js: e.tensor_tensor(
                        out=qdT[hb], in0=qT_h, in1=eGbc[gb][:, js], op=ALU.mult),
                        r=[("qTb", gb), ("eGbc", gb, j // 4)], w=[("qdT", hb)])
                    yield
                    cur = 0
                    for lvl in range(6):
                        nxt = 1 - cur
                        Pc, Qc, Pn, Qn = Pb[hb][cur], Qb[hb][cur], Pb[hb][nxt], Qb[hb][nxt]
                        bk3, k3 = nbank()
                        psP, psQ2 = bk3[:, 0:128], bk3[:, 128:256]
                        p.op("pe", lambda e, psP=psP, Pc=Pc, Qc=Qc: e.matmul(psP, lhsT=Qc, rhs=Pc, start=True, stop=True),
                             r=[("P", hb, cur), ("Q", hb, cur)], w=[k3])
                        if lvl < 5:
                            p.op("pe", lambda e, psQ2=psQ2, Pc=Pc, Qc=Qc: e.matmul(psQ2, lhsT=Pc, rhs=Qc, start=True, stop=True),
                                 r=[("P", hb, cur), ("Q", hb, cur)], w=[k3])
                        yield
                        evac(Pn, psP, [k3], [("P", hb, nxt)])
                        if lvl < 5:
                            evac(Qn, psQ2, [k3], [("Q", hb, nxt)])
                        yield
                        Rc, Rn = Rb[hb][cur], Rb[hb][nxt]
                        bk4, k4 = nbank()
                        psR = bk4[:, 0:128]
                        p.op("pe", lambda e, psR=psR, Pn=Pn, Rc=Rc: e.matmul(psR, lhsT=Pn, rhs=Rc, start=True, stop=True),
                             r=[("P", hb, nxt), ("R", hb, cur)], w=[k4])
                        yield
                        p.op("dve", lambda e, psR=psR, Rc=Rc, Rn=Rn: e.tensor_tensor(out=Rn, in0=psR, in1=Rc, op=ALU.add),
                             r=[k4, ("R", hb, cur)], w=[("R", hb, nxt)])
                        yield
                        cur = nxt
                    Rf = Rb[hb][cur]
                    kRf = ("R", hb, cur)
                    bk5, k5 = nbank()
                    psU, psW = bk5[:, 0:128], bk5[:, 128:256]
                    p.op("pe", lambda e, psU=psU, Rf=Rf, hb=hb: e.matmul(psU, lhsT=Rf, rhs=Vb[hb], start=True, stop=True),
                         r=[kRf, ("Vb", hb)], w=[k5])
                    p.op("pe", lambda e, psW=psW, Rf=Rf, hb=hb: e.matmul(psW, lhsT=Kbg[hb], rhs=Rf, start=True, stop=True),
                         r=[kRf, ("Kbg", hb)], w=[k5])
                    yield
                    evac(usb[hb], psU, [k5], [("usb", hb)])
                    evac(wT[hb], psW, [k5], [("wT", hb)])
                    yield
                    Sh = S[:, (d * GH + h) * 128:(d * GH + h + 1) * 128]
                    kS = ("S", d, h)
                    bk6, k6 = nbank()
                    psa = bk6[:, 0:128]
                    p.op("pe", lambda e, hb=hb, Sh=Sh, psa=psa: e.matmul(psa, lhsT=wT[hb], rhs=Sh, start=True, stop=True),
                         r=[("wT", hb), kS], w=[k6])
                    yield
                    p.op("dve", lambda e, hb=hb, psa=psa: e.tensor_tensor(out=vn[hb], in0=usb[hb], in1=psa, op=ALU.subtract),
                         r=[k6, ("usb", hb)], w=[("vn", hb)])
                    yield
                    bk7, k7 = nbank()
                    psb_, psc = bk7[:, 0:128], bk7[:, 128:256]
                    p.op("pe", lambda e, hb=hb, Sh=Sh, psb_=psb_: e.matmul(psb_, lhsT=qdT[hb], rhs=Sh, start=True, stop=False),
                         r=[("qdT", hb), kS], w=[k7])
                    p.op("pe", lambda e, hb=hb, psb_=psb_: e.matmul(psb_, lhsT=attnT[hb], rhs=vn[hb], start=False, stop=True),
                         r=[("attnT", hb), ("vn", hb)], w=[k7])
                    p.op("pe", lambda e, hb=hb, psc=psc: e.matmul(psc, lhsT=kend[hb], rhs=vn[hb], start=True, stop=True),
                         r=[("kend", hb), ("vn", hb)], w=[k7])
                    yield
                    p.op("act", lambda e, gb=gb, js=js, psb_=psb_: e.copy(out=osb[gb][:, js], in_=psb_), r=[k7], w=[("osb", gb, j)])
                    p.op("dve", lambda e, Sh=Sh, ub=ub, h=h, psc=psc: e.scalar_tensor_tensor(
                        out=Sh, in0=Sh, scalar=ge[ub][:, h:h + 1], in1=psc, op0=ALU.mult, op1=ALU.add),
                        r=[k7, kS, ("ge", ub)], w=[kS])

                for sg in range(0, HG, HGI):
                    gens_ = [head_gen(j, j % NH) for j in range(sg, sg + HGI)]
                    while gens_:
                        for g_ in list(gens_):
                            try:
                                next(g_)
                            except StopIteration:
                                gens_.remove(g_)
                p.dma("act", self.o_d[d, t * 128:(t + 1) * 128, h0 * 128:(h0 + HG) * 128], osb[gb],
                      r=[("osb", gb, j) for j in range(HG)], w=["od"])
    p.barrier()


K.stage_gdn = _stage_gdn


POOL_WIN = (2, 4, 8, 16)
NC3 = 20 * 128


def _pool_M(row_len, n, w):
    pos = np.arange(n) % row_len
    base = np.arange(n) - pos
    half = w // 2
    lo = np.clip(pos - half, 0, row_len - 1)
    hi = np.clip(pos + half - 1, 0, row_len - 1)
    M = np.zeros((n, n), np.float64)
    for tp in range(n):
        cnt = hi[tp] - lo[tp] + 1
        M[tp, base[tp] + lo[tp]:base[tp] + hi[tp] + 1] = 1.0 / cnt
        M[tp, tp] -= 1.0
    return M


def make_consts3():
    c = np.zeros((128, NC3), np.float32)
    for g, w in enumerate(POOL_WIN):
        Ml = _pool_M(64, 128, w)
        c[:, g * 128:(g + 1) * 128] = Ml.T
        Mc = _pool_M(256, 256, w)
        for i in range(2):
            for j in range(2):
                idx = 4 + g * 4 + i * 2 + j
                c[:, idx * 128:(idx + 1) * 128] = Mc[i * 128:(i + 1) * 128, j * 128:(j + 1) * 128].T
    return c


def _stage_mixout0(self, c3_d, poolw_d, tiles=range(NT)):
    p, A = self.p, self.A
    A.reset()
    v0 = self.v0
    self.yT_d = self.scratch("yT0", [NT, 128, KC * 128], BF16)
    c3 = A.f32(NC3)
    p.dma("sp", c3, c3_d, w=["c3"])
    pw = A.f32(8 * 256)
    p.dma("sp", pw.rearrange("p (g k d) -> p g k d", g=4, k=2), poolw_d.rearrange("g (k p) d -> p g k d", p=128), w=["pw"])
    pu = [A.f32(2048) for _ in range(2)]
    of_ = A.f32(3072)
    ob_ = A.f32(3072)
    zs = A.f32(3072)
    sq = A.f32(3072)
    y = [A.f32(D) for _ in range(2)]
    dT = A.f32(1024)
    yT = [A.bf16(D) for _ in range(2)]
    ss = A.f32(24)
    rstd = A.f32(24)
    inv128 = 1.0 / 128.0
    for it, t in enumerate(tiles):
        b = it % 2
        tsl = slice(t * 128, (t + 1) * 128)
        if t < 2:
            p.dma("sp", pu[b].rearrange("p (j c) -> p j c", j=2), self.poolu_d[0:256, :].rearrange("(j p) c -> p j c", p=128),
                  w=[("pu", b)])
        else:
            p.dma("sp", pu[b][:, 0:1024], self.poolu_d[tsl, :], w=[("pu", b)])
        p.dma("pool", of_, self.o_d[0, tsl, :], w=["of"])
        p.dma("pool", ob_, self.o_d[1, tsl, :], w=["ob"])
        p.dma("sp", zs, self.z_d[tsl, :], w=["zs"])
        for cc in range(8):
            g = cc // 2
            ps = self.psb[cc // 4]
            pk = ("pg", 0, cc // 4)
            o_ = ps[:, (cc % 4) * 128:(cc % 4 + 1) * 128]
            if t < 2:
                for j in range(2):
                    idx = 4 + g * 4 + t * 2 + j
                    p.op("pe", lambda e, o_=o_, b=b, cc=cc, j=j, idx=idx: e.matmul(
                        o_, lhsT=pu[b][:, j * 1024 + cc * 128:j * 1024 + (cc + 1) * 128], rhs=c3[:, idx * 128:(idx + 1) * 128],
                        start=(j == 0), stop=(j == 1)), r=[("pu", b), "c3"], w=[pk])
            else:
                p.op("pe", lambda e, o_=o_, b=b, cc=cc, g=g: e.matmul(
                    o_, lhsT=pu[b][:, cc * 128:(cc + 1) * 128], rhs=c3[:, g * 128:(g + 1) * 128], start=True, stop=True),
                    r=[("pu", b), "c3"], w=[pk])
        for h2 in range(2):
            p.op("act", lambda e, h2=h2: e.copy(out=dT[:, h2 * 512:(h2 + 1) * 512], in_=self.psb[h2]),
                 r=[("pg", 0, h2)], w=[("dT", h2)])
        for g in range(4):
            ps = self.psb[2 + g // 2]
            pk = ("pg", 1, g // 2)
            o_ = ps[:, (g % 2) * 256:(g % 2 + 1) * 256]
            for k2 in range(2):
                cc = g * 2 + k2
                p.op("pe", lambda e, o_=o_, cc=cc, g=g, k2=k2: e.matmul(
                    o_, lhsT=dT[:, cc * 128:(cc + 1) * 128], rhs=pw[:, (g * 2 + k2) * 256:(g * 2 + k2 + 1) * 256],
                    start=(k2 == 0), stop=(k2 == 1)), r=[("dT", cc // 4), "pw"], w=[pk])
        for h2 in range(2):
            p.op("dve", lambda e, h2=h2, b=b: e.tensor_tensor(
                out=y[b][:, h2 * 512:(h2 + 1) * 512], in0=self.psb[2 + h2], in1=v0[:, V0_PSC + h2 * 512:V0_PSC + (h2 + 1) * 512],
                op=ALU.mult), r=[("pg", 1, h2), "v0"], w=[("y", b, h2)])
        p.op("pool", lambda e: e.tensor_tensor(out=of_, in0=of_, in1=ob_, op=ALU.add), r=["of", "ob"], w=["of"])
        p.op("pool", lambda e: e.tensor_tensor(out=sq, in0=of_, in1=of_, op=ALU.mult), r=["of"], w=["sq"])
        p.op("dve", lambda e: e.tensor_reduce(out=ss, in_=sq.rearrange("p (h d) -> p h d", h=24), axis=AX.X, op=ALU.add),
             r=["sq"], w=["ss"])
        p.op("act", lambda e: e.activation(out=rstd, in_=ss, func=AF.Sqrt, bias=self.epsc, scale=inv128),
             r=["ss", "consts"], w=["rstd"])
        p.op("dve", lambda e: e.reciprocal(out=rstd, in_=rstd), r=["rstd"], w=["rstd"])
        for h in range(24):
            p.op("dve", lambda e, h=h, b=b: e.scalar_tensor_tensor(
                out=y[b][:, 1024 + h * 128:1024 + (h + 1) * 128], in0=of_[:, h * 128:(h + 1) * 128], scalar=rstd[:, h:h + 1],
                in1=v0[:, V0_GNW:V0_GNW + 128], op0=ALU.mult, op1=ALU.mult), r=["of", "rstd", "v0"], w=[("y", b, 2 + h)])
        p.op("pool", lambda e, b=b: e.tensor_tensor(out=y[b][:, 1024:4096], in0=y[b][:, 1024:4096], in1=zs, op=ALU.mult),
             r=[("y", b, 2 + h) for h in range(24)] + ["zs"], w=[("y", b, "g")])
        for q4 in range(8):
            ps = self.psb[4 + q4 % 4]
            pk = ("pT", q4 % 4)
            for j in range(4):
                kc = q4 * 4 + j
                p.op("pe", lambda e, ps=ps, j=j, kc=kc, b=b: e.transpose(ps[:, j * 128:(j + 1) * 128],
                                                                          y[b][:, kc * 128:(kc + 1) * 128], self.ident),
                     r=[("y", b, 0), ("y", b, 1), ("y", b, "g"), "consts"], w=[pk])
            if q4 % 2 == 0:
                p.op("act", lambda e, ps=ps, q4=q4, b=b: e.copy(out=yT[b][:, q4 * 512:(q4 + 1) * 512], in_=ps),
                     r=[pk], w=[("yT", b, q4)])
            else:
                p.op("dve", lambda e, ps=ps, q4=q4, b=b: e.tensor_copy(out=yT[b][:, q4 * 512:(q4 + 1) * 512], in_=ps),
                     r=[pk], w=[("yT", b, q4)])
        p.dma("act", self.yT_d[t], yT[b], r=[("yT", b, q4) for q4 in range(8)], w=["yTd"])
    p.barrier()


K.stage_mixout0 = _stage_mixout0


def _make_res_ep(self, src_d, dst_d, gi_L, gi_C, part_d=None):
    p = self.p
    st = {"cnt": 0, "blk": -1}

    def ep(t, bi, blk, ps_list, pkeys):
        A2 = Arena(self.arena_t, 44 * 1024)
        A2.reset(self.ep_base)
        gs = {"L": A2.f32(512), "C": A2.f32(512)}
        xs = [A2.f32(512) for _ in range(2)]
        tmp = [A2.f32(512) for _ in range(2)]
        pp = [A2.f32(512) for _ in range(2)]
        c0 = blk["c0"]
        nbw = blk["nbw"]
        if st["blk"] != bi:
            st["blk"] = bi
            p.dma("sp", gs["L"][:, 0:nbw], self.gbc_d[gi_L, :, c0:c0 + nbw], w=[("gs", "L")])
            p.dma("sp", gs["C"][:, 0:nbw], self.gbc_d[gi_C, :, c0:c0 + nbw], w=[("gs", "C")])
        i = st["cnt"] % 2
        st["cnt"] += 1
        side = "C" if t < 2 else "L"
        tsl = slice(t * 128, (t + 1) * 128)
        p.dma("pool", xs[i][:, 0:nbw], src_d[tsl, c0:c0 + nbw], w=[("xs", i)])
        if part_d is not None:
            p.dma("pool", pp[i][:, 0:nbw], part_d[tsl, c0:c0 + nbw], w=[("pp", i)])
            p.op("dve", lambda e: e.tensor_tensor(out=pp[i][:, 0:nbw], in0=ps_list[0][:, 0:nbw], in1=pp[i][:, 0:nbw], op=ALU.add),
                 r=[pkeys[0], ("pp", i)], w=[("pp", i)])
            p.op("dve", lambda e: e.tensor_tensor(out=tmp[i][:, 0:nbw], in0=pp[i][:, 0:nbw], in1=gs[side][:, 0:nbw], op=ALU.mult),
                 r=[("pp", i), ("gs", side)], w=[("tmp", i)])
        else:
            p.op("dve", lambda e: e.tensor_tensor(out=tmp[i][:, 0:nbw], in0=ps_list[0][:, 0:nbw], in1=gs[side][:, 0:nbw], op=ALU.mult),
                 r=[pkeys[0], ("gs", side)], w=[("tmp", i)])
        p.op("pool", lambda e: e.tensor_tensor(out=tmp[i][:, 0:nbw], in0=tmp[i][:, 0:nbw], in1=xs[i][:, 0:nbw], op=ALU.add),
             r=[("tmp", i), ("xs", i)], w=[("tmp", i)])
        p.dma("act", dst_d[tsl, c0:c0 + nbw], tmp[i][:, 0:nbw], r=[("tmp", i)], w=["resout"])
    return ep


K.make_res_ep = _make_res_ep


def _stage_outproj(self, yT_d, wout_d, src_d, dst_d, tiles=range(NT)):
    blocks = [dict(srcs=[wout_d[:, c:c + 512]], nbw=512, c0=c) for c in range(0, D, 512)]
    self.gemm_tm(yT_d, KC, blocks, self.make_res_ep(src_d, dst_d, 0, 1), tiles)


K.stage_outproj = _stage_outproj


def make_router(inp, layer):
    gw = inp["moe_group_w"][layer]
    ew = inp["moe_expert_w"][layer].transpose(1, 0, 2).reshape(D, 32)
    w = np.concatenate([gw, ew], 1)
    wr = np.ascontiguousarray(w.reshape(KC, 128, 36).transpose(1, 0, 2)).reshape(128, KC * 36)
    bias = np.concatenate([inp["moe_group_b"][layer].reshape(4), inp["moe_expert_b"][layer].reshape(32)])
    rb = np.broadcast_to(bias[None, :], (128, 36))
    return np.ascontiguousarray(np.concatenate([wr, rb], 1).astype(np.float32))


NRT = KC * 36 + 36


def _stage_router(self, fT32_d, rt_d, tiles=range(NT)):
    p, A = self.p, self.A
    A.reset()
    rt = A.f32(NRT)
    p.dma("sp", rt, rt_d, w=["rt"])
    wr = rt[:, 0:KC * 36].rearrange("p (k c) -> p k c", k=KC)
    rb = rt[:, KC * 36:KC * 36 + 36]
    ft = [A.f32(D) for _ in range(2)]
    gate = self.gate_sb

    def T(n):
        return A.f32(n)
    lg, ohg, eg, sel, oh1, sel2, oh2, g8 = T(36), T(4), T(4), T(8), T(8), T(8), T(8), T(8)
    gmax, ngmax, sumeg, pg, top1, top2, dlt, e21, den, w1, w2 = [T(1) for _ in range(11)]
    for it, t in enumerate(tiles):
        b = it % 2
        p.dma("sp" if it % 2 else "pool", ft[b], fT32_d[t], w=[("ft", b)])
        ft3 = ft[b].rearrange("p (k c) -> p k c", k=KC)
        ps = self.psb[it % 2]
        pk = ("pg", 0, it % 2)
        for kc in range(KC):
            p.op("pe", lambda e, ps=ps, ft3=ft3, kc=kc: e.matmul(ps[:, 0:36], lhsT=ft3[:, kc, :], rhs=wr[:, kc, :],
                                                                 start=(kc == 0), stop=(kc == KC - 1)),
                 r=[("ft", b), "rt"], w=[pk])
        R = "rtr"

        def dv(fn, extra_r=()):
            p.op("dve", fn, r=[R] + list(extra_r), w=[R])

        def ac(fn):
            p.op("act", fn, r=[R], w=[R])
        dv(lambda e, ps=ps: e.tensor_tensor(out=lg, in0=ps[:, 0:36], in1=rb, op=ALU.add), [pk, "rt"])
        dv(lambda e: e.tensor_reduce(out=gmax, in_=lg[:, 0:4], axis=AX.X, op=ALU.max))
        dv(lambda e: e.tensor_scalar(out=ohg, in0=lg[:, 0:4], scalar1=gmax[:, 0:1], scalar2=None, op0=ALU.is_equal))
        dv(lambda e: e.tensor_scalar(out=ngmax, in0=gmax, scalar1=-1.0, scalar2=None, op0=ALU.mult))
        ac(lambda e: e.activation(out=eg, in_=lg[:, 0:4], func=AF.Exp, bias=ngmax[:, 0:1]))
        dv(lambda e: e.tensor_reduce(out=sumeg, in_=eg, axis=AX.X, op=ALU.add))
        dv(lambda e: e.reciprocal(out=pg, in_=sumeg))
        dv(lambda e: e.tensor_scalar(out=sel, in0=lg[:, 4:12], scalar1=ohg[:, 0:1], scalar2=None, op0=ALU.mult))
        for g in range(1, 4):
            dv(lambda e, g=g: e.scalar_tensor_tensor(out=sel, in0=lg[:, 4 + 8 * g:12 + 8 * g], scalar=ohg[:, g:g + 1], in1=sel,
                                                      op0=ALU.mult, op1=ALU.add))
        dv(lambda e: e.tensor_reduce(out=top1, in_=sel, axis=AX.X, op=ALU.max))
        dv(lambda e: e.tensor_scalar(out=oh1, in0=sel, scalar1=top1[:, 0:1], scalar2=None, op0=ALU.is_equal))
        dv(lambda e: e.scalar_tensor_tensor(out=sel2, in0=oh1, scalar=-1.0e30, in1=sel, op0=ALU.mult, op1=ALU.add))
        dv(lambda e: e.tensor_reduce(out=top2, in_=sel2, axis=AX.X, op=ALU.max))
        dv(lambda e: e.tensor_scalar(out=oh2, in0=sel2, scalar1=top2[:, 0:1], scalar2=None, op0=ALU.is_equal))
        dv(lambda e: e.tensor_tensor(out=dlt, in0=top2, in1=top1, op=ALU.subtract))
        ac(lambda e: e.activation(out=e21, in_=dlt, func=AF.Exp))
        dv(lambda e: e.tensor_scalar(out=den, in0=e21, scalar1=1.0, scalar2=None, op0=ALU.add))
        dv(lambda e: e.reciprocal(out=w1, in_=den))
        dv(lambda e: e.tensor_tensor(out=w2, in0=e21, in1=w1, op=ALU.mult))
        dv(lambda e: e.tensor_tensor(out=w1, in0=w1, in1=pg, op=ALU.mult))
        dv(lambda e: e.tensor_tensor(out=w2, in0=w2, in1=pg, op=ALU.mult))
        dv(lambda e: e.tensor_scalar(out=g8, in0=oh1, scalar1=w1[:, 0:1], scalar2=None, op0=ALU.mult))
        dv(lambda e: e.scalar_tensor_tensor(out=g8, in0=oh2, scalar=w2[:, 0:1], in1=g8, op0=ALU.mult, op1=ALU.add))
        for g in range(4):
            p.op("dve", lambda e, g=g, t=t: e.tensor_scalar(out=gate[:, t * 32 + g * 8:t * 32 + (g + 1) * 8], in0=g8,
                                                           scalar1=ohg[:, g:g + 1], scalar2=None, op0=ALU.mult),
                 r=[R], w=[("gate", t)])
    p.barrier()


K.stage_router = _stage_router


def _stage_moe(self, fT_d, w1_d, w3_d, w2_d, src_d, dst_d, tiles=range(NT), tag="0"):
    p = self.p
    gate = self.gate_sb
    self.aT_d = self.scratch(f"aT_{tag}", [NT, 128, 128 * 128], BF16)
    self.ypart_d = self.scratch(f"ypart_{tag}", [NTOK, D], F32)
    st = {"cnt": 0}

    def ep_up(t, bi, blk, ps_list, pkeys):
        A2 = Arena(self.arena_t, 44 * 1024)
        A2.reset(self.ep_base)
        s_sb = [A2.f32(512) for _ in range(2)]
        a_sb = [A2.f32(512) for _ in range(2)]
        aT = [A2.bf16(512) for _ in range(2)]
        i = st["cnt"] % 2
        st["cnt"] += 1
        ex = blk["e"]
        p.op("act", lambda e: e.activation(out=s_sb[i], in_=ps_list[0], func=AF.Silu), r=[pkeys[0]], w=[("s_sb", i)])
        p.op("dve", lambda e: e.scalar_tensor_tensor(out=a_sb[i], in0=s_sb[i], scalar=gate[:, t * 32 + ex:t * 32 + ex + 1],
                                                     in1=ps_list[1], op0=ALU.mult, op1=ALU.mult),
             r=[("s_sb", i), pkeys[1], ("gate", t)], w=[("a_sb", i)])
        pst = self.psb[4 + i]
        for j in range(4):
            p.op("pe", lambda e, j=j: e.transpose(pst[:, j * 128:(j + 1) * 128], a_sb[i][:, j * 128:(j + 1) * 128], self.ident),
                 r=[("a_sb", i), "consts"], w=[("pst4", i)])
        p.op("act", lambda e: e.copy(out=aT[i], in_=pst), r=[("pst4", i)], w=[("aT", i)])
        p.dma("act", self.aT_d[t][:, ex * 512:(ex + 1) * 512], aT[i], r=[("aT", i)], w=["aTd"])

    blocks = [dict(srcs=[w1_d[ex], w3_d[ex]], nbw=512, e=ex) for ex in range(32)]
    self.gemm_tm(fT_d, KC, blocks, ep_up, tiles, wsets=1)

    st2 = {"cnt": 0}

    def ep_part(t, bi, blk, ps_list, pkeys):
        A2 = Arena(self.arena_t, 44 * 1024)
        A2.reset(self.ep_base)
        pb = [A2.f32(512) for _ in range(2)]
        i = st2["cnt"] % 2
        st2["cnt"] += 1
        c0 = blk["c0"]
        p.op("act", lambda e: e.copy(out=pb[i], in_=ps_list[0]), r=[pkeys[0]], w=[("pb", i)])
        p.dma("act", self.ypart_d[t * 128:(t + 1) * 128, c0:c0 + 512], pb[i], r=[("pb", i)], w=["ypd"])

    blocks = [dict(srcs=[w2_d[0:8192, c:c + 512]], nbw=512, c0=c) for c in range(0, D, 512)]
    self.gemm_tm(self.aT_d, 64, blocks, ep_part, tiles, wsets=1, at_c0=0)
    blocks = [dict(srcs=[w2_d[8192:16384, c:c + 512]], nbw=512, c0=c) for c in range(0, D, 512)]
    self.gemm_tm(self.aT_d, 64, blocks, self.make_res_ep(src_d, dst_d, 2, 3, part_d=self.ypart_d), tiles, wsets=1, at_c0=64)


K.stage_moe = _stage_moe


LH = 6
V1_GB = 0
V1_NW = 24
NV1 = 536
C4_CC = 0
C4_SC = 512
NC4 = 1024


def make_vecs1(inp):
    v = np.zeros((128, NV1), np.float32)
    gb = inp["gla_gate_b"][0]
    v[:, V1_GB:V1_GB + 24] = gb.reshape(2, 12, 128).transpose(2, 0, 1).reshape(128, 24)
    v[:, V1_NW:V1_NW + 512] = inp["gla_norm_w"][0].reshape(1, 512)
    return v


def make_gup(inp):
    gu = inp["gla_gate_up"][0]
    v = np.zeros((64, 3072), np.float32)
    v[0:16, 0:1536] = gu[0]
    v[32:48, 1536:3072] = gu[1]
    return v


def make_lrw(inp):
    w = inp["cd_w_in"][0][:, 10240:10272]
    o = np.zeros((D, 64), np.float32)
    o[:, 0:16] = w[:, 0:16]
    o[:, 32:48] = w[:, 16:32]
    return o


def make_consts4():
    c = np.zeros((128, NC4), np.float32)
    i = np.arange(256)
    ang = 2 * np.pi * np.outer(i, i) / 256.0
    Cc = np.cos(ang) / 16.0
    Sc = np.sin(ang) / 16.0
    c[:, C4_CC:C4_CC + 512] = Cc.reshape(2, 128, 256).transpose(1, 0, 2).reshape(128, 512)
    c[:, C4_SC:C4_SC + 512] = Sc.reshape(2, 128, 256).transpose(1, 0, 2).reshape(128, 512)
    return c


def make_dft_big():
    import ml_dtypes
    i = np.arange(4096)
    ang = 2 * np.pi * ((np.outer(i, i)) % 4096) / 4096.0
    cn = (np.cos(ang) / 64.0).astype(ml_dtypes.bfloat16)
    sn = (-np.sin(ang) / 64.0).astype(ml_dtypes.bfloat16)
    return cn, sn


def _stage_inproj1(self, hT_d, w_d, lrw_d, v1_d):
    p = self.p
    self.v1t = self.PA_v1
    p.dma("sp", self.v1t[:, 0:NV1], v1_d, w=["v1"])
    self.rawf_d = self.scratch("rawf", [8, 128, NTOK], F32)
    self.rawqk_d = self.scratch("rawqk1", [24, 128, NTOK], F32)
    self.v1_d = self.scratch("v1tm", [NTOK, 3072], F32)
    self.z1_d = self.scratch("zs1", [NTOK, 3072], F32)
    self.lrT_d = self.scratch("lrT", [64, NTOK], F32)

    def mk_ep_tm(dst, silu):
        st = {"cnt": 0}

        def ep(t, bi, blk, ps_list, pkeys):
            A2 = Arena(self.arena_t, 44 * 1024)
            A2.reset(self.ep_base)
            bufs = [A2.f32(512) for _ in range(2)]
            i = st["cnt"] % 2
            st["cnt"] += 1
            ob = bufs[i]
            if silu:
                p.op("act", lambda e: e.activation(out=ob, in_=ps_list[0], func=AF.Silu), r=[pkeys[0]], w=[("epo", i)])
            else:
                p.op("act", lambda e: e.copy(out=ob, in_=ps_list[0]), r=[pkeys[0]], w=[("epo", i)])
            p.dma("act", dst[t * 128:(t + 1) * 128, bi * 512:(bi + 1) * 512], ob, r=[("epo", i)], w=["eptm"])
        return ep

    def mk_ep_fm(dst, ncol=512):
        st = {"cnt": 0}

        def ep(t, bi, blk, ps_list, pkeys):
            A2 = Arena(self.arena_t, 44 * 1024)
            A2.reset(self.ep_base)
            tm = [A2.f32(512) for _ in range(2)]
            fm = [A2.f32(512) for _ in range(2)]
            i = st["cnt"] % 2
            st["cnt"] += 1
            nj = ncol // 128 if ncol >= 128 else 1
            p.op("act", lambda e: e.copy(out=tm[i][:, 0:ncol], in_=ps_list[0][:, 0:ncol]), r=[pkeys[0]], w=[("tm", i)])
            pst = self.psb[4 + i]
            if ncol >= 128:
                for j in range(nj):
                    p.op("pe", lambda e, j=j: e.transpose(pst[:, j * 128:(j + 1) * 128], tm[i][:, j * 128:(j + 1) * 128], self.ident),
                         r=[("tm", i), "consts"], w=[("pst4", i)])
                p.op("dve", lambda e: e.tensor_copy(out=fm[i], in_=pst), r=[("pst4", i)], w=[("fm", i)])
                p.dma("act", dst[bi * 4:(bi + 1) * 4, :, t * 128:(t + 1) * 128].rearrange("j p c -> p j c"),
                      fm[i].rearrange("p (j c) -> p j c", j=4), r=[("fm", i)], w=["epfm"])
            else:
                p.op("pe", lambda e: e.transpose(pst[0:ncol, 0:128], tm[i][:, 0:ncol], self.ident),
                     r=[("tm", i), "consts"], w=[("pst4", i)])
                p.op("dve", lambda e: e.tensor_copy(out=fm[i][0:ncol, 0:128], in_=pst[0:ncol, 0:128]), r=[("pst4", i)], w=[("fm", i)])
                p.dma("act", dst[:, t * 128:(t + 1) * 128], fm[i][0:ncol, 0:128], r=[("fm", i)], w=["epfm"])
        return ep

    self.gemm_tm(hT_d, KC, [dict(srcs=[w_d[:, c:c + 512]], nbw=512) for c in range(0, 1024, 512)], mk_ep_fm(self.rawf_d), range(NT))
    self.gemm_tm(hT_d, KC, [dict(srcs=[w_d[:, 1024 + c:1024 + c + 512]], nbw=512) for c in range(0, 3072, 512)],
                 mk_ep_fm(self.rawqk_d), range(NT))
    self.gemm_tm(hT_d, KC, [dict(srcs=[w_d[:, 4096 + c:4096 + c + 512]], nbw=512) for c in range(0, 3072, 512)],
                 mk_ep_tm(self.v1_d, False), range(NT))
    self.gemm_tm(hT_d, KC, [dict(srcs=[w_d[:, 7168 + c:7168 + c + 512]], nbw=512) for c in range(0, 3072, 512)],
                 mk_ep_tm(self.z1_d, True), range(NT))
    self.gemm_tm(hT_d, KC, [dict(srcs=[lrw_d], nbw=64)], mk_ep_fm(self.lrT_d, ncol=64), range(NT))


K.stage_inproj1 = _stage_inproj1


def _stage_gk(self, gup_d):
    p, A = self.p, self.A
    A.reset()
    v1 = self.v1t
    self.gkT_d = self.scratch("gkT", [2, 12, 128, NTOK], F32)
    lrT = A.f32(NTOK)
    p.dma("sp", lrT[0:64, :], self.lrT_d, w=["lrT"])
    gup = A.f32(3072)
    p.dma("sp", gup[0:64, :], gup_d, w=["gup"])
    negb = A.f32(24)
    p.op("dve", lambda e: e.tensor_scalar(out=negb, in0=v1[:, V1_GB:V1_GB + 24], scalar1=-1.0, scalar2=None, op0=ALU.mult),
         r=["v1"], w=["negb1"])
    ob = [A.f32(512) for _ in range(2)]
    cnt = 0
    for d in range(2):
        for ch in range(12):
            for c0 in range(0, NTOK, 512):
                cn = min(512, NTOK - c0)
                i = cnt % 2
                cnt += 1
                ps = self.psb[i]
                pk = ("pg", 0, i)
                p.op("pe", lambda e, ps=ps, d=d, ch=ch, c0=c0, cn=cn: e.matmul(
                    ps[:, 0:cn], lhsT=gup[0:64, d * 1536 + ch * 128:d * 1536 + (ch + 1) * 128],
                    rhs=lrT[0:64, c0:c0 + cn], start=True, stop=True), r=["gup", "lrT"], w=[pk])
                p.op("act", lambda e, ps=ps, i=i, d=d, ch=ch, cn=cn: e.activation(
                    out=ob[i][:, 0:cn], in_=ps[:, 0:cn], func=AF.Exp, bias=negb[:, d * 12 + ch:d * 12 + ch + 1], scale=-1.0),
                    r=[pk, "negb1"], w=[("gko", i)])
                p.op("act", lambda e, i=i, cn=cn: e.activation(out=ob[i][:, 0:cn], in_=ob[i][:, 0:cn], func=AF.Ln, bias=self.ones[:, 0:1]),
                     r=[("gko", i), "consts"], w=[("gko", i)])
                p.op("dve", lambda e, i=i, cn=cn: e.tensor_scalar(out=ob[i][:, 0:cn], in0=ob[i][:, 0:cn], scalar1=-1.0 / 16.0,
                                                                 scalar2=None, op0=ALU.mult), r=[("gko", i)], w=[("gko", i)])
                p.dma("sp", self.gkT_d[d, ch, :, c0:c0 + cn], ob[i][:, 0:cn], r=[("gko", i)], w=["gkd"])
    p.barrier()


K.stage_gk = _stage_gk


def _stage_gla(self, c2_d, nsteps=NT):
    p, A = self.p, self.A
    A.reset()
    self.o1_d = self.scratch("ogla", [2, NTOK, 3072], F32)
    c2 = A.f32(NC2)
    p.dma("sp", c2, c2_d, w=["c2"])
    S = A.f32(2 * LH * 2 * 512)
    p.op("pool", lambda e: e.memset(S, 0.0), w=[("S1", d, h) for d in range(2) for h in range(LH)])
    onesf = self.ones
    NB = 2
    qT = [A.f32(256) for _ in range(NB)]
    kT = [A.f32(256) for _ in range(NB)]
    gk = [A.f32(256) for _ in range(NB)]
    vt = [A.f32(512) for _ in range(NB)]
    G = [A.f32(256) for _ in range(NB)]
    eP = [A.f32(256) for _ in range(NB)]
    eN = [A.f32(256) for _ in range(NB)]
    eE = [A.f32(256) for _ in range(NB)]
    qd = [A.f32(256) for _ in range(NB)]
    ki = [A.f32(256) for _ in range(NB)]
    keT = [A.f32(256) for _ in range(NB)]
    ke = [A.f32(256) for _ in range(NB)]
    attnT = [A.f32(128) for _ in range(NB)]
    tot = [A.f32(2) for _ in range(NB)]
    ge = [A.f32(2) for _ in range(NB)]
    osb = [A.f32(512) for _ in range(NB)]
    bctr = [0]
    BANKS = [0, 1, 2, 3, 4, 5, 6, 7]

    def nbank():
        i = BANKS[bctr[0] % len(BANKS)]
        bctr[0] += 1
        return self.psb[i], ("bank", i)

    order = {0: list(range(NT)), 1: [1, 0] + list(range(NT - 1, 1, -1))}
    uc = 0
    for step in range(nsteps):
        for d in range(2):
            t = order[d][step]
            tsl = slice(t * 128, (t + 1) * 128)
            mk = c2[:, C2_MBI:C2_MBI + 128] if d == 0 else c2[:, C2_MFI:C2_MFI + 128]
            for h in range(LH):
                b = uc % NB
                uc += 1
                p.dma("sp", qT[b].rearrange("p (k c) -> p k c", k=2), self.rawqk_d[2 * h:2 * h + 2, :, tsl].rearrange("k p c -> p k c"),
                      w=[("qT1", b)])
                p.dma("pool", kT[b].rearrange("p (k c) -> p k c", k=2),
                      self.rawqk_d[12 + 2 * h:12 + 2 * h + 2, :, tsl].rearrange("k p c -> p k c"), w=[("kT1", b)])
                p.dma("sp", gk[b].rearrange("p (k c) -> p k c", k=2), self.gkT_d[d, 2 * h:2 * h + 2, :, tsl].rearrange("k p c -> p k c"),
                      w=[("gk1", b)])
                p.dma("pool", vt[b], self.v1_d[tsl, h * 512:(h + 1) * 512], w=[("vt1", b)])
                for kc in range(2):
                    ks = slice(kc * 128, (kc + 1) * 128)
                    p.op("dve", lambda e, b=b, ks=ks: e.tensor_tensor_scan(out=G[b][:, ks], data0=onesf, data1=gk[b][:, ks],
                                                                          initial=0.0, op0=ALU.mult, op1=ALU.add),
                         r=[("gk1", b), "consts"], w=[("G1", b, kc)])
                    p.op("pool", lambda e, b=b, kc=kc: e.tensor_copy(out=tot[b][:, kc:kc + 1], in_=G[b][:, kc * 128 + 127:kc * 128 + 128]),
                         r=[("G1", b, kc)], w=[("tot1", b, kc)])
                    if d == 1:
                        p.op("dve", lambda e, b=b, ks=ks, kc=kc: e.tensor_scalar(out=G[b][:, ks], in0=G[b][:, ks], scalar1=-1.0,
                                                                                 scalar2=tot[b][:, kc:kc + 1], op0=ALU.mult, op1=ALU.add),
                             r=[("G1", b, kc), ("tot1", b, kc)], w=[("G1", b, kc)])
                        p.op("dve", lambda e, b=b, ks=ks: e.tensor_tensor(out=G[b][:, ks], in0=G[b][:, ks], in1=gk[b][:, ks], op=ALU.add),
                             r=[("G1", b, kc), ("gk1", b)], w=[("G1", b, kc)])
                    p.op("act", lambda e, b=b, ks=ks: e.activation(out=eP[b][:, ks], in_=G[b][:, ks], func=AF.Exp),
                         r=[("G1", b, kc)], w=[("eP", b, kc)])
                    p.op("act", lambda e, b=b, ks=ks: e.activation(out=eN[b][:, ks], in_=G[b][:, ks], func=AF.Exp, scale=-1.0),
                         r=[("G1", b, kc)], w=[("eN", b, kc)])
                    p.op("act", lambda e, b=b, ks=ks, kc=kc: e.activation(out=eE[b][:, ks], in_=G[b][:, ks], func=AF.Exp, scale=-1.0,
                                                                          bias=tot[b][:, kc:kc + 1]),
                         r=[("G1", b, kc), ("tot1", b, kc)], w=[("eE", b, kc)])
                    p.op("act", lambda e, b=b, kc=kc: e.activation(out=ge[b][:, kc:kc + 1], in_=tot[b][:, kc:kc + 1], func=AF.Exp),
                         r=[("tot1", b, kc)], w=[("ge1", b, kc)])
                    p.op("dve", lambda e, b=b, ks=ks: e.scalar_tensor_tensor(out=qd[b][:, ks], in0=qT[b][:, ks], scalar=0.0625,
                                                                             in1=eP[b][:, ks], op0=ALU.mult, op1=ALU.mult),
                         r=[("qT1", b), ("eP", b, kc)], w=[("qd", b, kc)])
                    p.op("pool", lambda e, b=b, ks=ks: e.tensor_tensor(out=ki[b][:, ks], in0=kT[b][:, ks], in1=eN[b][:, ks], op=ALU.mult),
                         r=[("kT1", b), ("eN", b, kc)], w=[("ki", b, kc)])
                    p.op("pool", lambda e, b=b, ks=ks: e.tensor_tensor(out=keT[b][:, ks], in0=kT[b][:, ks], in1=eE[b][:, ks], op=ALU.mult),
                         r=[("kT1", b), ("eE", b, kc)], w=[("keT", b, kc)])
                bk1, k1 = nbank()
                for kc in range(2):
                    ks = slice(kc * 128, (kc + 1) * 128)
                    p.op("pe", lambda e, bk1=bk1, b=b, ks=ks, kc=kc: e.matmul(bk1[:, 0:128], lhsT=ki[b][:, ks], rhs=qd[b][:, ks],
                                                                              start=(kc == 0), stop=(kc == 1)),
                         r=[("ki", b, kc), ("qd", b, kc)], w=[k1])
                p.op("dve", lambda e, bk1=bk1, b=b, mk=mk: e.tensor_tensor(out=attnT[b], in0=bk1[:, 0:128], in1=mk, op=ALU.mult),
                     r=[k1, "c2"], w=[("attnT1", b)])
                bk2, k2 = nbank()
                for kc in range(2):
                    ks = slice(kc * 128, (kc + 1) * 128)
                    p.op("pe", lambda e, bk2=bk2, b=b, ks=ks: e.transpose(bk2[:, ks], keT[b][:, ks], self.ident),
                         r=[("keT", b, kc), "consts"], w=[k2])
                p.op("act", lambda e, bk2=bk2, b=b: e.copy(out=ke[b], in_=bk2[:, 0:256]), r=[k2], w=[("ke", b)])
                bk3, k3 = nbank()
                Sb = (d * LH + h) * 1024
                kS = ("S1", d, h)
                for kc in range(2):
                    ks = slice(kc * 128, (kc + 1) * 128)
                    p.op("pe", lambda e, bk3=bk3, b=b, ks=ks, kc=kc, Sb=Sb: e.matmul(
                        bk3, lhsT=qd[b][:, ks], rhs=S[:, Sb + kc * 512:Sb + (kc + 1) * 512], start=(kc == 0), stop=False),
                        r=[("qd", b, kc), kS], w=[k3])
                p.op("pe", lambda e, bk3=bk3, b=b: e.matmul(bk3, lhsT=attnT[b], rhs=vt[b], start=False, stop=True),
                     r=[("attnT1", b), ("vt1", b)], w=[k3])
                p.op("act", lambda e, bk3=bk3, b=b: e.copy(out=osb[b], in_=bk3), r=[k3], w=[("osb1", b)])
                p.dma("act", self.o1_d[d, tsl, h * 512:(h + 1) * 512], osb[b], r=[("osb1", b)], w=["o1d"])
                for kc in range(2):
                    ks = slice(kc * 128, (kc + 1) * 128)
                    bk4, k4 = nbank()
                    p.op("pe", lambda e, bk4=bk4, b=b, ks=ks: e.matmul(bk4, lhsT=ke[b][:, ks], rhs=vt[b], start=True, stop=True),
                         r=[("ke", b), ("vt1", b)], w=[k4])
                    Ss = S[:, Sb + kc * 512:Sb + (kc + 1) * 512]
                    p.op("dve", lambda e, bk4=bk4, Ss=Ss, b=b, kc=kc: e.scalar_tensor_tensor(
                        out=Ss, in0=Ss, scalar=ge[b][:, kc:kc + 1], in1=bk4, op0=ALU.mult, op1=ALU.add),
                        r=[k4, kS, ("ge1", b, kc)], w=[kS])
    p.barrier()


K.stage_gla = _stage_gla


def _stage_fourier(self, c4_d, cn_d, sn_d):
    p, A = self.p, self.A
    A.reset()
    self.PQ_d = self.scratch("PQ", [4, 128, 32 * 512], BF16)
    self.fT_d = self.scratch("fTf", [8, 128, NLAT], F32)
    c4 = A.f32(NC4)
    p.dma("sp", c4, c4_d, w=["c4"])
    ut = [A.f32(256) for _ in range(2)]
    pq = [A.bf16(512) for _ in range(2)]
    cnt = 0
    for g in range(4):
        for tt in range(32):
            t = tt + 2
            i = cnt % 2
            cnt += 1
            p.dma("sp", ut[i].rearrange("p (k c) -> p k c", k=2),
                  self.rawf_d[2 * g:2 * g + 2, :, t * 128:(t + 1) * 128].rearrange("k p c -> p k c"), w=[("ut", i)])
            ps = self.psb[i]
            pk = ("pg", 0, i)
            for part, off in ((0, C4_CC), (1, C4_SC)):
                for k2 in range(2):
                    p.op("pe", lambda e, ps=ps, i=i, k2=k2, part=part, off=off: e.matmul(
                        ps[:, part * 256:(part + 1) * 256], lhsT=ut[i][:, k2 * 128:(k2 + 1) * 128],
                        rhs=c4[:, off + k2 * 256:off + (k2 + 1) * 256], start=(k2 == 0), stop=(k2 == 1)),
                        r=[("ut", i), "c4"], w=[pk])
            p.op("act", lambda e, ps=ps, i=i: e.copy(out=pq[i], in_=ps), r=[pk], w=[("pq", i)])
            p.dma("act", self.PQ_d[g][:, tt * 512:(tt + 1) * 512], pq[i], r=[("pq", i)], w=["PQd"])
    p.barrier()
    A.reset()
    cnb = A.bf16(32 * 512)
    snb = A.bf16(32 * 512)
    PQ = [A.bf16(32 * 512) for _ in range(2)]
    ob = [A.f32(512) for _ in range(2)]
    cnt = 0
    oc = 0
    for nb in range(8):
        p.dma("sp", cnb.rearrange("p (t c) -> p t c", t=32), cn_d[:, nb * 512:(nb + 1) * 512].rearrange("(t p) c -> p t c", p=128),
              w=["cnb"])
        p.dma("pool", snb.rearrange("p (t c) -> p t c", t=32), sn_d[:, nb * 512:(nb + 1) * 512].rearrange("(t p) c -> p t c", p=128),
              w=["snb"])
        for g in range(4):
            i = cnt % 2
            cnt += 1
            p.dma("sp" if i else "pool", PQ[i], self.PQ_d[g], w=[("PQ", i)])
            for cc in range(2):
                j = oc % 2
                oc += 1
                ps = self.psb[j]
                pk = ("pg", 0, j)
                for tt in range(32):
                    p.op("pe", lambda e, ps=ps, i=i, tt=tt, cc=cc: e.matmul(
                        ps, lhsT=PQ[i][:, tt * 512 + cc * 128:tt * 512 + (cc + 1) * 128], rhs=cnb[:, tt * 512:(tt + 1) * 512],
                        start=(tt == 0), stop=False), r=[("PQ", i), "cnb"], w=[pk])
                    p.op("pe", lambda e, ps=ps, i=i, tt=tt, cc=cc: e.matmul(
                        ps, lhsT=PQ[i][:, tt * 512 + 256 + cc * 128:tt * 512 + 256 + (cc + 1) * 128], rhs=snb[:, tt * 512:(tt + 1) * 512],
                        start=False, stop=(tt == 31)), r=[("PQ", i), "snb"], w=[pk])
                p.op("act", lambda e, ps=ps, j=j: e.copy(out=ob[j], in_=ps), r=[pk], w=[("fob", j)])
                p.dma("act", self.fT_d[g * 2 + cc][:, nb * 512:(nb + 1) * 512], ob[j], r=[("fob", j)], w=["fTd"])
    p.barrier()


K.stage_fourier = _stage_fourier


def _stage_mixout1(self, fw_d, tiles=range(2, NT)):
    p, A = self.p, self.A
    A.reset()
    v1 = self.v1t
    self.yT1_d = self.scratch("yT1", [NT, 128, KC * 128], BF16)
    fw = A.f32(8 * 256)
    p.dma("sp", fw.rearrange("p (g k d) -> p g k d", g=4, k=2), fw_d.rearrange("g (k p) d -> p g k d", p=128), w=["fw"])
    ft = [A.f32(1024) for _ in range(2)]
    of_ = A.f32(3072)
    ob_ = A.f32(3072)
    zs = A.f32(3072)
    sq = A.f32(3072)
    y = [A.f32(D) for _ in range(2)]
    yT = [A.bf16(D) for _ in range(2)]
    ss = A.f32(8)
    rstd = A.f32(8)
    for it, t in enumerate(tiles):
        b = it % 2
        tt = t - 2
        tsl = slice(t * 128, (t + 1) * 128)
        p.dma("sp", ft[b].rearrange("p (k c) -> p k c", k=8), self.fT_d[:, :, tt * 128:(tt + 1) * 128].rearrange("k p c -> p k c"),
              w=[("ft1", b)])
        p.dma("pool", of_, self.o1_d[0, tsl, :], w=["of"])
        p.dma("pool", ob_, self.o1_d[1, tsl, :], w=["ob"])
        p.dma("sp", zs, self.z1_d[tsl, :], w=["zs"])
        for g in range(4):
            ps = self.psb[g // 2]
            pk = ("pg", 0, g // 2)
            o_ = ps[:, (g % 2) * 256:(g % 2 + 1) * 256]
            for k2 in range(2):
                cc = g * 2 + k2
                p.op("pe", lambda e, o_=o_, cc=cc, b=b: e.matmul(
                    o_, lhsT=ft[b][:, cc * 128:(cc + 1) * 128], rhs=fw[:, cc * 256:(cc + 1) * 256], start=(cc % 2 == 0), stop=(cc % 2 == 1)),
                    r=[("ft1", b), "fw"], w=[pk])
        for h2 in range(2):
            p.op("act", lambda e, h2=h2, b=b: e.copy(out=y[b][:, h2 * 512:(h2 + 1) * 512], in_=self.psb[h2]),
                 r=[("pg", 0, h2)], w=[("y", b, h2)])
        p.op("pool", lambda e: e.tensor_tensor(out=of_, in0=of_, in1=ob_, op=ALU.add), r=["of", "ob"], w=["of"])
        p.op("pool", lambda e: e.tensor_tensor(out=sq, in0=of_, in1=of_, op=ALU.mult), r=["of"], w=["sq"])
        p.op("dve", lambda e: e.tensor_reduce(out=ss[:, 0:6], in_=sq.rearrange("p (h d) -> p h d", h=6), axis=AX.X, op=ALU.add),
             r=["sq"], w=["ss"])
        p.op("act", lambda e: e.activation(out=rstd[:, 0:6], in_=ss[:, 0:6], func=AF.Sqrt, bias=self.epsc, scale=1.0 / 512.0),
             r=["ss", "consts"], w=["rstd"])
        p.op("dve", lambda e: e.reciprocal(out=rstd[:, 0:6], in_=rstd[:, 0:6]), r=["rstd"], w=["rstd"])
        for h in range(6):
            p.op("dve", lambda e, h=h, b=b: e.scalar_tensor_tensor(
                out=y[b][:, 1024 + h * 512:1024 + (h + 1) * 512], in0=of_[:, h * 512:(h + 1) * 512], scalar=rstd[:, h:h + 1],
                in1=v1[:, V1_NW:V1_NW + 512], op0=ALU.mult, op1=ALU.mult), r=["of", "rstd", "v1"], w=[("y", b, 2 + h)])
        p.op("pool", lambda e, b=b: e.tensor_tensor(out=y[b][:, 1024:4096], in0=y[b][:, 1024:4096], in1=zs, op=ALU.mult),
             r=[("y", b, 2 + h) for h in range(6)] + ["zs"], w=[("y", b, "g")])
        for q4 in range(8):
            ps = self.psb[4 + q4 % 4]
            pk = ("pT", q4 % 4)
            for j in range(4):
                kc = q4 * 4 + j
                p.op("pe", lambda e, ps=ps, j=j, kc=kc, b=b: e.transpose(ps[:, j * 128:(j + 1) * 128],
                                                                          y[b][:, kc * 128:(kc + 1) * 128], self.ident),
                     r=[("y", b, 0), ("y", b, 1), ("y", b, "g"), "consts"], w=[pk])
            if q4 % 2 == 0:
                p.op("act", lambda e, ps=ps, q4=q4, b=b: e.copy(out=yT[b][:, q4 * 512:(q4 + 1) * 512], in_=ps),
                     r=[pk], w=[("yT", b, q4)])
            else:
                p.op("dve", lambda e, ps=ps, q4=q4, b=b: e.tensor_copy(out=yT[b][:, q4 * 512:(q4 + 1) * 512], in_=ps),
                     r=[pk], w=[("yT", b, q4)])
        p.dma("act", self.yT1_d[t], yT[b], r=[("yT", b, q4) for q4 in range(8)], w=["yTd"])
    p.barrier()


K.stage_mixout1 = _stage_mixout1


def _stage_final(self, x_d, fnw_d, out_d, tiles=range(2, NT)):
    p, A = self.p, self.A
    A.reset()
    fnw = A.f32(D)
    p.dma("sp", fnw, fnw_d, w=["fnw"])
    xt = [A.f32(D) for _ in range(2)]
    junk = A.f32(D)
    ss = [A.f32(1) for _ in range(2)]
    for it, t in enumerate(tiles):
        b = it % 2
        p.dma("sp", xt[b], x_d[t * 128:(t + 1) * 128, :], w=[("xt", b)])
        p.op("act", lambda e, b=b: e.activation(out=junk, in_=xt[b], func=AF.Square, accum_out=ss[b]), r=[("xt", b)], w=["junk", ("ss", b)])
        p.op("act", lambda e, b=b: e.activation(out=ss[b], in_=ss[b], func=AF.Sqrt, bias=self.epsc, scale=1.0 / D),
             r=[("ss", b), "consts"], w=[("ss", b)])
        p.op("dve", lambda e, b=b: e.reciprocal(out=ss[b], in_=ss[b]), r=[("ss", b)], w=[("ss", b)])
        p.op("dve", lambda e, b=b: e.scalar_tensor_tensor(out=xt[b], in0=xt[b], scalar=ss[b][:, 0:1], in1=fnw, op0=ALU.mult, op1=ALU.mult),
             r=[("xt", b), ("ss", b), "fnw"], w=[("xt", b)])
        p.dma("act", out_d[(t - 2) * 128:(t - 1) * 128, :], xt[b], r=[("xt", b)], w=["outd"])
    p.barrier()


K.stage_final = _stage_final


W_NAMES = ["modw0", "modw1", "win0", "wout0", "poolw", "win1", "lrw", "gup", "wout1", "fw",
           "w1_0", "w3_0", "w2_0", "w1_1", "w3_1", "w2_1"]


def build_full():
    k = K()
    k.setup()
    e = k.ext_in
    xin_d = e("xin", [NTOK, D])
    cvec_d = e("cvec", [64, 128])
    modw_d = [e("modw0", [D, 6 * D]), e("modw1", [D, 6 * D])]
    vecs_d = [e("vecs0", [128, NVEC]), e("vecs1", [128, NVEC])]
    v0_d = e("v0", [128, NV0])
    v1_d = e("v1", [128, NV1])
    c2_d = e("c2", [128, NC2])
    c3_d = e("c3", [128, NC3])
    c4_d = e("c4", [128, NC4])
    cn_d = e("cn", [4096, 4096], BF16)
    sn_d = e("sn", [4096, 4096], BF16)
    win0_d = e("win0", [D, 13408])
    wout0_d = e("wout0", [D, D])
    poolw_d = e("poolw", [4, 256, 256])
    win1_d = e("win1", [D, 10272])
    lrw_d = e("lrw", [D, 64])
    gup_d = e("gup", [64, 3072])
    wout1_d = e("wout1", [D, D])
    fw_d = e("fw", [4, 256, 256])
    rt_d = [e("rt0", [128, NRT]), e("rt1", [128, NRT])]
    w1_d = [e("w1_0", [32, D, 512]), e("w1_1", [32, D, 512])]
    w3_d = [e("w3_0", [32, D, 512]), e("w3_1", [32, D, 512])]
    w2_d = [e("w2_0", [32 * 512, D]), e("w2_1", [32 * 512, D])]
    fnw_d = e("fnw", [128, D])
    out_d = k.ext_out("out", [NLAT, D])
    k.stage_mod(0, cvec_d, modw_d[0], vecs_d[0])
    hT0 = k.scratch("hT0", [NT, 128, KC * 128], BF16)
    k.stage_norm(xin_d, 0, out_bf_d=hT0)
    k.stage_inproj0(hT0, win0_d, v0_d)
    k.stage_conv0()
    k.stage_gdn(c2_d)
    k.stage_mixout0(c3_d, poolw_d)
    x1_d = k.scratch("x1", [NTOK, D], F32)
    k.stage_outproj(k.yT_d, wout0_d, xin_d, x1_d)
    fT0_32 = k.scratch("fT0_32", [NT, 128, KC * 128], F32)
    ftm0 = k.scratch("ftm0", [NTOK, D], BF16)
    k.stage_norm(x1_d, 1, out_f32_d=fT0_32, out_tm_d=ftm0)
    x2_d = k.scratch("x2", [NTOK, D], F32)
    k.stage_moe_sparse(0, fT0_32, ftm0, rt_d[0], c2_d, w1_d[0], w3_d[0], w2_d[0], x1_d, x2_d)
    LT = range(2, NT)
    k.stage_mod(1, cvec_d, modw_d[1], vecs_d[1])
    hT1 = k.scratch("hT1", [NT, 128, KC * 128], BF16)
    k.stage_norm(x2_d, 0, out_bf_d=hT1)
    k.stage_inproj1(hT1, win1_d, lrw_d, v1_d)
    k.stage_gk(gup_d)
    k.stage_gla(c2_d)
    k.stage_fourier(c4_d, cn_d, sn_d)
    k.stage_mixout1(fw_d)
    x3_d = k.scratch("x3", [NTOK, D], F32)
    k.stage_outproj(k.yT1_d, wout1_d, x2_d, x3_d, tiles=LT)
    fT1_32 = k.scratch("fT1_32", [NT, 128, KC * 128], F32)
    ftm1 = k.scratch("ftm1", [NTOK, D], BF16)
    k.stage_norm(x3_d, 1, out_f32_d=fT1_32, out_tm_d=ftm1, tiles=LT)
    x4_d = k.scratch("x4", [NTOK, D], F32)
    k.stage_moe_sparse(1, fT1_32, ftm1, rt_d[1], c2_d, w1_d[1], w3_d[1], w2_d[1], x3_d, x4_d, tiles=LT)
    k.stage_final(x4_d, fnw_d, out_d)
    k.p.build()
    return k


def host_inputs(inp, b, shared):
    m = dict(shared)
    m["xin"] = np.ascontiguousarray(np.concatenate([inp["ctx"][b], inp["x"][b]], 0))
    m["cvec"] = np.ascontiguousarray(np.concatenate([inp["c"][b].reshape(32, 128), inp["c_ctx"].reshape(32, 128)], 0))
    return m


def host_shared(inp):
    cn, sn = make_dft_big()
    s = {
        "consts": make_consts(), "c2": make_consts2(), "c3": make_consts3(), "c4": make_consts4(), "cn": cn, "sn": sn,
        "modw0": inp["mod_w"][0], "modw1": inp["mod_w"][1],
        "vecs0": make_vecs(inp, 0), "vecs1": make_vecs(inp, 1), "v0": make_vecs0(inp), "v1": make_vecs1(inp),
        "win0": inp["ab_w_in"][0], "wout0": inp["ab_w_out"][0], "poolw": inp["pool_w"][0],
        "win1": inp["cd_w_in"][0], "lrw": make_lrw(inp), "gup": make_gup(inp), "wout1": inp["cd_w_out"][0],
        "fw": inp["fourier_w"][0], "rt0": make_router(inp, 0), "rt1": make_router(inp, 1),
        "w1_0": inp["moe_w1"][0], "w3_0": inp["moe_w3"][0], "w2_0": inp["moe_w2"][0].reshape(32 * 512, D),
        "w1_1": inp["moe_w1"][1], "w3_1": inp["moe_w3"][1], "w2_1": inp["moe_w2"][1].reshape(32 * 512, D),
        "fnw": np.ascontiguousarray(np.broadcast_to(inp["final_norm_w"].reshape(1, D), (128, D))),
    }
    return {k_: np.ascontiguousarray(v) for k_, v in s.items()}


N_CORES = 4


def kernel(**inputs):
    inp = {k_: np.asarray(v) for k_, v in inputs.items()}
    k = build_full()
    shared = host_shared(inp)
    in_maps = [host_inputs(inp, b, shared) for b in range(N_CORES)]
    res = run_bass_kernel_spmd(k.nc, in_maps, core_ids=list(range(N_CORES)))
    out = np.stack([np.asarray(res.results[b]["out"]) for b in range(N_CORES)], 0)
    return out.astype(np.float32)


def _stage_moe_sparse(self, L, fT32_d, ftm_d, rt_d, c2_d, w1_d, w3_d, w2_d, src_d, dst_d, tiles=range(NT)):
    p, A = self.p, self.A
    tiles = list(tiles)
    ntl = len(tiles)
    NB = (2 * ntl * 128) // 128 + 32
    NJ = ntl
    tg = f"m{L}"
    w1b_d = self.scratch(tg + "w1b", [4096, 16384], BF16)
    w3b_d = self.scratch(tg + "w3b", [4096, 16384], BF16)
    w2b_d = self.scratch(tg + "w2b", [4096, 16384], BF16)
    xbuf_d = self.scratch(tg + "xbuf", [NB * 128, D], BF16)
    ybuf_d = self.scratch(tg + "ybuf", [NB * 128, D], F32)
    idx_d = self.scratch(tg + "idx", [NT, 128, 2], I32)
    wts_d = self.scratch(tg + "wts", [NT, 128, 2], F32)
    widx_d = self.scratch(tg + "widx", [128, NB], I32)

    A.reset()
    stg = [A.f32(4096) for _ in range(3)]
    wbf = [A.bf16(4096) for _ in range(3)]
    cnt = 0
    ce = ["pool", "dve", "act"]
    for ex in range(32):
        for (srcw, dstw, kind) in ((w1_d, w1b_d, 0), (w3_d, w3b_d, 0), (w2_d, w2b_d, 1)):
            for q in range(4):
                i = cnt % 3
                cnt += 1
                if kind == 0:
                    src = srcw[ex].rearrange("(k p) c -> p k c", p=128)[:, q * 8:(q + 1) * 8, :]
                    p.dma("sp", stg[i].rearrange("p (k c) -> p k c", k=8), src, w=[("stgA", i)])
                else:
                    p.dma("sp", stg[i], srcw[ex * 512 + q * 128:ex * 512 + (q + 1) * 128, :], w=[("stgA", i)])
                if ce[i] == "act":
                    p.op("act", lambda e, i=i: e.copy(out=wbf[i], in_=stg[i]), r=[("stgA", i)], w=[("wbfA", i)])
                else:
                    p.op(ce[i], lambda e, i=i: e.tensor_copy(out=wbf[i], in_=stg[i]), r=[("stgA", i)], w=[("wbfA", i)])
                p.dma("act", dstw[ex * 128:(ex + 1) * 128, q * 4096:(q + 1) * 4096], wbf[i], r=[("wbfA", i)], w=["wbd"])
    p.barrier()

    A.reset()
    rt = A.f32(NRT)
    p.dma("sp", rt, rt_d, w=["rt"])
    wr = rt[:, 0:KC * 36].rearrange("p (k c) -> p k c", k=KC)
    rb = rt[:, KC * 36:KC * 36 + 36]
    ltm = A.f32(128)
    p.dma("sp", ltm, c2_d[:, C2_MBS:C2_MBS + 128], w=["ltm"])
    ft = [A.f32(D) for _ in range(2)]
    gate = A.f32(NT * 32)
    M_all = A.f32(NT * 32)
    pos_all = A.f32(NT * 32)
    cum = A.f32(32)
    p.op("pool", lambda e: e.memset(cum, 0.0), w=["cum"])

    def T(n):
        return A.f32(n)
    lg, ohg, eg, sel, oh1, sel2, oh2, g8, ohs = T(36), T(4), T(4), T(8), T(8), T(8), T(8), T(8), T(8)
    gmax, ngmax, sumeg, pg, top1, top2, dlt, e21, den, w1, w2 = [T(1) for _ in range(11)]
    R = "rtr"

    def dv(fn, extra_r=(), extra_w=()):
        p.op("dve", fn, r=[R] + list(extra_r), w=[R] + list(extra_w))

    def ac(fn):
        p.op("act", fn, r=[R], w=[R])

    for it, t in enumerate(tiles):
        b = it % 2
        p.dma("sp" if it % 2 else "pool", ft[b], fT32_d[t], w=[("ft", b)])
        ft3 = ft[b].rearrange("p (k c) -> p k c", k=KC)
        ps = self.psb[it % 2]
        pk = ("pg", 0, it % 2)
        for kc in range(KC):
            p.op("pe", lambda e, ps=ps, ft3=ft3, kc=kc: e.matmul(ps[:, 0:36], lhsT=ft3[:, kc, :], rhs=wr[:, kc, :],
                                                                 start=(kc == 0), stop=(kc == KC - 1)),
                 r=[("ft", b), "rt"], w=[pk])
        dv(lambda e, ps=ps: e.tensor_tensor(out=lg, in0=ps[:, 0:36], in1=rb, op=ALU.add), [pk, "rt"])
        dv(lambda e: e.tensor_reduce(out=gmax, in_=lg[:, 0:4], axis=AX.X, op=ALU.max))
        dv(lambda e: e.tensor_scalar(out=ohg, in0=lg[:, 0:4], scalar1=gmax[:, 0:1], scalar2=None, op0=ALU.is_equal))
        dv(lambda e: e.tensor_scalar(out=ngmax, in0=gmax, scalar1=-1.0, scalar2=None, op0=ALU.mult))
        ac(lambda e: e.activation(out=eg, in_=lg[:, 0:4], func=AF.Exp, bias=ngmax[:, 0:1]))
        dv(lambda e: e.tensor_reduce(out=sumeg, in_=eg, axis=AX.X, op=ALU.add))
        dv(lambda e: e.reciprocal(out=pg, in_=sumeg))
        dv(lambda e: e.tensor_scalar(out=sel, in0=lg[:, 4:12], scalar1=ohg[:, 0:1], scalar2=None, op0=ALU.mult))
        for g in range(1, 4):
            dv(lambda e, g=g: e.scalar_tensor_tensor(out=sel, in0=lg[:, 4 + 8 * g:12 + 8 * g], scalar=ohg[:, g:g + 1], in1=sel,
                                                      op0=ALU.mult, op1=ALU.add))
        dv(lambda e: e.tensor_reduce(out=top1, in_=sel, axis=AX.X, op=ALU.max))
        dv(lambda e: e.tensor_scalar(out=oh1, in0=sel, scalar1=top1[:, 0:1], scalar2=None, op0=ALU.is_equal))
        dv(lambda e: e.scalar_tensor_tensor(out=sel2, in0=oh1, scalar=-1.0e30, in1=sel, op0=ALU.mult, op1=ALU.add))
        dv(lambda e: e.tensor_reduce(out=top2, in_=sel2, axis=AX.X, op=ALU.max))
        dv(lambda e: e.tensor_scalar(out=oh2, in0=sel2, scalar1=top2[:, 0:1], scalar2=None, op0=ALU.is_equal))
        dv(lambda e: e.tensor_tensor(out=dlt, in0=top2, in1=top1, op=ALU.subtract))
        ac(lambda e: e.activation(out=e21, in_=dlt, func=AF.Exp))
        dv(lambda e: e.tensor_scalar(out=den, in0=e21, scalar1=1.0, scalar2=None, op0=ALU.add))
        dv(lambda e: e.reciprocal(out=w1, in_=den))
        dv(lambda e: e.tensor_tensor(out=w2, in0=e21, in1=w1, op=ALU.mult))
        dv(lambda e: e.tensor_tensor(out=w1, in0=w1, in1=pg, op=ALU.mult))
        dv(lambda e: e.tensor_tensor(out=w2, in0=w2, in1=pg, op=ALU.mult))
        dv(lambda e: e.tensor_scalar(out=g8, in0=oh1, scalar1=w1[:, 0:1], scalar2=None, op0=ALU.mult))
        dv(lambda e: e.scalar_tensor_tensor(out=g8, in0=oh2, scalar=w2[:, 0:1], in1=g8, op0=ALU.mult, op1=ALU.add))
        dv(lambda e: e.tensor_tensor(out=ohs, in0=oh1, in1=oh2, op=ALU.add))
        for g in range(4):
            dv(lambda e, g=g, it=it: e.tensor_scalar(out=gate[:, it * 32 + g * 8:it * 32 + (g + 1) * 8], in0=g8,
                                                     scalar1=ohg[:, g:g + 1], scalar2=None, op0=ALU.mult), (), [("gateB", it)])
            dv(lambda e, g=g, it=it: e.tensor_scalar(out=M_all[:, it * 32 + g * 8:it * 32 + (g + 1) * 8], in0=ohs,
                                                     scalar1=ohg[:, g:g + 1], scalar2=None, op0=ALU.mult), (), [("Mall", it)])
        ps2 = self.psb[2 + it % 2]
        pk2 = ("pg", 1, it % 2)
        Mt = M_all[:, it * 32:(it + 1) * 32]
        p.op("pe", lambda e, ps2=ps2, Mt=Mt: e.matmul(ps2[:, 0:32], lhsT=ltm, rhs=Mt, start=True, stop=True),
             r=[("Mall", it), "ltm"], w=[pk2])
        p.op("pe", lambda e, ps2=ps2, Mt=Mt: e.matmul(ps2[:, 32:64], lhsT=self.ones, rhs=Mt, start=True, stop=True),
             r=[("Mall", it), "consts"], w=[pk2])
        p.op("dve", lambda e, ps2=ps2, it=it: e.tensor_tensor(out=pos_all[:, it * 32:(it + 1) * 32], in0=ps2[:, 0:32], in1=cum, op=ALU.add),
             r=[pk2, "cum"], w=[("posall", it)])
        p.op("dve", lambda e, ps2=ps2: e.tensor_tensor(out=cum, in0=ps2[:, 32:64], in1=cum, op=ALU.add),
             r=[pk2, "cum", ("posall", it)], w=["cum"])
    nblk, padded, pend, pstart = T(32), T(32), T(32), T(32)
    p.op("pool", lambda e: e.memset(nblk, 0.0), w=["nblk"])
    for j in range(NJ):
        p.op("dve", lambda e, j=j: e.scalar_tensor_tensor(out=nblk, in0=cum, scalar=float(128 * j), in1=nblk, op0=ALU.is_gt, op1=ALU.add),
             r=["cum", "nblk"], w=["nblk"])
    p.op("dve", lambda e: e.tensor_scalar(out=padded, in0=nblk, scalar1=128.0, scalar2=None, op0=ALU.mult), r=["nblk"], w=["padded"])
    p.op("dve", lambda e: e.tensor_tensor_scan(out=pend, data0=self.ones[:, 0:32], data1=padded, initial=0.0, op0=ALU.mult, op1=ALU.add),
         r=["padded", "consts"], w=["pend"])
    p.op("dve", lambda e: e.tensor_tensor(out=pstart, in0=pend, in1=padded, op=ALU.subtract), r=["pend", "padded"], w=["pstart"])
    cmp_all = A.f32(NB * 32)
    be = A.f32(NB)
    same = A.f32(NB)
    wif = A.f32(NB)
    wii = A.f32(NB).bitcast(I32)
    for bb in range(NB):
        p.op("dve", lambda e, bb=bb: e.tensor_scalar(out=cmp_all[:, bb * 32:(bb + 1) * 32], in0=pend, scalar1=float(128 * bb), scalar2=None,
                                                     op0=ALU.is_le), r=["pend"], w=["cmpall"])
    p.op("dve", lambda e: e.tensor_reduce(out=be, in_=cmp_all.rearrange("p (b e) -> p b e", e=32), axis=AX.X, op=ALU.add),
         r=["cmpall"], w=["be"])
    p.op("dve", lambda e: e.tensor_scalar(out=be, in0=be, scalar1=31.0, scalar2=None, op0=ALU.min), r=["be"], w=["be"])
    p.op("pool", lambda e: e.memset(same, 0.0), w=["same"])
    p.op("dve", lambda e: e.tensor_tensor(out=same[:, 1:NB], in0=be[:, 1:NB], in1=be[:, 0:NB - 1], op=ALU.is_equal),
         r=["be", "same"], w=["same"])
    p.op("dve", lambda e: e.tensor_scalar(out=wif, in0=be, scalar1=128.0, scalar2=self.consts[:, C_IOTA:C_IOTA + 1], op0=ALU.mult, op1=ALU.add),
         r=["be", "consts"], w=["wif"])
    p.op("dve", lambda e: e.scalar_tensor_tensor(out=wif, in0=same, scalar=1.0e6, in1=wif, op0=ALU.mult, op1=ALU.add),
         r=["same", "wif"], w=["wif"])
    p.op("dve", lambda e: e.tensor_copy(out=wii, in_=wif), r=["wif"], w=["wii"])
    p.dma("sp", widx_d, wii, r=["wii"], w=["widxd"])
    v, ohA, v2 = T(32), T(32), T(32)
    dA, dB, wA, wB, wsum = T(1), T(1), T(1), T(1), T(1)
    oi = [A.f32(2) for _ in range(2)]
    ow = [A.f32(2) for _ in range(2)]
    for it, t in enumerate(tiles):
        b = it % 2
        Mt = M_all[:, it * 32:(it + 1) * 32]
        gt_ = gate[:, it * 32:(it + 1) * 32]
        oib = oi[b].bitcast(I32)
        dv(lambda e, it=it: e.tensor_tensor(out=v, in0=pos_all[:, it * 32:(it + 1) * 32], in1=pstart, op=ALU.add),
           [("posall", it), "pstart"])
        dv(lambda e, Mt=Mt: e.scalar_tensor_tensor(out=v, in0=v, scalar=1.0, in1=Mt, op0=ALU.add, op1=ALU.mult), [("Mall", it)])
        dv(lambda e: e.tensor_reduce(out=dA, in_=v, axis=AX.X, op=ALU.max))
        dv(lambda e: e.tensor_scalar(out=ohA, in0=v, scalar1=dA[:, 0:1], scalar2=None, op0=ALU.is_equal))
        dv(lambda e: e.tensor_scalar(out=v2, in0=ohA, scalar1=-1.0, scalar2=1.0, op0=ALU.mult, op1=ALU.add))
        dv(lambda e: e.tensor_tensor(out=v2, in0=v2, in1=v, op=ALU.mult))
        dv(lambda e: e.tensor_reduce(out=dB, in_=v2, axis=AX.X, op=ALU.max))
        dv(lambda e, gt_=gt_: e.tensor_tensor(out=ohA, in0=ohA, in1=gt_, op=ALU.mult), [("gateB", it)])
        dv(lambda e: e.tensor_reduce(out=wA, in_=ohA, axis=AX.X, op=ALU.add))
        dv(lambda e, gt_=gt_: e.tensor_reduce(out=wsum, in_=gt_, axis=AX.X, op=ALU.add), [("gateB", it)])
        dv(lambda e, b=b: e.tensor_copy(out=ow[b][:, 0:1], in_=wA), (), [("ow", b)])
        dv(lambda e, b=b: e.tensor_tensor(out=ow[b][:, 1:2], in0=wsum, in1=wA, op=ALU.subtract), (), [("ow", b)])
        dv(lambda e, oib=oib: e.tensor_scalar(out=oib[:, 0:1], in0=dA, scalar1=-1.0, scalar2=None, op0=ALU.add), (), [("oi", b)])
        dv(lambda e, oib=oib: e.tensor_scalar(out=oib[:, 1:2], in0=dB, scalar1=-1.0, scalar2=None, op0=ALU.add), (), [("oi", b)])
        p.dma("sp", idx_d[t], oib, r=[("oi", b)], w=["idxd"])
        p.dma("sp", wts_d[t], ow[b], r=[("ow", b)], w=["wtsd"])
    p.barrier()

    A.reset()
    zt = A.bf16(D)
    p.op("pool", lambda e: e.memset(zt, 0.0), w=["zt"])
    for bb in range(NB):
        p.dma("sp" if bb % 2 else "act", xbuf_d[bb * 128:(bb + 1) * 128, :], zt, r=["zt"], w=["xbufz"])
    p.barrier()
    fr = [A.bf16(D) for _ in range(2)]
    ii = [A.f32(2).bitcast(I32) for _ in range(2)]
    nrow = NB * 128
    for it, t in enumerate(tiles):
        b = it % 2
        p.dma("sp", fr[b], ftm_d[t * 128:(t + 1) * 128, :], w=[("fr", b)])
        p.dma("sp", ii[b], idx_d[t], w=[("ii", b)])
        for s_ in range(2):
            p.dmaop("pool", lambda e, b=b, s_=s_: e.indirect_dma_start(
                out=xbuf_d, out_offset=bass.IndirectOffsetOnAxis(ap=ii[b][:, s_:s_ + 1], axis=0), in_=fr[b], in_offset=None,
                bounds_check=p.reg(e, nrow - 1), oob_is_err=False), r=[("fr", b), ("ii", b)], w=["xbufs"])
    p.barrier()

    A.reset()
    wix = A.f32(NB).bitcast(I32)
    p.dma("sp", wix, widx_d, w=["wix"])
    identb = A.bf16(128)
    p.op("dve", lambda e: e.tensor_copy(out=identb, in_=self.ident), r=["consts"], w=["identb"])
    w1b = A.bf16(16384)
    w3b = A.bf16(16384)
    w2b = A.bf16(16384)
    xb = [A.bf16(D) for _ in range(2)]
    xT = A.bf16(D)
    s_sb = A.f32(512)
    a_bf = A.bf16(512)
    aT = A.bf16(512)
    yb = [A.f32(D) for _ in range(2)]
    for bb in range(NB):
        b = bb % 2
        for (wb_, wd_, key) in ((w1b, w1b_d, "w1b"), (w3b, w3b_d, "w3b"), (w2b, w2b_d, "w2b")):
            p.dmaop("pool", lambda e, wb_=wb_, wd_=wd_, bb=bb: e.indirect_dma_start(
                out=wb_, out_offset=None, in_=wd_, in_offset=bass.IndirectOffsetOnAxis(ap=wix[:, bb:bb + 1], axis=0),
                bounds_check=p.reg(e, 4095), oob_is_err=False), r=["wix"], w=[key])
        p.dma("sp", xb[b], xbuf_d[bb * 128:(bb + 1) * 128, :], w=[("xb", b)])
        for q in range(8):
            pst = self.psb[q % 2]
            pk = ("pg", 0, q % 2)
            pstb = pst.bitcast(BF16)
            for j in range(4):
                kc = q * 4 + j
                p.op("pe", lambda e, pstb=pstb, j=j, kc=kc, b=b: e.transpose(pstb[:, j * 128:(j + 1) * 128],
                                                                            xb[b][:, kc * 128:(kc + 1) * 128], identb),
                     r=[("xb", b), "identb"], w=[pk])
            if q % 2 == 0:
                p.op("act", lambda e, pstb=pstb, q=q: e.copy(out=xT[:, q * 512:(q + 1) * 512], in_=pstb[:, 0:512]), r=[pk], w=[("xT", q)])
            else:
                p.op("dve", lambda e, pstb=pstb, q=q: e.tensor_copy(out=xT[:, q * 512:(q + 1) * 512], in_=pstb[:, 0:512]), r=[pk], w=[("xT", q)])
        psA, psB = self.psb[2], self.psb[3]
        for (ps_, wb_, key, pk) in ((psA, w1b, "w1b", ("pg", 1, 0)), (psB, w3b, "w3b", ("pg", 1, 1))):
            for kc in range(KC):
                p.op("pe", lambda e, ps_=ps_, wb_=wb_, kc=kc: e.matmul(ps_, lhsT=xT[:, kc * 128:(kc + 1) * 128],
                                                                      rhs=wb_[:, kc * 512:(kc + 1) * 512], start=(kc == 0), stop=(kc == KC - 1)),
                     r=[("xT", kc // 4), key], w=[pk])
        p.op("act", lambda e, psA=psA: e.activation(out=s_sb, in_=psA, func=AF.Silu), r=[("pg", 1, 0)], w=["s_sb"])
        p.op("dve", lambda e, psB=psB: e.tensor_tensor(out=a_bf, in0=s_sb, in1=psB, op=ALU.mult), r=["s_sb", ("pg", 1, 1)], w=["a_bf"])
        pst = self.psb[0]
        pstb = pst.bitcast(BF16)
        for j in range(4):
            p.op("pe", lambda e, pstb=pstb, j=j: e.transpose(pstb[:, j * 128:(j + 1) * 128], a_bf[:, j * 128:(j + 1) * 128], identb),
                 r=["a_bf", "identb"], w=[("pg", 0, 0)])
        p.op("act", lambda e, pstb=pstb: e.copy(out=aT, in_=pstb[:, 0:512]), r=[("pg", 0, 0)], w=["aT"])
        for cb in range(8):
            psy = self.psb[4 + cb % 4]
            pk = ("pT", cb % 4)
            for j in range(4):
                p.op("pe", lambda e, psy=psy, j=j, cb=cb: e.matmul(psy, lhsT=aT[:, j * 128:(j + 1) * 128],
                                                                  rhs=w2b[:, j * 4096 + cb * 512:j * 4096 + (cb + 1) * 512],
                                                                  start=(j == 0), stop=(j == 3)), r=["aT", "w2b"], w=[pk])
            if cb % 2 == 0:
                p.op("act", lambda e, psy=psy, cb=cb, b=b: e.copy(out=yb[b][:, cb * 512:(cb + 1) * 512], in_=psy), r=[pk], w=[("yb", b, cb)])
            else:
                p.op("dve", lambda e, psy=psy, cb=cb, b=b: e.tensor_copy(out=yb[b][:, cb * 512:(cb + 1) * 512], in_=psy), r=[pk], w=[("yb", b, cb)])
        p.dma("act", ybuf_d[bb * 128:(bb + 1) * 128, :], yb[b], r=[("yb", b, cb) for cb in range(8)], w=["ybufd"])
    p.barrier()

    A.reset()
    g2r = {"L": A.f32(D), "C": A.f32(D)}
    p.dma("sp", g2r["L"], self.gbc_d[2], w=["g2L"])
    p.dma("sp", g2r["C"], self.gbc_d[3], w=["g2C"])
    yA = [A.f32(D) for _ in range(2)]
    yB = [A.f32(D) for _ in range(2)]
    xs = [A.f32(D) for _ in range(2)]
    ii2 = [A.f32(2).bitcast(I32) for _ in range(2)]
    ww = [A.f32(2) for _ in range(2)]
    for it, t in enumerate(tiles):
        b = it % 2
        side = "C" if t < 2 else "L"
        tsl = slice(t * 128, (t + 1) * 128)
        p.dma("sp", ii2[b], idx_d[t], w=[("ii2", b)])
        p.dma("sp", ww[b], wts_d[t], w=[("ww", b)])
        p.dma("sp", xs[b], src_d[tsl, :], w=[("xsE", b)])
        for (yy, s_, key) in ((yA, 0, "yA"), (yB, 1, "yB")):
            p.dmaop("pool", lambda e, yy=yy, s_=s_, b=b: e.indirect_dma_start(
                out=yy[b], out_offset=None, in_=ybuf_d, in_offset=bass.IndirectOffsetOnAxis(ap=ii2[b][:, s_:s_ + 1], axis=0),
                bounds_check=p.reg(e, nrow - 1), oob_is_err=False), r=[("ii2", b)], w=[(key, b)])
        p.op("dve", lambda e, b=b: e.tensor_scalar(out=yA[b], in0=yA[b], scalar1=ww[b][:, 0:1], scalar2=None, op0=ALU.mult),
             r=[("yA", b), ("ww", b)], w=[("yA", b)])
        p.op("dve", lambda e, b=b: e.scalar_tensor_tensor(out=yA[b], in0=yB[b], scalar=ww[b][:, 1:2], in1=yA[b], op0=ALU.mult, op1=ALU.add),
             r=[("yA", b), ("yB", b), ("ww", b)], w=[("yA", b)])
        p.op("pool", lambda e, b=b, side=side: e.tensor_tensor(out=yA[b], in0=yA[b], in1=g2r[side], op=ALU.mult),
             r=[("yA", b), "g2" + side], w=[("yA", b)])
        p.op("pool", lambda e, b=b: e.tensor_tensor(out=yA[b], in0=yA[b], in1=xs[b], op=ALU.add),
             r=[("yA", b), ("xsE", b)], w=[("yA", b)])
        p.dma("act", dst_d[tsl, :], yA[b], r=[("yA", b)], w=["dstE"])
    p.barrier()


K.stage_moe_sparse = _stage_moe_sparse
```
